# Optimizing a Trainium2 kernel written in Bass

```python
import jax, jax.numpy as jnp
from jax import lax
import numpy as np

D_MODEL = 2048
BATCH = 8
SEQ = 2048
DEPTH = 2

N_MIXERS = 2
NORM_EPS = 1e-6
GATED_NORM_EPS = 1e-5

SSM_EXPAND = 2
D_INNER = SSM_EXPAND * D_MODEL
SSM_HEAD_DIM = 64
SSM_HEADS = D_INNER // SSM_HEAD_DIM
SSM_GROUPS = 8
SSM_STATE = 128
SSM_CONV = 5
SSD_CHUNK = 128
D_XBC = D_INNER + 2 * SSM_GROUPS * SSM_STATE
D_SSM_IN = D_INNER + D_XBC + 2 * SSM_HEADS

GMLP_CHUNK = 128
D_GATE = 2 * D_MODEL
GMLP_GROUPS = 16
GMLP_GROUP_DIM = D_GATE // GMLP_GROUPS

N_EXPERTS = 32
TOP_K = 4
D_EXPERT = D_MODEL
SWIGLU_ALPHA = 1.702
SWIGLU_LIMIT = 7.0
MOE_BLOCK = 128

kernel_name = 'bidir_hybrid_ssd_gmlp_moe_adaln'

F32 = jnp.float32


def rmsnorm(x, g, eps=NORM_EPS):
    xf = x.astype(F32)
    y = xf * lax.rsqrt(jnp.mean(xf * xf, axis=-1, keepdims=True) + eps)
    return (y * g.astype(F32)).astype(x.dtype)


def modulate(h, shift, scale):
    return h * (1 + scale[:, None, :]) + shift[:, None, :]


def centred_depthwise_conv(x, w, b):
    k = w.shape[0]
    y = lax.conv_general_dilated(x, w[:, None, :].astype(x.dtype), window_strides=(1,),
                                 padding=[(k // 2, k // 2)],
                                 dimension_numbers=('NWC', 'WIO', 'NWC'),
                                 feature_group_count=x.shape[-1])
    return y + b.astype(x.dtype)


def ssd_chunked(x, dt, a, bm, cm):
    bsz, seq, n_heads, p = x.shape
    g, n = bm.shape[-2:]
    r = n_heads // g
    q = SSD_CHUNK
    nc = seq // q
    xc = x.astype(F32).reshape(bsz, nc, q, g, r, p)
    dtc = dt.reshape(bsz, nc, q, g, r)
    bc = bm.astype(F32).reshape(bsz, nc, q, g, n)
    cc = cm.astype(F32).reshape(bsz, nc, q, g, n)
    a_cum = jnp.cumsum(dtc * a.reshape(g, r), axis=2)
    a_cum_t = jnp.moveaxis(a_cum, 2, -1)
    dtx = xc * dtc[..., None]
    seg = a_cum_t[..., :, None] - a_cum_t[..., None, :]
    lower = jnp.tril(jnp.ones((q, q), bool))
    decay = jnp.exp(jnp.where(lower, seg, -jnp.inf))
    scores = jnp.einsum('bcign,bcjgn->bcgij', cc, bc)
    y_diag = jnp.einsum('bcgrij,bcjgrp->bcigrp', scores[:, :, :, None] * decay, dtx)
    decay_to_end = jnp.exp(a_cum_t[..., -1:] - a_cum_t)
    states = jnp.einsum('bcjgn,bcgrj,bcjgrp->bcgrpn', bc, decay_to_end, dtx)
    chunk_decay = jnp.exp(a_cum_t[..., -1])

    def step(h, inp):
        st, dec = inp
        return h * dec[..., None, None] + st, h

    h0 = jnp.zeros((bsz, g, r, p, n), F32)
    _, prev = lax.scan(step, h0, (jnp.moveaxis(states, 1, 0), jnp.moveaxis(chunk_decay, 1, 0)))
    prev = jnp.moveaxis(prev, 0, 1)
    y_off = jnp.einsum('bcign,bcgrpn->bcigrp', cc, prev) * jnp.exp(a_cum)[..., None]
    return (y_diag + y_off).reshape(bsz, seq, n_heads, p)


def gated_rmsnorm(y, z, g):
    yf = (y.astype(F32) * jax.nn.silu(z.astype(F32))).reshape(*y.shape[:-1], SSM_GROUPS, -1)
    yf = yf * lax.rsqrt(jnp.mean(yf * yf, axis=-1, keepdims=True) + GATED_NORM_EPS)
    return (yf.reshape(y.shape) * g.astype(F32)).astype(y.dtype)


def mamba2_bidir_mixer(h, w_in, conv_w, conv_b, dt_bias, a_log, d_skip, norm_g, w_out):
    bsz, seq, _ = h.shape
    zxbcdt = h @ w_in
    z = zxbcdt[..., :D_INNER]
    xbc = jax.nn.silu(centred_depthwise_conv(zxbcdt[..., D_INNER:D_INNER + D_XBC], conv_w, conv_b))
    dt_raw = zxbcdt[..., D_INNER + D_XBC:].reshape(bsz, seq, 2, SSM_HEADS)
    xs = xbc[..., :D_INNER].reshape(bsz, seq, SSM_HEADS, SSM_HEAD_DIM)
    bm = xbc[..., D_INNER:D_INNER + SSM_GROUPS * SSM_STATE].reshape(bsz, seq, SSM_GROUPS, SSM_STATE)
    cm = xbc[..., D_INNER + SSM_GROUPS * SSM_STATE:].reshape(bsz, seq, SSM_GROUPS, SSM_STATE)
    dt = jax.nn.softplus(dt_raw.astype(F32) + dt_bias.astype(F32))
    a = -jnp.exp(a_log.astype(F32))
    flip = lambda t: jnp.flip(t, axis=1)
    y_fwd = ssd_chunked(xs, dt[:, :, 0], a[0], bm, cm)
    y_bwd = flip(ssd_chunked(flip(xs), flip(dt[:, :, 1]), a[1], flip(bm), flip(cm)))
    y = y_fwd + y_bwd + d_skip.astype(F32)[:, None] * xs.astype(F32)
    y = y.reshape(bsz, seq, D_INNER).astype(h.dtype)
    return gated_rmsnorm(y, z, norm_g) @ w_out


def gmlp_chunk_mixer(h, w_in, b_in, ln_g, ln_b, w_s, b_s, w_out, b_out):
    bsz, seq, _ = h.shape
    uv = jax.nn.gelu(h @ w_in + b_in, approximate=False)
    u, v = uv[..., :D_GATE], uv[..., D_GATE:]
    vf = v.astype(F32)
    mu = jnp.mean(vf, axis=-1, keepdims=True)
    var = jnp.mean(jnp.square(vf - mu), axis=-1, keepdims=True)
    v = ((vf - mu) * lax.rsqrt(var + NORM_EPS) * ln_g.astype(F32) + ln_b.astype(F32)).astype(h.dtype)
    nc = seq // GMLP_CHUNK
    v = v.reshape(bsz, nc, GMLP_CHUNK, GMLP_GROUPS, GMLP_GROUP_DIM)
    v = jnp.einsum('gij,bcjgd->bcigd', w_s, v) + b_s.T[:, :, None]
    return (u * v.reshape(bsz, seq, D_GATE)) @ w_out + b_out


def clamped_swiglu(gu):
    gate, lin = gu[..., 0::2], gu[..., 1::2]
    gate = jnp.minimum(gate, SWIGLU_LIMIT)
    lin = jnp.clip(lin, -SWIGLU_LIMIT, SWIGLU_LIMIT)
    return gate * jax.nn.sigmoid(SWIGLU_ALPHA * gate) * (lin + 1)


def moe_ffn(h, w_router, b_router, w_up, b_up, w_down, b_down):
    t, d = h.shape
    logits = h.astype(F32) @ w_router.astype(F32) + b_router.astype(F32)
    top_logits, top_idx = lax.top_k(logits, TOP_K)
    gates = jax.nn.softmax(top_logits, axis=-1)
    n_assign = t * TOP_K
    flat_e = top_idx.reshape(-1)
    order = jnp.argsort(flat_e, stable=True)
    sorted_e = flat_e[order]
    counts = jnp.bincount(flat_e, length=N_EXPERTS)
    padded = (counts + MOE_BLOCK - 1) // MOE_BLOCK * MOE_BLOCK
    ends_pad = jnp.cumsum(padded)
    start_pad = ends_pad - padded
    start = jnp.cumsum(counts) - counts
    dest = start_pad[sorted_e] + (jnp.arange(n_assign) - start[sorted_e])
    n_rows = -(-n_assign // MOE_BLOCK) * MOE_BLOCK + N_EXPERTS * MOE_BLOCK
    n_blocks = n_rows // MOE_BLOCK
    row_token = jnp.full((n_rows,), t, jnp.int32).at[dest].set((order // TOP_K).astype(jnp.int32))
    row_gate = jnp.zeros((n_rows,), F32).at[dest].set(gates.reshape(-1)[order])
    block_expert = jnp.minimum(
        jnp.searchsorted(ends_pad, jnp.arange(n_blocks) * MOE_BLOCK, side='right'), N_EXPERTS - 1)
    h_pad = jnp.concatenate([h, jnp.zeros((1, d), h.dtype)], axis=0)
    xb = h_pad[row_token].reshape(n_blocks, MOE_BLOCK, d)

    def expert_block(args):
        xblk, e = args
        gu = xblk @ w_up[e] + b_up[e]
        return clamped_swiglu(gu) @ w_down[e] + b_down[e]

    yb = lax.map(expert_block, (xb, block_expert))
    y_rows = yb.reshape(n_rows, d) * row_gate[:, None].astype(h.dtype)
    out = jnp.zeros((t + 1, d), h.dtype).at[row_token].add(y_rows)
    return out[:t]


def setup_inputs(seed: int = 0) -> dict:
    key = jax.random.key(seed)
    ks = iter(jax.random.split(key, 40))
    n_a = (DEPTH + 1) // 2
    n_b = DEPTH // 2
    nrm = lambda shape, s: jax.random.normal(next(ks), shape, F32) * s
    dt0 = jnp.exp(jax.random.uniform(next(ks), (n_a, 2, SSM_HEADS), F32, np.log(1e-3), np.log(1e-1)))
    return {
        'x': nrm((BATCH, SEQ, D_MODEL), 1.0),
        'c': nrm((BATCH, D_MODEL), 1.0),
        'ada_w': nrm((DEPTH, D_MODEL, 6 * D_MODEL), 0.5 * D_MODEL ** -0.5),
        'ada_b': nrm((DEPTH, 6 * D_MODEL), 0.02),
        'norm_mix_g': 1.0 + nrm((DEPTH, D_MODEL), 0.05),
        'norm_ffn_g': 1.0 + nrm((DEPTH, D_MODEL), 0.05),
        'ssm_w_in': nrm((n_a, D_MODEL, D_SSM_IN), D_MODEL ** -0.5),
        'ssm_conv_w': nrm((n_a, SSM_CONV, D_XBC), SSM_CONV ** -0.5),
        'ssm_conv_b': nrm((n_a, D_XBC), 0.02),
        'ssm_dt_bias': dt0 + jnp.log(-jnp.expm1(-dt0)),
        'ssm_a_log': jnp.log(jax.random.uniform(next(ks), (n_a, 2, SSM_HEADS), F32, 1.0, 16.0)),
        'ssm_d': 1.0 + nrm((n_a, SSM_HEADS), 0.1),
        'ssm_norm_g': 1.0 + nrm((n_a, D_INNER), 0.05),
        'ssm_w_out': nrm((n_a, D_INNER, D_MODEL), D_INNER ** -0.5),
        'gmlp_w_in': nrm((n_b, D_MODEL, 2 * D_GATE), D_MODEL ** -0.5),
        'gmlp_b_in': nrm((n_b, 2 * D_GATE), 0.02),
        'gmlp_ln_g': 1.0 + nrm((n_b, D_GATE), 0.05),
        'gmlp_ln_b': nrm((n_b, D_GATE), 0.02),
        'gmlp_w_s': nrm((n_b, GMLP_GROUPS, GMLP_CHUNK, GMLP_CHUNK), GMLP_CHUNK ** -0.5),
        'gmlp_b_s': 1.0 + nrm((n_b, GMLP_GROUPS, GMLP_CHUNK), 0.1),
        'gmlp_w_out': nrm((n_b, D_GATE, D_MODEL), D_GATE ** -0.5),
        'gmlp_b_out': nrm((n_b, D_MODEL), 0.02),
        'moe_w_router': nrm((DEPTH, D_MODEL, N_EXPERTS), D_MODEL ** -0.5),
        'moe_b_router': nrm((DEPTH, N_EXPERTS), 0.01),
        'moe_w_up': nrm((DEPTH, N_EXPERTS, D_MODEL, 2 * D_EXPERT), D_MODEL ** -0.5),
        'moe_b_up': nrm((DEPTH, N_EXPERTS, 2 * D_EXPERT), 0.02),
        'moe_w_down': nrm((DEPTH, N_EXPERTS, D_EXPERT, D_MODEL), D_EXPERT ** -0.5),
        'moe_b_down': nrm((DEPTH, N_EXPERTS, D_MODEL), 0.02),
        'final_g': 1.0 + nrm((D_MODEL,), 0.05),
    }


def reference(x, c, ada_w, ada_b, norm_mix_g, norm_ffn_g,
              ssm_w_in, ssm_conv_w, ssm_conv_b, ssm_dt_bias, ssm_a_log, ssm_d, ssm_norm_g, ssm_w_out,
              gmlp_w_in, gmlp_b_in, gmlp_ln_g, gmlp_ln_b, gmlp_w_s, gmlp_b_s, gmlp_w_out, gmlp_b_out,
              moe_w_router, moe_b_router, moe_w_up, moe_b_up, moe_w_down, moe_b_down, final_g):
    bsz, seq, d = x.shape
    c_act = jax.nn.silu(c)
    for i in range(DEPTH):
        mod = c_act @ ada_w[i] + ada_b[i]
        sh1, sc1, g1, sh2, sc2, g2 = jnp.split(mod, 6, axis=-1)
        h = modulate(rmsnorm(x, norm_mix_g[i]), sh1, sc1)
        j = i // N_MIXERS
        if i % N_MIXERS == 0:
            y = mamba2_bidir_mixer(h, ssm_w_in[j], ssm_conv_w[j], ssm_conv_b[j], ssm_dt_bias[j],
                                   ssm_a_log[j], ssm_d[j], ssm_norm_g[j], ssm_w_out[j])
        else:
            y = gmlp_chunk_mixer(h, gmlp_w_in[j], gmlp_b_in[j], gmlp_ln_g[j], gmlp_ln_b[j],
                                 gmlp_w_s[j], gmlp_b_s[j], gmlp_w_out[j], gmlp_b_out[j])
        x = x + g1[:, None, :] * y
        h = modulate(rmsnorm(x, norm_ffn_g[i]), sh2, sc2)
        y = moe_ffn(h.reshape(bsz * seq, d), moe_w_router[i], moe_b_router[i], moe_w_up[i],
                    moe_b_up[i], moe_w_down[i], moe_b_down[i]).reshape(bsz, seq, d)
        x = x + g2[:, None, :] * y
    return rmsnorm(x, final_g)
```

```python
import numpy as np
import concourse.bass as bass
import concourse.mybir as mybir
from concourse.bass_utils import run_bass_kernel_spmd

F32 = mybir.dt.float32
BF16 = mybir.dt.bfloat16
AF = mybir.ActivationFunctionType
ALU = mybir.AluOpType
AX = mybir.AxisListType

D = 2048
L = 2048
NT = 16
KC = 16
D_INNER = 4096
D_SSM_IN = 10368
import os
N_EXP = 32
DBG_NE = int(os.environ.get("MOE_NE", "32"))
DBG_TB = int(os.environ.get("MOE_TB", "4"))
DBG_LVL = int(os.environ.get("MOE_LVL", "9"))
ENGS = ["tensor", "vector", "scalar", "gpsimd", "sync"]


class Sched:
    def __init__(self, nc):
        self.nc = nc
        self.ops = {e: [] for e in ENGS}
        self.track = {}
        self.dma_cnt = {}
        self.last_dma = {}

    def op(self, eng, fn, reads=(), writes=(), dma=False, semkey=None):
        deps = set()
        for r in reads:
            t = self.track.get(r)
            if t and t["w"] is not None:
                deps.add(t["w"])
        for w in writes:
            t = self.track.get(w)
            if t:
                if t["w"] is not None:
                    deps.add(t["w"])
                deps.update(t["r"])
        idx = len(self.ops[eng])
        if dma:
            if semkey is None:
                semkey = writes[0] if (writes and not str(writes[0]).startswith("dram:")) else reads[0]
            cnt = self.dma_cnt.get(semkey, 0) + 1
            self.dma_cnt[semkey] = cnt
            ref = ("dma", semkey, cnt)
        else:
            ref = ("eng", eng, idx)
        if eng == "tensor":
            deps = {d for d in deps if not (d[0] == "eng" and d[1] == "tensor")}
        self.ops[eng].append(dict(fn=fn, deps=deps, ref=ref, dma=dma, needed=False, semkey=semkey))
        for r in reads:
            self.track.setdefault(r, {"w": None, "r": []})["r"].append(ref)
        for w in writes:
            self.track[w] = {"w": ref, "r": []}
        return ref

    def barrier(self):
        deps = set()
        for e in ENGS:
            for i in range(len(self.ops[e]) - 1, -1, -1):
                o = self.ops[e][i]
                if o["fn"] is not None and not o["dma"]:
                    deps.add(o["ref"])
                    break
        for k, c in self.dma_cnt.items():
            deps.add(("dma", k, c))
        for e in ENGS:
            self.ops[e].append(dict(fn=None, deps=set(deps), ref=None, dma=False, needed=False, semkey=None))
        self.track = {}

    def emit(self, stack):
        nc = self.nc
        for e in ENGS:
            for o in self.ops[e]:
                for d in o["deps"]:
                    if d[0] == "eng":
                        self.ops[d[1]][d[2]]["needed"] = True
        cum = {}
        for e in ENGS:
            c = 0
            arr = []
            for o in self.ops[e]:
                if o["needed"]:
                    c += 1
                arr.append(c)
            cum[e] = arr
        esem = {e: stack.enter_context(nc.semaphore("s_" + e)) for e in ENGS}
        dsem = {}
        for k in self.dma_cnt:
            dsem[k] = stack.enter_context(nc.semaphore("d%d" % len(dsem)))
        block = stack.enter_context(nc.Block())

        def make(e):
            def body(eng):
                waited = {}
                for o in self.ops[e]:
                    for d in sorted(o["deps"], key=lambda z: str(z)):
                        if d[0] == "eng":
                            sem, val, key = esem[d[1]], cum[d[1]][d[2]], ("e", d[1])
                        else:
                            sem, val, key = dsem[d[1]], 16 * d[2], ("d", d[1])
                        if waited.get(key, 0) >= val:
                            continue
                        eng.wait_ge(sem, val)
                        waited[key] = val
                    if o["fn"] is None:
                        continue
                    ins = o["fn"](eng)
                    if o["dma"]:
                        ins.then_inc(dsem[o["semkey"]], 16)
                    elif o["needed"]:
                        ins.then_inc(esem[e], 1)
            return body

        for e in ENGS:
            getattr(block, e)(make(e))


class Arena:
    def __init__(self, nc, limit=229376):
        self.nc = nc
        self.off = 18560
        self.limit = limit
        self.n = 0

    def alloc(self, name, shape, dtype):
        esz = 2 if dtype == BF16 else 4
        per_part = int(np.prod(shape[1:])) * esz
        off = (self.off + 63) // 64 * 64
        assert off + per_part <= self.limit, "SBUF arena overflow %s %d" % (name, off + per_part)
        self.n += 1
        t = self.nc.alloc_sbuf_tensor_at("%s_%d" % (name, self.n), list(shape), dtype, offset=off)
        self.off = off + per_part
        return t

    def mark(self):
        return self.off

    def reset(self, m):
        self.off = m


def build_program(flow=("ssm", "moe0", "gmlp", "moe1", "final"), taps=()):
    import contextlib
    nc = bass.Bass("TRN2", target_bir_lowering=False)
    S = Sched(nc)
    A = Arena(nc)
    stack = contextlib.ExitStack()

    def din(name, shape, dt=F32):
        return nc.dram_tensor(name, list(shape), dt, kind="ExternalInput").ap()

    def dscr(name, shape, dt=F32):
        return nc.dram_tensor(name, list(shape), dt, kind="Internal").ap()

    x_in = din("x", [L, D])
    c_col = din("c_col", [128, KC])
    consts = din("consts", [6, 128, 128])
    ada_w = din("ada_w", [2, D, 6 * D])
    ada_b = din("ada_b", [2, 6 * D])
    ng_col = din("ng_col", [128, 4, KC])
    final_g = din("final_g", [D])
    out = nc.dram_tensor("out", [L, D], F32, kind="ExternalOutput").ap()
    tap_aps = {}
    for (tn, tshape) in taps:
        tap_aps[tn] = nc.dram_tensor("tap_" + tn, list(tshape), F32, kind="ExternalOutput").ap()

    g_w_in = din("g_w_in", [1, D, 8192]); g_b_in = din("g_b_in", [1, 8192])
    g_ln_g = din("g_ln_g", [1, 4096]); g_ln_b = din("g_ln_b", [1, 4096])
    g_w_s = din("g_w_s", [1, 16, 128, 128]); g_bs_col = din("g_bs_col", [128, 16])
    g_w_out = din("g_w_out", [1, 4096, D]); g_b_out = din("g_b_out", [1, D])
    m_wr_col = din("m_wr_col", [128, 2, KC, 32]); m_br = din("m_br", [2, 32])
    has_moe = any(st.startswith("moe") for st in flow)
    m_bup_col = din("m_bup_col", [128, 2, 32, 16, 2]); m_b_down = din("m_b_down", [2, 32, D])
    if has_moe:
        m_w_up = [din("m_w_up%d" % i, [DBG_NE, D, 4096]) for i in range(2)]
        m_w_down = [din("m_w_down%d" % i, [DBG_NE, D, D]) for i in range(2)]
    s_w_in = din("s_w_in", [1, D, D_SSM_IN]); s_conv_col = din("s_conv_col", [128, 48, 5])
    s_convb_col = din("s_convb_col", [128, 48]); s_dtb_col = din("s_dtb_col", [128, 1]); s_alog_col = din("s_alog_col", [128, 1])
    s_d_rep = din("s_d_rep", [4096]); s_norm_g = din("s_norm_g", [4096]); s_w_out = din("s_w_out", [1, 4096, D])
    zs_d = dscr("zs_d", [L, 4096]); xsd = dscr("xsd", [L, 4096])
    bct_d = dscr("bct_d", [16, 128, L], BF16); btok_d = dscr("btok_d", [L, 1024], BF16)
    uv_d = dscr("uv_d", [L, 8192])
    at_d = dscr("at_d", [NT, 128, 32, 128], BF16)
    mod_d = dscr("mod_d", [2, 6 * D])
    xs_d = [dscr("xs%d" % i, [L, D]) for i in range(4)]

    cst = A.alloc("cst", [128, 6, 128], F32)
    ident, tri_le, tri_ge, u_gt, u_lt, ones = [cst[:, i, :] for i in range(6)]
    identb = A.alloc("identb", [128, 128], BF16)
    cstb = A.alloc("cstb", [128, 6, 128], BF16)
    tri_le_b, tri_ge_b, ones_b = cstb[:, 1, :], cstb[:, 2, :], cstb[:, 5, :]
    modcol = A.alloc("modcol", [128, 2, 96], F32)
    ngc = A.alloc("ngc", [128, 4, KC], F32)
    gs_col = A.alloc("gs_col", [128, 4, 2, KC], F32)
    ps = [stack.enter_context(nc.psum_tensor("ps%d" % i, [128, 512], F32)) for i in range(8)]

    S.op("sync", lambda e: e.dma_start(out=cst[:], in_=consts.rearrange("c p n -> p c n")),
         writes=["cst"], dma=True)
    S.op("sync", lambda e: e.dma_start(out=ngc[:], in_=ng_col), writes=["ngc"], dma=True)
    S.op("vector", lambda e: e.tensor_copy(out=identb[:], in_=ident), reads=["cst"], writes=["identb"])
    S.op("vector", lambda e: e.tensor_copy(out=cstb[:], in_=cst[:]), reads=["cst"], writes=["cstb"])

    m0 = A.mark()
    ccol = A.alloc("ccol", [128, KC], F32)
    cact = A.alloc("cact", [128, KC], BF16)
    wa = A.alloc("wa", [128, 2, KC, 512], BF16)
    abrow = A.alloc("abrow", [1, 2, 512], F32)
    mrow = A.alloc("mrow", [1, 2, 512], F32)
    S.op("sync", lambda e: e.dma_start(out=ccol[:], in_=c_col), writes=["ccol"], dma=True)
    S.op("scalar", lambda e: e.activation(out=cact[:], in_=ccol[:], func=AF.Silu), reads=["ccol"], writes=["cact"])
    blk = 0
    for l in range(2):
        for nb in range(24):
            s = blk % 2
            S.op("gpsimd", lambda e, l=l, nb=nb, s=s: e.dma_start(
                out=wa[:, s], in_=ada_w[l, :, nb * 512:(nb + 1) * 512].rearrange("(k p) n -> p k n", p=128)),
                writes=[("wa", s)], dma=True)
            S.op("sync", lambda e, l=l, nb=nb, s=s: e.dma_start(
                out=abrow[:, s, :], in_=ada_b[l:l + 1, nb * 512:(nb + 1) * 512]),
                writes=[("abrow", s)], dma=True)
            pb = blk % 2
            for kc in range(KC):
                S.op("tensor", lambda e, kc=kc, s=s, pb=pb: e.matmul(
                    ps[pb][0:1, :], lhsT=cact[:, kc:kc + 1], rhs=wa[:, s, kc, :],
                    start=(kc == 0), stop=(kc == KC - 1)),
                    reads=["cact", ("wa", s)], writes=[("ps", pb)])
            S.op("vector", lambda e, s=s, pb=pb: e.tensor_tensor(
                out=mrow[:, s, :], in0=ps[pb][0:1, :], in1=abrow[:, s, :], op=ALU.add),
                reads=[("ps", pb), ("abrow", s)], writes=[("mrow", s)])
            S.op("sync", lambda e, l=l, nb=nb, s=s: e.dma_start(
                out=mod_d[l:l + 1, nb * 512:(nb + 1) * 512], in_=mrow[:, s, :]),
                reads=[("mrow", s)], writes=["dram:mod"], dma=True, semkey=("mrow_st", s))
            blk += 1
    mrows = A.alloc("mrows", [96, 128], F32)
    for l in range(2):
        S.op("sync", lambda e, l=l: e.dma_start(out=mrows[:], in_=mod_d[l].rearrange("(j p) -> j p", p=128)),
             reads=["dram:mod"], writes=["mrows"], dma=True)
        S.op("tensor", lambda e: e.transpose(ps[2][:, 0:96], mrows[:], ident[0:96, 0:96]),
             reads=["mrows", "cst"], writes=[("ps", 2)])
        S.op("vector", lambda e, l=l: e.tensor_copy(out=modcol[:, l, :], in_=ps[2][:, 0:96]),
             reads=[("ps", 2)], writes=["modcol"])
    for l in range(2):
        for half in range(2):
            sub = 2 * l + half
            base = 48 * half
            S.op("vector", lambda e, l=l, sub=sub, base=base: e.scalar_tensor_tensor(
                out=gs_col[:, sub, 0, :], in0=modcol[:, l, base + 16:base + 32], scalar=1.0,
                in1=ngc[:, sub, :], op0=ALU.add, op1=ALU.mult),
                reads=["modcol", "ngc"], writes=["gs_col"])
            S.op("vector", lambda e, l=l, sub=sub, base=base: e.tensor_copy(
                out=gs_col[:, sub, 1, :], in_=modcol[:, l, base:base + 16]),
                reads=["modcol"], writes=["gs_col"])
    S.barrier()
    A.reset(m0)
    if "modcol" in tap_aps:
        S.op("sync", lambda e: e.dma_start(out=tap_aps["modcol"], in_=modcol[:].rearrange("p l j -> p (l j)")),
             reads=["modcol"], writes=["dram:tap1"], dma=True, semkey="tap1")

    def prologue(sub, x_src, hT, tiles=None, router=None):
        m = A.mark()
        tiles = list(range(NT)) if tiles is None else tiles
        t0 = tiles[0]
        if router is not None:
            wrh, wrl, brbc, gT = router
            hf = A.alloc("hf", [128, 2, 4, 128], F32)
            hfh = A.alloc("hfh", [128, 2, 4, 128], BF16)
            hfl = A.alloc("hfl", [128, 2, 4, 128], BF16)
            rg = A.alloc("rg", [128, 2, 160], F32)
        xt = A.alloc("xt", [128, 2, D], F32)
        st = A.alloc("st", [128, 2, 4], F32)
        xn1 = A.alloc("xn", [128, D], F32)
        sq = xn1
        for tt in tiles:
            s = tt % 2
            tl = tt - t0
            S.op("sync", lambda e, tt=tt, s=s: e.dma_start(out=xt[:, s, :], in_=x_src[tt * 128:(tt + 1) * 128, :]),
                 writes=[("xt", s)], dma=True)
            S.op("vector", lambda e, s=s: e.tensor_tensor(out=sq[:], in0=xt[:, s, :], in1=xt[:, s, :], op=ALU.mult),
                 reads=[("xt", s)], writes=["xn"])
            S.op("vector", lambda e, s=s: e.reduce_sum(out=st[:, s, 0:1], in_=sq[:], axis=AX.X),
                 reads=["xn"], writes=[("st", s)])
            S.op("vector", lambda e, s=s: e.tensor_scalar(out=st[:, s, 1:2], in0=st[:, s, 0:1], scalar1=1.0 / D,
                                                          scalar2=1e-6, op0=ALU.mult, op1=ALU.add),
                 reads=[("st", s)], writes=[("st", s)])
            S.op("scalar", lambda e, s=s: e.activation(out=st[:, s, 3:4], in_=st[:, s, 1:2], func=AF.Sqrt),
                 reads=[("st", s)], writes=[("st", s)])
            S.op("vector", lambda e, s=s: e.reciprocal(out=st[:, s, 2:3], in_=st[:, s, 3:4]),
                 reads=[("st", s)], writes=[("st", s)])
            S.op("scalar", lambda e, s=s: e.activation(out=xn1[:], in_=xt[:, s, :], func=AF.Copy,
                                                       scale=st[:, s, 2:3]),
                 reads=[("xt", s), ("st", s)], writes=["xn"])
            for q in range(4):
                pb = (tt * 4 + q) % 4
                for j in range(4):
                    kc = q * 4 + j
                    S.op("tensor", lambda e, s=s, kc=kc, pb=pb, j=j: e.transpose(
                        ps[pb][:, j * 128:(j + 1) * 128], xn1[:, kc * 128:(kc + 1) * 128], ident),
                        reads=["xn", "cst"], writes=[("ps", pb)])
                for j in range(4):
                    kc = q * 4 + j
                    S.op("scalar", lambda e, kc=kc, pb=pb, j=j, tl=tl: e.activation(
                        out=hT[:, kc, tl * 128:(tl + 1) * 128], in_=ps[pb][:, j * 128:(j + 1) * 128],
                        func=AF.Identity, scale=gs_col[:, sub, 0, kc:kc + 1], bias=gs_col[:, sub, 1, kc:kc + 1]),
                        reads=[("ps", pb), "gs_col"], writes=[("hT", tl)])
                if router is not None:
                    hs = q % 2
                    for j in range(4):
                        kc = q * 4 + j
                        S.op("vector", lambda e, kc=kc, pb=pb, j=j, hs=hs: e.tensor_scalar(
                            out=hf[:, hs, j, :], in0=ps[pb][:, j * 128:(j + 1) * 128],
                            scalar1=gs_col[:, sub, 0, kc:kc + 1], scalar2=gs_col[:, sub, 1, kc:kc + 1],
                            op0=ALU.mult, op1=ALU.add),
                            reads=["gs_col"], writes=[("hf", hs), ("ps", pb)])
                    S.op("vector", lambda e, hs=hs: e.tensor_copy(out=hfh[:, hs], in_=hf[:, hs]),
                         reads=[("hf", hs)], writes=[("hfh", hs)])
                    S.op("vector", lambda e, hs=hs: e.tensor_tensor(out=hfl[:, hs], in0=hf[:, hs], in1=hfh[:, hs], op=ALU.subtract),
                         reads=[("hf", hs), ("hfh", hs)], writes=[("hfl", hs)])
                    for j in range(4):
                        kc = q * 4 + j
                        for pi, (a_, b_) in enumerate(((hfh, wrh), (hfh, wrl), (hfl, wrh))):
                            S.op("tensor", lambda e, kc=kc, j=j, hs=hs, a_=a_, b_=b_, pi=pi: e.matmul(
                                ps[5][:, 0:32], lhsT=a_[:, hs, j, :], rhs=b_[:, kc, :],
                                start=(kc == 0 and pi == 0), stop=(kc == KC - 1 and pi == 2)),
                                reads=[("hfh", hs), ("hfl", hs), "wr"], writes=[("ps", 5)])
            if router is not None and DBG_LVL >= 0:
                r = tt % 2
                lg, mx8, sel, nm, ex, ssum = (rg[:, r, 0:32], rg[:, r, 32:40], rg[:, r, 40:72], rg[:, r, 72:73],
                                              rg[:, r, 80:112], rg[:, r, 112:114])
                gts = rg[:, r, 120:152]
                K_ = ("rg", r)
                S.op("vector", lambda e, lg=lg: e.tensor_tensor(out=lg, in0=ps[5][:, 0:32], in1=brbc[:], op=ALU.add),
                     reads=[("ps", 5), "brbc"], writes=[K_])
                S.op("vector", lambda e, lg=lg, mx8=mx8: e.max(out=mx8, in_=lg), reads=[K_], writes=[K_])
                S.op("vector", lambda e, lg=lg, mx8=mx8, sel=sel: e.tensor_scalar(
                    out=sel, in0=lg, scalar1=mx8[:, 3:4], scalar2=None, op0=ALU.is_ge), reads=[K_], writes=[K_])
                S.op("vector", lambda e, mx8=mx8, nm=nm: e.tensor_scalar(
                    out=nm, in0=mx8[:, 0:1], scalar1=-1.0, scalar2=None, op0=ALU.mult), reads=[K_], writes=[K_])
                S.op("scalar", lambda e, lg=lg, nm=nm, ex=ex: e.activation(out=ex, in_=lg, func=AF.Exp, bias=nm),
                     reads=[K_], writes=[K_])
                S.op("vector", lambda e, ex=ex, sel=sel: e.tensor_tensor(out=ex, in0=ex, in1=sel, op=ALU.mult),
                     reads=[K_], writes=[K_])
                S.op("vector", lambda e, ex=ex, ssum=ssum: e.reduce_sum(out=ssum[:, 0:1], in_=ex, axis=AX.X),
                     reads=[K_], writes=[K_])
                S.op("vector", lambda e, ssum=ssum: e.reciprocal(out=ssum[:, 1:2], in_=ssum[:, 0:1]),
                     reads=[K_], writes=[K_])
                S.op("vector", lambda e, ex=ex, ssum=ssum, gts=gts: e.tensor_scalar(
                    out=gts, in0=ex, scalar1=ssum[:, 1:2], scalar2=None, op0=ALU.mult), reads=[K_], writes=[K_])
                S.op("tensor", lambda e, gts=gts: e.transpose(ps[6][0:32, 0:128], gts, ident),
                     reads=[K_, "cst"], writes=[("ps", 6)])
                S.op("vector", lambda e, tl=tl: e.tensor_copy(out=gT[:, tl * 128:(tl + 1) * 128], in_=ps[6][0:32, 0:128]),
                     reads=[("ps", 6)], writes=["gT"])
        S.barrier()
        A.reset(m)

    def final_norm(x_src):
        m = A.mark()
        xt = A.alloc("xt", [128, 2, D], F32)
        sq = A.alloc("sq", [128, D], F32)
        st = A.alloc("st", [128, 2, 4], F32)
        fg = A.alloc("fg", [128, D], F32)
        yo = A.alloc("yo", [128, 2, D], F32)
        S.op("sync", lambda e: e.dma_start(out=fg[:], in_=final_g.partition_broadcast(128)), writes=["fg"], dma=True)
        for tt in range(NT):
            s = tt % 2
            S.op("sync", lambda e, tt=tt, s=s: e.dma_start(out=xt[:, s, :], in_=x_src[tt * 128:(tt + 1) * 128, :]),
                 writes=[("xt", s)], dma=True)
            S.op("vector", lambda e, s=s: e.tensor_tensor(out=sq[:], in0=xt[:, s, :], in1=xt[:, s, :], op=ALU.mult),
                 reads=[("xt", s)], writes=["sq"])
            S.op("vector", lambda e, s=s: e.reduce_sum(out=st[:, s, 0:1], in_=sq[:], axis=AX.X),
                 reads=["sq"], writes=[("st", s)])
            S.op("vector", lambda e, s=s: e.tensor_scalar(out=st[:, s, 1:2], in0=st[:, s, 0:1], scalar1=1.0 / D,
                                                          scalar2=1e-6, op0=ALU.mult, op1=ALU.add),
                 reads=[("st", s)], writes=[("st", s)])
            S.op("scalar", lambda e, s=s: e.activation(out=st[:, s, 3:4], in_=st[:, s, 1:2], func=AF.Sqrt),
                 reads=[("st", s)], writes=[("st", s)])
            S.op("vector", lambda e, s=s: e.reciprocal(out=st[:, s, 2:3], in_=st[:, s, 3:4]),
                 reads=[("st", s)], writes=[("st", s)])
            S.op("vector", lambda e, s=s: e.scalar_tensor_tensor(
                out=yo[:, s, :], in0=xt[:, s, :], scalar=st[:, s, 2:3], in1=fg[:], op0=ALU.mult, op1=ALU.mult),
                reads=[("xt", s), ("st", s), "fg"], writes=[("yo", s)])
            S.op("sync", lambda e, tt=tt, s=s: e.dma_start(out=out[tt * 128:(tt + 1) * 128, :], in_=yo[:, s, :]),
                 reads=[("yo", s)], writes=["dram:out"], dma=True, semkey=("yo_st", s))
        A.reset(m)

    def linear_tok(hT, w_ap, ncols, evac, kcn=KC, tag="lt"):
        m = A.mark()
        wp = A.alloc("wp_" + tag, [128, 2, kcn, 512], BF16)
        cnt = 0
        for nb in range(ncols // 512):
            s = nb % 2
            S.op("gpsimd", lambda e, nb=nb, s=s: e.dma_start(
                out=wp[:, s], in_=w_ap[:, nb * 512:(nb + 1) * 512].rearrange("(k p) n -> p k n", p=128)),
                writes=[("wp", s)], dma=True)
            for tt in range(NT):
                pb = cnt % 4
                cnt += 1
                for kc in range(kcn):
                    S.op("tensor", lambda e, kc=kc, s=s, pb=pb, tt=tt: e.matmul(
                        ps[pb][:, :], lhsT=hT[:, kc, tt * 128:(tt + 1) * 128], rhs=wp[:, s, kc, :],
                        start=(kc == 0), stop=(kc == kcn - 1)),
                        reads=[("hT", tt), ("wp", s)], writes=[("ps", pb)])
                evac(tt, nb, ps[pb], pb)
        A.reset(m)

    def out_proj(at_d, w_ap, bias_ap, gate_row_ap, x_src, x_dst):
        m = A.mark()
        at = A.alloc("at", [128, 8, 32, 128], BF16)
        wp = A.alloc("wpo", [128, 2, 32, 512], BF16)
        gbc = A.alloc("gbc", [128, D], F32)
        bbc = A.alloc("bbc", [128, D], F32)
        xt = A.alloc("xto", [128, 2, 512], F32)
        yt = A.alloc("yto", [128, 2, 512], F32)
        S.op("sync", lambda e: e.dma_start(out=gbc[:], in_=gate_row_ap.partition_broadcast(128)), writes=["gbc"], dma=True)
        if bias_ap is not None:
            S.op("sync", lambda e: e.dma_start(out=bbc[:], in_=bias_ap.partition_broadcast(128)), writes=["bbc"], dma=True)
        cnt = 0
        for th in range(2):
            for t8 in range(8):
                S.op("sync", lambda e, th=th, t8=t8: e.dma_start(out=at[:, t8], in_=at_d[th * 8 + t8]),
                     writes=[("at", t8)], dma=True)
            for db in range(4):
                s = (th * 4 + db) % 2
                S.op("gpsimd", lambda e, db=db, s=s: e.dma_start(
                    out=wp[:, s], in_=w_ap[:, db * 512:(db + 1) * 512].rearrange("(k p) n -> p k n", p=128)),
                    writes=[("wpo", s)], dma=True)
                for t8 in range(8):
                    tt = th * 8 + t8
                    pb = cnt % 4
                    xs_ = cnt % 2
                    cnt += 1
                    S.op("sync", lambda e, tt=tt, db=db, xs_=xs_: e.dma_start(
                        out=xt[:, xs_, :], in_=x_src[tt * 128:(tt + 1) * 128, db * 512:(db + 1) * 512]),
                        writes=[("xto", xs_)], dma=True)
                    for kc in range(32):
                        S.op("tensor", lambda e, kc=kc, s=s, pb=pb, t8=t8: e.matmul(
                            ps[pb][:, :], lhsT=at[:, t8, kc, :], rhs=wp[:, s, kc, :],
                            start=(kc == 0), stop=(kc == 31)),
                            reads=[("at", t8), ("wpo", s)], writes=[("ps", pb)])
                    if bias_ap is not None:
                        S.op("vector", lambda e, pb=pb, xs_=xs_, db=db: e.tensor_tensor(
                            out=yt[:, xs_, :], in0=ps[pb][:, :], in1=bbc[:, db * 512:(db + 1) * 512], op=ALU.add),
                            reads=[("ps", pb), "bbc"], writes=[("yto", xs_)])
                        S.op("vector", lambda e, xs_=xs_, db=db: e.tensor_tensor(
                            out=yt[:, xs_, :], in0=yt[:, xs_, :], in1=gbc[:, db * 512:(db + 1) * 512], op=ALU.mult),
                            reads=[("yto", xs_), "gbc"], writes=[("yto", xs_)])
                    else:
                        S.op("vector", lambda e, pb=pb, xs_=xs_, db=db: e.tensor_tensor(
                            out=yt[:, xs_, :], in0=ps[pb][:, :], in1=gbc[:, db * 512:(db + 1) * 512], op=ALU.mult),
                            reads=[("ps", pb), "gbc"], writes=[("yto", xs_)])
                    S.op("vector", lambda e, xs_=xs_: e.tensor_tensor(
                        out=yt[:, xs_, :], in0=yt[:, xs_, :], in1=xt[:, xs_, :], op=ALU.add),
                        reads=[("yto", xs_), ("xto", xs_)], writes=[("yto", xs_)])
                    S.op("sync", lambda e, tt=tt, db=db, xs_=xs_: e.dma_start(
                        out=x_dst[tt * 128:(tt + 1) * 128, db * 512:(db + 1) * 512], in_=yt[:, xs_, :]),
                        reads=[("yto", xs_)], writes=["dram:xdst"], dma=True, semkey=("yto_st", xs_))
        S.barrier()
        A.reset(m)

    psb = [p[:].bitcast(BF16) for p in ps]

    def gmlp(sub, l, x_src, x_dst, dbg=None):
        w_in = g_w_in[0]
        m = A.mark()
        hT = A.alloc("hT", [128, KC, L], BF16)
        prologue(sub, x_src, hT)
        bb = A.alloc("bb", [128, 2, 512], F32)
        uvt = A.alloc("uvt", [128, 4, 512], F32)
        state = {"n": 0}

        def evac(tt, nb, pt, pb):
            k = state["n"] % 4
            state["n"] += 1
            bs_ = nb % 2
            if tt == 0:
                S.op("sync", lambda e, nb=nb, bs_=bs_: e.dma_start(
                    out=bb[:, bs_, :], in_=g_b_in[0, nb * 512:(nb + 1) * 512].partition_broadcast(128)),
                    writes=[("bb", bs_)], dma=True)
            S.op("vector", lambda e, k=k, bs_=bs_, pt=pt: e.tensor_tensor(
                out=uvt[:, k, :], in0=pt[:, :], in1=bb[:, bs_, :], op=ALU.add),
                reads=[("ps", pb), ("bb", bs_)], writes=[("uvt", k)])
            S.op("scalar", lambda e, k=k: e.activation(out=uvt[:, k, :], in_=uvt[:, k, :], func=AF.Gelu),
                 reads=[("uvt", k)], writes=[("uvt", k)])
            S.op("sync", lambda e, k=k, tt=tt, nb=nb: e.dma_start(
                out=uv_d[tt * 128:(tt + 1) * 128, nb * 512:(nb + 1) * 512], in_=uvt[:, k, :]),
                reads=[("uvt", k)], writes=["dram:uv"], dma=True, semkey=("uvt_st", k))
        linear_tok(hT, w_in, 8192, evac, tag="g1")
        S.barrier()
        A.reset(m)
        if dbg in ("uv", "uv2"):
            c0 = 0 if dbg == "uv" else 4096
            S.op("sync", lambda e: e.dma_start(out=out, in_=uv_d[:, c0:c0 + 2048]), writes=["dram:out"], dma=True, semkey="cp_out")
            return
        m = A.mark()
        lng = A.alloc("lng", [128, 4096], F32)
        lnb = A.alloc("lnb", [128, 4096], F32)
        wsT = A.alloc("wsT", [128, 16, 128], BF16)
        wsf = A.alloc("wsf", [128, 128], F32)
        bsc = A.alloc("bsc", [128, 16], F32)
        ut = A.alloc("ut", [128, 2, 4096], F32)
        vt = A.alloc("vt", [128, 2, 4096], F32)
        sq2 = A.alloc("sq2", [128, 4096], F32)
        vn = A.alloc("vn", [128, 4096], BF16)
        pp = A.alloc("pp", [128, 4096], BF16)
        ppT = A.alloc("ppT", [128, 2, 32, 128], BF16)
        st = A.alloc("st2", [128, 2, 8], F32)
        S.op("sync", lambda e: e.dma_start(out=lng[:], in_=g_ln_g[0].partition_broadcast(128)), writes=["lng"], dma=True)
        S.op("sync", lambda e: e.dma_start(out=lnb[:], in_=g_ln_b[0].partition_broadcast(128)), writes=["lnb"], dma=True)
        S.op("sync", lambda e: e.dma_start(out=bsc[:], in_=g_bs_col), writes=["bsc"], dma=True)
        for g in range(16):
            S.op("sync", lambda e, g=g: e.dma_start(out=wsf[:], in_=g_w_s[0, g]), writes=["wsf"], dma=True)
            S.op("tensor", lambda e: e.transpose(ps[4][:, 0:128], wsf[:], ident), reads=["wsf", "cst"], writes=[("ps", 4)])
            S.op("vector", lambda e, g=g: e.tensor_copy(out=wsT[:, g, :], in_=ps[4][:, 0:128]),
                 reads=[("ps", 4)], writes=["wsT"])
        for tt in range(NT):
            s = tt % 2
            S.op("sync", lambda e, tt=tt, s=s: e.dma_start(out=ut[:, s, :], in_=uv_d[tt * 128:(tt + 1) * 128, 0:4096]),
                 reads=["dram:uv"], writes=[("ut", s)], dma=True)
            S.op("sync", lambda e, tt=tt, s=s: e.dma_start(out=vt[:, s, :], in_=uv_d[tt * 128:(tt + 1) * 128, 4096:8192]),
                 reads=["dram:uv"], writes=[("vt", s)], dma=True)
            S.op("vector", lambda e, s=s: e.reduce_sum(out=st[:, s, 0:1], in_=vt[:, s, :], axis=AX.X),
                 reads=[("vt", s)], writes=[("st2", s)])
            S.op("vector", lambda e, s=s: e.tensor_tensor(out=sq2[:], in0=vt[:, s, :], in1=vt[:, s, :], op=ALU.mult),
                 reads=[("vt", s)], writes=["sq2"])
            S.op("vector", lambda e, s=s: e.reduce_sum(out=st[:, s, 1:2], in_=sq2[:], axis=AX.X),
                 reads=["sq2"], writes=[("st2", s)])
            S.op("vector", lambda e, s=s: e.tensor_scalar(out=st[:, s, 2:4], in0=st[:, s, 0:2], scalar1=1.0 / 4096,
                                                          scalar2=None, op0=ALU.mult),
                 reads=[("st2", s)], writes=[("st2", s)])
            S.op("vector", lambda e, s=s: e.tensor_tensor(out=st[:, s, 4:5], in0=st[:, s, 2:3], in1=st[:, s, 2:3], op=ALU.mult),
                 reads=[("st2", s)], writes=[("st2", s)])
            S.op("vector", lambda e, s=s: e.tensor_tensor(out=st[:, s, 5:6], in0=st[:, s, 3:4], in1=st[:, s, 4:5], op=ALU.subtract),
                 reads=[("st2", s)], writes=[("st2", s)])
            S.op("scalar", lambda e, s=s: e.activation(out=st[:, s, 6:7], in_=st[:, s, 5:6], func=AF.Sqrt, bias=1e-6),
                 reads=[("st2", s)], writes=[("st2", s)])
            S.op("vector", lambda e, s=s: e.reciprocal(out=st[:, s, 7:8], in_=st[:, s, 6:7]),
                 reads=[("st2", s)], writes=[("st2", s)])
            S.op("vector", lambda e, s=s: e.tensor_scalar(out=sq2[:], in0=vt[:, s, :], scalar1=st[:, s, 2:3],
                                                          scalar2=st[:, s, 7:8], op0=ALU.subtract, op1=ALU.mult),
                 reads=[("vt", s), ("st2", s)], writes=["sq2"])
            S.op("vector", lambda e: e.tensor_tensor(out=sq2[:], in0=sq2[:], in1=lng[:], op=ALU.mult),
                 reads=["sq2", "lng"], writes=["sq2"])
            S.op("vector", lambda e: e.tensor_tensor(out=vn[:], in0=sq2[:], in1=lnb[:], op=ALU.add),
                 reads=["sq2", "lnb"], writes=["vn"])
            for g2 in range(8):
                pb = g2 % 4
                for gg in range(2):
                    g = g2 * 2 + gg
                    S.op("tensor", lambda e, g=g, gg=gg, pb=pb: e.matmul(
                        ps[pb][:, gg * 256:(gg + 1) * 256], lhsT=wsT[:, g, :], rhs=vn[:, g * 256:(g + 1) * 256],
                        start=True, stop=True),
                        reads=["wsT", "vn"], writes=[("ps", pb)])
                for gg in range(2):
                    g = g2 * 2 + gg
                    S.op("vector", lambda e, g=g, gg=gg, pb=pb, s=s: e.scalar_tensor_tensor(
                        out=pp[:, g * 256:(g + 1) * 256], in0=ps[pb][:, gg * 256:(gg + 1) * 256], scalar=bsc[:, g:g + 1],
                        in1=ut[:, s, g * 256:(g + 1) * 256], op0=ALU.add, op1=ALU.mult),
                        reads=[("ps", pb), "bsc", ("ut", s)], writes=["pp"])
            for q in range(4):
                pb = 4 + q % 4
                for j in range(8):
                    kc = q * 8 + j
                    S.op("tensor", lambda e, kc=kc, pb=pb, j=j: e.transpose(
                        psb[pb][:, j * 128:(j + 1) * 128], pp[:, kc * 128:(kc + 1) * 128], identb[:]),
                        reads=["pp", "identb"], writes=[("ps", pb)])
                S.op("scalar", lambda e, q=q, pb=pb, s=s: e.copy(
                    out=ppT[:, s, q * 8:(q + 1) * 8, :].rearrange("p a b -> p (a b)"), in_=psb[pb][:, :]),
                    reads=[("ps", pb)], writes=[("ppT", s)])
            S.op("sync", lambda e, tt=tt, s=s: e.dma_start(out=at_d[tt], in_=ppT[:, s]),
                 reads=[("ppT", s)], writes=["dram:at"], dma=True, semkey=("ppT_st", s))
        S.barrier()
        A.reset(m)
        if dbg == "at":
            for tt in range(NT):
                S.op("gpsimd", lambda e, tt=tt: e.dma_start(
                    out=out[tt * 128:(tt + 1) * 128, :].rearrange("p (k t) -> p k t", k=16), in_=at_d[tt, :, 0:16, :]),
                    writes=["dram:out"], dma=True, semkey="cp_out")
            return
        out_proj(at_d, g_w_out[0], g_b_out[0], mod_d[l, 2 * D:3 * D], x_src, x_dst)

    def ssm(sub, l, x_src, x_dst, dbg=None):
        w_in = s_w_in[0]
        m_all = A.mark()
        dt_tok = A.alloc("dt_tok", [128, NT, 128], F32)
        dta_tok = A.alloc("dta_tok", [128, NT, 128], F32)
        dta_hi = A.alloc("dta_hi", [128, NT, 128], F32)
        m = A.mark()
        hT = A.alloc("hT", [128, KC, L], BF16)
        prologue(sub, x_src, hT)
        zt = A.alloc("zt", [128, 4, 512], F32)
        state = {"n": 0}

        def evac_z(tt, nb, pt, pb):
            k = state["n"] % 4
            state["n"] += 1
            S.op("scalar", lambda e, k=k, pt=pt: e.activation(out=zt[:, k, :], in_=pt[:, :], func=AF.Silu),
                 reads=[("ps", pb)], writes=[("zt", k)])
            S.op("sync", lambda e, k=k, tt=tt, nb=nb: e.dma_start(
                out=zs_d[tt * 128:(tt + 1) * 128, nb * 512:(nb + 1) * 512], in_=zt[:, k, :]),
                reads=[("zt", k)], writes=["dram:zs"], dma=True, semkey=("zt_st", k))
        linear_tok(hT, w_in, 4096, evac_z, tag="s1")
        S.barrier()
        if dbg == "s1":
            S.op("sync", lambda e: e.dma_start(out=out, in_=zs_d[:, 0:2048]), writes=["dram:out"], dma=True, semkey="cp_out")
            return
        wp = A.alloc("wp2", [128, 2, KC, 512], BF16)
        cw = A.alloc("cw", [128, 48, 5], F32)
        cb = A.alloc("cb", [128, 48], F32)
        dtb = A.alloc("dtb", [128, 1], F32)
        acol = A.alloc("acol", [128, 1], F32)
        xc = A.alloc("xc", [128, 2, L + 4], F32)
        acc = A.alloc("acc", [128, L], F32)
        xo = A.alloc("xo", [128, L], F32)
        bco = A.alloc("bco", [128, 2, L], BF16)
        xtok = A.alloc("xtok", [128, 2, NT, 128], F32)
        btk = A.alloc("btk", [128, 2, NT, 128], BF16)
        S.op("sync", lambda e: e.dma_start(out=cw[:], in_=s_conv_col), writes=["cw"], dma=True)
        S.op("sync", lambda e: e.dma_start(out=cb[:], in_=s_convb_col), writes=["cb"], dma=True)
        S.op("sync", lambda e: e.dma_start(out=dtb[:], in_=s_dtb_col), writes=["dtb"], dma=True)
        S.op("sync", lambda e: e.dma_start(out=acol[:], in_=s_alog_col), writes=["acol"], dma=True)
        S.op("scalar", lambda e: e.activation(out=acol[:], in_=acol[:], func=AF.Exp), reads=["acol"], writes=["acol"])
        S.op("vector", lambda e: e.tensor_scalar(out=acol[:], in0=acol[:], scalar1=-1.0, scalar2=None, op0=ALU.mult),
             reads=["acol"], writes=["acol"])
        for s_ in range(2):
            S.op("vector", lambda e, s_=s_: e.memset(xc[:, s_, :], 0.0), writes=[("xc", s_)])
        for grp in range(13):
            ws_ = grp % 2
            ncol = 512 if grp < 12 else 128
            c0 = 4096 + grp * 512
            S.op("gpsimd", lambda e, ws_=ws_, c0=c0, ncol=ncol: e.dma_start(
                out=wp[:, ws_, :, 0:ncol], in_=w_in[:, c0:c0 + ncol].rearrange("(k p) n -> p k n", p=128)),
                writes=[("wp2", ws_)], dma=True)
            for j in range(ncol // 128):
                ch = grp * 4 + j
                for tb in range(4):
                    for kc in range(KC):
                        S.op("tensor", lambda e, kc=kc, ws_=ws_, j=j, tb=tb: e.matmul(
                            ps[tb][:, :], lhsT=wp[:, ws_, kc, j * 128:(j + 1) * 128], rhs=hT[:, kc, tb * 512:(tb + 1) * 512],
                            start=(kc == 0), stop=(kc == KC - 1)),
                            reads=[("wp2", ws_)] + [("hT", t) for t in range(tb * 4, tb * 4 + 4)], writes=[("ps", tb)])
                if ch < 48:
                    xs_ = ch % 2
                    for tb in range(4):
                        S.op("scalar", lambda e, tb=tb, xs_=xs_: e.copy(out=xc[:, xs_, 2 + tb * 512:2 + (tb + 1) * 512], in_=ps[tb][:, :]),
                             reads=[("ps", tb)], writes=[("xc", xs_)])
                    S.op("vector", lambda e, ch=ch, xs_=xs_: e.tensor_scalar(
                        out=acc[:], in0=xc[:, xs_, 0:L], scalar1=cw[:, ch, 0:1], scalar2=None, op0=ALU.mult),
                        reads=[("xc", xs_), "cw"], writes=["acc"])
                    for k in range(1, 5):
                        S.op("vector", lambda e, ch=ch, xs_=xs_, k=k: e.scalar_tensor_tensor(
                            out=acc[:], in0=xc[:, xs_, k:k + L], scalar=cw[:, ch, k:k + 1], in1=acc[:], op0=ALU.mult, op1=ALU.add),
                            reads=[("xc", xs_), "cw", "acc"], writes=["acc"])
                    if ch < 32:
                        S.op("scalar", lambda e, ch=ch: e.activation(out=xo[:], in_=acc[:], func=AF.Silu, bias=cb[:, ch:ch + 1]),
                             reads=["acc", "cb"], writes=["xo"])
                        ts_ = ch % 2
                        for q in range(4):
                            pb = 4 + q
                            for jj in range(4):
                                tt = q * 4 + jj
                                S.op("tensor", lambda e, tt=tt, pb=pb, jj=jj: e.transpose(
                                    ps[pb][:, jj * 128:(jj + 1) * 128], xo[:, tt * 128:(tt + 1) * 128], ident),
                                    reads=["xo", "cst"], writes=[("ps", pb)])
                            S.op("vector", lambda e, q=q, pb=pb, ts_=ts_: e.tensor_copy(
                                out=xtok[:, ts_, q * 4:(q + 1) * 4, :].rearrange("p a b -> p (a b)"), in_=ps[pb][:, :]),
                                reads=[("ps", pb)], writes=[("xtok", ts_)])
                        S.op("sync", lambda e, ch=ch, ts_=ts_: e.dma_start(
                            out=xsd[:, ch * 128:(ch + 1) * 128].rearrange("(t p) f -> p t f", p=128), in_=xtok[:, ts_]),
                            reads=[("xtok", ts_)], writes=["dram:xsd"], dma=True, semkey=("xtok_st", ts_))
                    else:
                        bs_ = ch % 2
                        S.op("scalar", lambda e, ch=ch, bs_=bs_: e.activation(out=bco[:, bs_, :], in_=acc[:], func=AF.Silu, bias=cb[:, ch:ch + 1]),
                             reads=["acc", "cb"], writes=[("bco", bs_)])
                        S.op("sync", lambda e, ch=ch, bs_=bs_: e.dma_start(out=bct_d[ch - 32], in_=bco[:, bs_, :]),
                             reads=[("bco", bs_)], writes=["dram:bct"], dma=True, semkey=("bco_st", bs_))
                        if ch < 40:
                            for q in range(2):
                                pb = 4 + q
                                for jj in range(8):
                                    tt = q * 8 + jj
                                    S.op("tensor", lambda e, tt=tt, pb=pb, jj=jj, bs_=bs_: e.transpose(
                                        psb[pb][:, jj * 128:(jj + 1) * 128], bco[:, bs_, tt * 128:(tt + 1) * 128], identb[:]),
                                        reads=[("bco", bs_), "identb"], writes=[("ps", pb)])
                                S.op("vector", lambda e, q=q, pb=pb, bs_=bs_: e.tensor_copy(
                                    out=btk[:, bs_, q * 8:(q + 1) * 8, :].rearrange("p a b -> p (a b)"), in_=psb[pb][:, :]),
                                    reads=[("ps", pb)], writes=[("btk", bs_)])
                            S.op("sync", lambda e, ch=ch, bs_=bs_: e.dma_start(
                                out=btok_d[:, (ch - 32) * 128:(ch - 31) * 128].rearrange("(t p) f -> p t f", p=128), in_=btk[:, bs_]),
                                reads=[("btk", bs_)], writes=["dram:btok"], dma=True, semkey=("btk_st", bs_))
                else:
                    for tb in range(4):
                        S.op("scalar", lambda e, tb=tb: e.activation(out=acc[:, tb * 512:(tb + 1) * 512], in_=ps[tb][:, :], func=AF.Exp, bias=dtb[:, 0:1]),
                             reads=[("ps", tb), "dtb"], writes=["acc"])
                    S.op("scalar", lambda e: e.activation(out=xo[:], in_=acc[:], func=AF.Ln, bias=1.0), reads=["acc"], writes=["xo"])
                    S.op("vector", lambda e: e.tensor_scalar(out=acc[:], in0=xo[:], scalar1=acol[:, 0:1], scalar2=None, op0=ALU.mult),
                         reads=["xo", "acol"], writes=["acc"])
                    for src, dstt, nm in ((xo, dt_tok, "dt_tok"), (acc, dta_tok, "dta_tok")):
                        for q in range(4):
                            pb = 4 + q
                            for jj in range(4):
                                tt = q * 4 + jj
                                S.op("tensor", lambda e, tt=tt, pb=pb, jj=jj, src=src: e.transpose(
                                    ps[pb][:, jj * 128:(jj + 1) * 128], src[:, tt * 128:(tt + 1) * 128], ident),
                                    reads=["xo", "acc", "cst"], writes=[("ps", pb)])
                            S.op("vector", lambda e, q=q, pb=pb, dstt=dstt: e.tensor_copy(
                                out=dstt[:, q * 4:(q + 1) * 4, :].rearrange("p a b -> p (a b)"), in_=ps[pb][:, :]),
                                reads=[("ps", pb)], writes=[nm])
        hbt = A.alloc("hbt", [128, NT, 128], BF16)
        S.op("vector", lambda e: e.tensor_copy(out=hbt[:], in_=dta_tok[:]), reads=["dta_tok"], writes=["hbt"])
        S.op("vector", lambda e: e.tensor_copy(out=dta_hi[:], in_=hbt[:]), reads=["hbt"], writes=["dta_hi"])
        S.op("vector", lambda e: e.tensor_tensor(out=dta_tok[:], in0=dta_tok[:], in1=dta_hi[:], op=ALU.subtract),
             reads=["dta_tok", "dta_hi"], writes=["dta_tok"])
        S.barrier()
        A.reset(m)
        if dbg in ("s2", "s2b"):
            if dbg == "s2":
                S.op("sync", lambda e: e.dma_start(out=out, in_=xsd[:, 0:2048]), writes=["dram:out"], dma=True, semkey="cp_out")
            else:
                S.op("gpsimd", lambda e: e.dma_start(out=out.rearrange("(c p) t -> c p t", p=128), in_=bct_d), writes=["dram:out"], dma=True, semkey="cp_out")
            return
        Et = A.alloc("Et", [128, NT, 128], F32)
        DTEt = A.alloc("DTEt", [128, NT, 128], F32)
        CDt = A.alloc("CDt", [128, NT, 128], F32)
        tmpc = A.alloc("tmpc", [128, 128], F32)
        hlb = A.alloc("hlb", [128, 2, 128], BF16)
        import os
        for tt in range(NT if not os.environ.get("SSM_SKIP_PRE") else 0):
            S.op("vector", lambda e, tt=tt: e.tensor_copy(out=hlb[:, 0, :], in_=dta_hi[:, tt, :]), reads=["dta_hi"], writes=["hlb"])
            S.op("vector", lambda e, tt=tt: e.tensor_copy(out=hlb[:, 1, :], in_=dta_tok[:, tt, :]), reads=["dta_tok"], writes=["hlb"])
            for pi in range(2):
                S.op("tensor", lambda e, pi=pi: e.matmul(ps[0][:, 0:64], lhsT=tri_le_b, rhs=hlb[:, pi, 0:64], start=(pi == 0), stop=(pi == 1)),
                     reads=["hlb", "cstb"], writes=[("ps", 0)])
            for pi in range(2):
                S.op("tensor", lambda e, pi=pi: e.matmul(ps[0][:, 64:128], lhsT=tri_ge_b, rhs=hlb[:, pi, 64:128], start=(pi == 0), stop=(pi == 1)),
                     reads=["hlb", "cstb"], writes=[("ps", 0)])
            for pi in range(2):
                S.op("tensor", lambda e, pi=pi: e.matmul(ps[1][:, 0:128], lhsT=ones_b, rhs=hlb[:, pi, :], start=(pi == 0), stop=(pi == 1)),
                     reads=["hlb", "cstb"], writes=[("ps", 1)])
            PL = int(os.environ.get("PRE_LEVEL", "9"))
            if PL < 2:
                continue
            S.op("scalar", lambda e, tt=tt: e.activation(out=Et[:, tt, :], in_=ps[0][:, 0:128], func=AF.Exp), reads=[("ps", 0)], writes=["Et"])
            S.op("scalar", lambda e, tt=tt: e.activation(out=CDt[:, tt, :], in_=ps[1][:, 0:128], func=AF.Exp), reads=[("ps", 1)], writes=["CDt"])
            for pi in range(2):
                S.op("tensor", lambda e, pi=pi: e.matmul(ps[2][:, 0:64], lhsT=cstb[:, 3, :], rhs=hlb[:, pi, 0:64], start=(pi == 0), stop=(pi == 1)),
                     reads=["hlb", "cstb"], writes=[("ps", 2)])
            for pi in range(2):
                S.op("tensor", lambda e, pi=pi: e.matmul(ps[2][:, 64:128], lhsT=cstb[:, 4, :], rhs=hlb[:, pi, 64:128], start=(pi == 0), stop=(pi == 1)),
                     reads=["hlb", "cstb"], writes=[("ps", 2)])
            S.op("scalar", lambda e, tt=tt: e.activation(out=DTEt[:, tt, :], in_=ps[2][:, 0:128], func=AF.Exp), reads=[("ps", 2)], writes=["DTEt"])
        import os
        STOP = int(os.environ.get("SSM_STOP", "0"))
        if STOP == 1:
            S.barrier(); return
        BT = A.alloc("BT", [128, L], BF16); CT = A.alloc("CT", [128, L], BF16)
        Btok = A.alloc("Btok", [128, NT, 128], BF16)
        xsg = A.alloc("xsg", [128, NT, 512], F32)
        dtx1 = A.alloc("dtx1", [128, NT, 512], BF16)
        dtx = [dtx1, dtx1]
        yacc = A.alloc("yacc_s", [128, NT, 512], F32)
        SM = [A.alloc("SMF", [128, NT, 128], F32), A.alloc("SMB", [128, NT, 128], F32)]
        prevT = A.alloc("prevT", [128, 512], F32); prevB = A.alloc("prevB", [128, 512], BF16)
        LW = A.alloc("LW", [128, 2, 2, 4, 128], BF16)
        EX = A.alloc("EX", [128, 2, 512], F32)
        MT = A.alloc("MT", [128, 2, 4, 128], BF16)
        t1 = A.alloc("t1", [128, 512], F32)
        dtxe = A.alloc("dtxe", [128, 512], BF16)
        zst = A.alloc("zst", [128, 2, 512], F32)
        ut = A.alloc("ut_s", [128, 512], F32); usq = t1
        vb = A.alloc("vb", [128, 512], BF16)
        vT = A.alloc("vT", [128, 2, 4, 128], BF16)
        Dbc = A.alloc("Dbc", [128, 512], F32); ngb = A.alloc("ngb", [128, 512], F32)
        stt = A.alloc("stt", [128, 2, 4], F32)
        nlw = 0
        for g in range(8):
            S.op("sync", lambda e, g=g: e.dma_start(out=BT[:], in_=bct_d[g]), reads=["dram:bct"], writes=["BT"], dma=True)
            S.op("sync", lambda e, g=g: e.dma_start(out=CT[:], in_=bct_d[8 + g]), reads=["dram:bct"], writes=["CT"], dma=True)
            S.op("sync", lambda e, g=g: e.dma_start(out=Btok[:], in_=btok_d[:, g * 128:(g + 1) * 128].rearrange("(t p) n -> p t n", p=128)),
                 reads=["dram:btok"], writes=["Btok"], dma=True)
            S.op("sync", lambda e, g=g: e.dma_start(out=xsg[:], in_=xsd[:, g * 512:(g + 1) * 512].rearrange("(t p) n -> p t n", p=128)),
                 reads=["dram:xsd"], writes=["xsg"], dma=True)
            S.op("sync", lambda e, g=g: e.dma_start(out=Dbc[:], in_=s_d_rep[g * 512:(g + 1) * 512].partition_broadcast(128)), writes=["Dbc"], dma=True)
            S.op("sync", lambda e, g=g: e.dma_start(out=ngb[:], in_=s_norm_g[g * 512:(g + 1) * 512].partition_broadcast(128)), writes=["ngb"], dma=True)
            for c in range(NT):
                S.op("vector", lambda e, c=c: e.tensor_tensor(out=yacc[:, c, :], in0=xsg[:, c, :], in1=Dbc[:], op=ALU.mult),
                     reads=["xsg", "Dbc"], writes=[("yacc_s", c)])
                S.op("tensor", lambda e, c=c: e.matmul(ps[7][:, 0:128], lhsT=BT[:, c * 128:(c + 1) * 128], rhs=CT[:, c * 128:(c + 1) * 128],
                                                        start=True, stop=True), reads=["BT", "CT"], writes=[("ps", 7)])
                S.op("vector", lambda e, c=c: e.tensor_tensor(out=SM[0][:, c, :], in0=ps[7][:, 0:128], in1=tri_le, op=ALU.mult),
                     reads=[("ps", 7), "cst"], writes=[("SM", 0, c)])
                S.op("vector", lambda e, c=c: e.tensor_tensor(out=SM[1][:, c, :], in0=ps[7][:, 0:128], in1=tri_ge, op=ALU.mult),
                     reads=[("ps", 7), "cst"], writes=[("SM", 1, c)])
            if STOP == 2:
                S.barrier(); return
            for d_ in range(2):
                if STOP == 3 and d_ == 1:
                    S.barrier(); return
                order = list(range(NT)) if d_ == 0 else list(range(NT - 1, -1, -1))
                umask = u_gt if d_ == 0 else u_lt
                trimb = tri_le_b if d_ == 0 else tri_ge_b
                hb = d_ * 64 + g * 8
                for c in range(NT):
                    S.op("vector", lambda e, c=c, d_=d_, hb=hb: e.tensor_tensor(
                        out=dtx[d_][:, c, :].rearrange("p (h d) -> p h d", h=8), in0=xsg[:, c, :].rearrange("p (h d) -> p h d", h=8),
                        in1=dt_tok[:, c, hb:hb + 8].unsqueeze(2).to_broadcast([128, 8, 64]), op=ALU.mult),
                        reads=["xsg", "dt_tok"], writes=[("dtx", c)])
                for ci, c in enumerate(order):
                    for hh in range(2):
                        ls = nlw % 2
                        nlw += 1
                        for h in range(4):
                            col = hb + hh * 4 + h
                            for pi, part in enumerate((dta_hi, dta_tok)):
                                S.op("scalar", lambda e, c=c, col=col, ls=ls, h=h, umask=umask, pi=pi, part=part: e.activation(
                                    out=LW[:, ls, pi, h, :], in_=umask, func=AF.Copy, scale=part[:, c, col:col + 1]),
                                    reads=["cst", "dta_tok", "dta_hi"], writes=[("LW", ls)])
                        pseg = 0 + ls
                        for h in range(4):
                            for pi in range(2):
                                S.op("tensor", lambda e, ls=ls, h=h, pseg=pseg, trimb=trimb, pi=pi: e.matmul(
                                    ps[pseg][:, h * 128:(h + 1) * 128], lhsT=LW[:, ls, pi, h, :], rhs=trimb,
                                    start=(pi == 0), stop=(pi == 1)),
                                    reads=[("LW", ls), "cstb"], writes=[("ps", pseg)])
                        S.op("scalar", lambda e, ls=ls, pseg=pseg: e.activation(out=EX[:, ls, :], in_=ps[pseg][:, :], func=AF.Exp),
                             reads=[("ps", pseg)], writes=[("EX", ls)])
                        S.op("vector", lambda e, ls=ls, c=c, d_=d_: e.tensor_tensor(
                            out=MT[:, ls], in0=EX[:, ls, :].rearrange("p (h i) -> p h i", h=4),
                            in1=SM[d_][:, c, :].unsqueeze(1).to_broadcast([128, 4, 128]), op=ALU.mult),
                            reads=[("EX", ls), ("SM", d_, c)], writes=[("MT", ls)])
                        for h in range(4):
                            hd = hh * 4 + h
                            S.op("tensor", lambda e, ls=ls, h=h, hd=hd, c=c, d_=d_: e.matmul(
                                ps[2][:, hd * 64:(hd + 1) * 64], lhsT=MT[:, ls, h, :], rhs=dtx[d_][:, c, hd * 64:(hd + 1) * 64],
                                start=True, stop=True),
                                reads=[("MT", ls), ("dtx", c)], writes=[("ps", 2)])
                    if ci > 0:
                        S.op("tensor", lambda e, c=c: e.matmul(ps[3][:, :], lhsT=CT[:, c * 128:(c + 1) * 128], rhs=prevB[:], start=True, stop=True),
                             reads=["CT", "prevB"], writes=[("ps", 3)])
                        S.op("vector", lambda e, c=c, hb=hb: e.tensor_tensor(
                            out=t1[:].rearrange("p (h d) -> p h d", h=8), in0=ps[3][:, :].rearrange("p (h d) -> p h d", h=8),
                            in1=Et[:, c, hb:hb + 8].unsqueeze(2).to_broadcast([128, 8, 64]), op=ALU.mult),
                            reads=[("ps", 3), "Et"], writes=["t1"])
                        S.op("vector", lambda e, c=c: e.tensor_tensor(out=t1[:], in0=t1[:], in1=yacc[:, c, :], op=ALU.add),
                             reads=["t1", ("yacc_s", c)], writes=["t1"])
                        S.op("vector", lambda e, c=c: e.tensor_tensor(out=yacc[:, c, :], in0=ps[2][:, :], in1=t1[:], op=ALU.add),
                             reads=[("ps", 2), "t1"], writes=[("yacc_s", c)])
                    else:
                        S.op("vector", lambda e, c=c: e.tensor_tensor(out=yacc[:, c, :], in0=ps[2][:, :], in1=yacc[:, c, :], op=ALU.add),
                             reads=[("ps", 2), ("yacc_s", c)], writes=[("yacc_s", c)])
                    if ci < NT - 1:
                        S.op("vector", lambda e, c=c, d_=d_, hb=hb: e.tensor_tensor(
                            out=dtxe[:].rearrange("p (h d) -> p h d", h=8), in0=dtx[d_][:, c, :].rearrange("p (h d) -> p h d", h=8),
                            in1=DTEt[:, c, hb:hb + 8].unsqueeze(2).to_broadcast([128, 8, 64]), op=ALU.mult),
                            reads=[("dtx", c), "DTEt"], writes=["dtxe"])
                        S.op("tensor", lambda e, c=c: e.matmul(ps[6][:, :], lhsT=Btok[:, c, :], rhs=dtxe[:], start=True, stop=True),
                             reads=["Btok", "dtxe"], writes=[("ps", 6)])
                        if ci == 0:
                            S.op("vector", lambda e: e.tensor_copy(out=prevT[:], in_=ps[6][:, :]), reads=[("ps", 6)], writes=["prevT"])
                        else:
                            S.op("vector", lambda e, c=c, hb=hb: e.tensor_tensor(
                                out=prevT[:].rearrange("p (h d) -> p h d", h=8), in0=prevT[:].rearrange("p (h d) -> p h d", h=8),
                                in1=CDt[:, c, hb:hb + 8].unsqueeze(2).to_broadcast([128, 8, 64]), op=ALU.mult),
                                reads=["prevT", "CDt"], writes=["prevT"])
                            S.op("vector", lambda e: e.tensor_tensor(out=prevT[:], in0=prevT[:], in1=ps[6][:, :], op=ALU.add),
                                 reads=["prevT", ("ps", 6)], writes=["prevT"])
                        S.op("scalar", lambda e: e.copy(out=prevB[:], in_=prevT[:]), reads=["prevT"], writes=["prevB"])
            if STOP == 4:
                S.barrier(); return
            for c in range(NT):
                zs_ = c % 2
                S.op("sync", lambda e, c=c, g=g, zs_=zs_: e.dma_start(out=zst[:, zs_, :], in_=zs_d[c * 128:(c + 1) * 128, g * 512:(g + 1) * 512]),
                     reads=["dram:zs"], writes=[("zst", zs_)], dma=True)
                S.op("vector", lambda e, c=c, zs_=zs_: e.tensor_tensor(out=ut[:], in0=yacc[:, c, :], in1=zst[:, zs_, :], op=ALU.mult),
                     reads=[("yacc_s", c), ("zst", zs_)], writes=["ut_s"])
                S.op("vector", lambda e: e.tensor_tensor(out=usq[:], in0=ut[:], in1=ut[:], op=ALU.mult), reads=["ut_s"], writes=["t1"])
                S.op("vector", lambda e, zs_=zs_: e.reduce_sum(out=stt[:, zs_, 0:1], in_=usq[:], axis=AX.X), reads=["t1"], writes=[("stt", zs_)])
                S.op("vector", lambda e, zs_=zs_: e.tensor_scalar(out=stt[:, zs_, 1:2], in0=stt[:, zs_, 0:1], scalar1=1.0 / 512, scalar2=1e-5,
                                                                  op0=ALU.mult, op1=ALU.add), reads=[("stt", zs_)], writes=[("stt", zs_)])
                S.op("scalar", lambda e, zs_=zs_: e.activation(out=stt[:, zs_, 2:3], in_=stt[:, zs_, 1:2], func=AF.Sqrt),
                     reads=[("stt", zs_)], writes=[("stt", zs_)])
                S.op("vector", lambda e, zs_=zs_: e.reciprocal(out=stt[:, zs_, 3:4], in_=stt[:, zs_, 2:3]), reads=[("stt", zs_)], writes=[("stt", zs_)])
                S.op("vector", lambda e, zs_=zs_: e.scalar_tensor_tensor(out=vb[:], in0=ut[:], scalar=stt[:, zs_, 3:4], in1=ngb[:],
                                                                          op0=ALU.mult, op1=ALU.mult),
                     reads=["ut_s", ("stt", zs_), "ngb"], writes=["vb"])
                for j in range(4):
                    S.op("tensor", lambda e, j=j: e.transpose(psb[5][:, j * 128:(j + 1) * 128], vb[:, j * 128:(j + 1) * 128], identb[:]),
                         reads=["vb", "identb"], writes=[("ps", 5)])
                S.op("scalar", lambda e, zs_=zs_: e.copy(out=vT[:, zs_].rearrange("p a b -> p (a b)"), in_=psb[5][:, 0:512]),
                     reads=[("ps", 5)], writes=[("vT", zs_)])
                S.op("sync", lambda e, c=c, g=g, zs_=zs_: e.dma_start(out=at_d[c, :, g * 4:(g + 1) * 4, :], in_=vT[:, zs_]),
                     reads=[("vT", zs_)], writes=["dram:at"], dma=True, semkey=("vT_st", zs_))
        S.barrier()
        A.reset(m_all)
        if dbg == "s3":
            for tt in range(NT):
                S.op("gpsimd", lambda e, tt=tt: e.dma_start(
                    out=out[tt * 128:(tt + 1) * 128, :].rearrange("p (k t) -> p k t", k=16), in_=at_d[tt, :, 0:16, :]),
                    writes=["dram:out"], dma=True, semkey="cp_out")
            return
        out_proj(at_d, s_w_out[0], None, mod_d[l, 2 * D:3 * D], x_src, x_dst)

    def moe(sub, l, x_src, x_dst):
        m = A.mark()
        hTb = A.alloc("hTb", [128, KC, 512], BF16)
        gT = A.alloc("gT", [32, 512], F32)
        sel_all = A.alloc("sel_all", [32, 32, 128], BF16)
        wr = A.alloc("wr", [128, KC, 32], F32)
        wrh = A.alloc("wrh", [128, KC, 32], BF16)
        wrl = A.alloc("wrl", [128, KC, 32], BF16)
        gTh = A.alloc("gTh", [32, 512], BF16)
        gTl = A.alloc("gTl", [32, 512], BF16)
        bdnb = A.alloc("bdnb", [32, D], BF16)
        brbc = A.alloc("brbc", [128, 32], F32)
        bup = A.alloc("bup", [128, 32, 16, 2], F32)
        g2bc = A.alloc("g2bc", [128, D], F32)
        wup = A.alloc("wup", [128, 2, KC, 256], BF16)
        wdn = A.alloc("wdn", [128, 2, KC, 512], BF16)
        actT = A.alloc("actT", [128, KC, 512], BF16)
        yacc = A.alloc("yacc", [128, 4, D], F32)
        gb = A.alloc("gb", [128, 2, 512], F32)
        tg = A.alloc("tg", [128, 512], F32)
        tsg = A.alloc("tsg", [128, 512], F32)
        tl_ = A.alloc("tl_", [128, 512], F32)
        S.op("sync", lambda e: e.dma_start(out=wr[:], in_=m_wr_col[:, l]), writes=["wr0"], dma=True)
        S.op("vector", lambda e: e.tensor_copy(out=wrh[:], in_=wr[:]), reads=["wr0"], writes=["wr"])
        S.op("vector", lambda e: e.tensor_tensor(out=wrl[:], in0=wr[:], in1=wrh[:], op=ALU.subtract), reads=["wr0", "wr"], writes=["wr"])
        S.op("sync", lambda e: e.dma_start(out=brbc[:], in_=m_br[l].partition_broadcast(128)), writes=["brbc"], dma=True)
        S.op("sync", lambda e: e.dma_start(out=bup[:], in_=m_bup_col[:, l]), writes=["bup"], dma=True)
        S.op("gpsimd", lambda e: e.dma_start(out=bdnb[:], in_=m_b_down[l]), writes=["bdn"], dma=True)
        S.op("sync", lambda e: e.dma_start(out=g2bc[:], in_=mod_d[l, 5 * D:6 * D].partition_broadcast(128)),
             writes=["g2bc"], dma=True)
        S.op("vector", lambda e: e.tensor_copy(
            out=sel_all[:], in_=ident[0:32, 0:32].unsqueeze(2).to_broadcast([32, 32, 128])),
            reads=["cst"], writes=["sel_all"])
        nup = 0
        ndn = 0
        npb = 0
        for tb in range(DBG_TB):
            if DBG_LVL <= -3:
                break
            prologue(sub, x_src, hTb, tiles=[tb * 4 + i for i in range(4)], router=(wrh, wrl, brbc, gT) if DBG_LVL >= -1 else None)
            if DBG_LVL <= 0:
                continue
            S.op("vector", lambda e: e.tensor_copy(out=gTh[:], in_=gT[:]), reads=["gT"], writes=["gTh"])
            S.op("vector", lambda e: e.tensor_tensor(out=gTl[:], in0=gT[:], in1=gTh[:], op=ALU.subtract), reads=["gT", "gTh"], writes=["gTl"])
            for tl in range(4):
                for db in range(4):
                    pb = 6 + (tl * 4 + db) % 2
                    S.op("tensor", lambda e, tl=tl, db=db, pb=pb: e.matmul(
                        ps[pb][:, :], lhsT=gTh[:, tl * 128:(tl + 1) * 128], rhs=bdnb[:, db * 512:(db + 1) * 512],
                        start=True, stop=True), reads=["gTh", "bdn"], writes=[("ps", pb)])
                    S.op("vector", lambda e, tl=tl, db=db, pb=pb: e.tensor_copy(
                        out=yacc[:, tl, db * 512:(db + 1) * 512], in_=ps[pb][:, :]),
                        reads=[("ps", pb)], writes=[("yacc", tl)])
            for ex in range(DBG_NE if DBG_LVL >= 2 else 0):
                gs = ex % 2
                S.op("tensor", lambda e, ex=ex: e.matmul(ps[4][:, :], lhsT=sel_all[:, ex, :], rhs=gTh[:, :],
                                                          start=True, stop=False),
                     reads=["sel_all", "gTh"], writes=[("ps", 4)])
                S.op("tensor", lambda e, ex=ex: e.matmul(ps[4][:, :], lhsT=sel_all[:, ex, :], rhs=gTl[:, :],
                                                          start=False, stop=True),
                     reads=["sel_all", "gTl"], writes=[("ps", 4)])
                S.op("scalar", lambda e, gs=gs: e.copy(out=gb[:, gs, :], in_=ps[4][:, :]),
                     reads=[("ps", 4)], writes=[("gb", gs)])
                for fc in range(KC):
                    us = nup % 2
                    nup += 1
                    S.op("gpsimd", lambda e, ex=ex, fc=fc, us=us: e.dma_start(
                        out=wup[:, us], in_=m_w_up[l][ex, :, fc * 256:(fc + 1) * 256].rearrange("(k p) n -> p k n", p=128)),
                        writes=[("wup", us)], dma=True)
                    pg = (npb % 2) * 2
                    pl = pg + 1
                    npb += 1
                    for half, pbank in ((0, pg), (1, pl)):
                        for kc in range(KC):
                            S.op("tensor", lambda e, kc=kc, us=us, half=half, pbank=pbank: e.matmul(
                                ps[pbank][:, :], lhsT=wup[:, us, kc, half::2], rhs=hTb[:, kc, :],
                                start=(kc == 0), stop=(kc == KC - 1)),
                                reads=[("wup", us), ("hT", 0), ("hT", 1), ("hT", 2), ("hT", 3)], writes=[("ps", pbank)])
                    S.op("vector", lambda e, ex=ex, fc=fc, pg=pg: e.tensor_scalar(
                        out=tg[:], in0=ps[pg][:, :], scalar1=bup[:, ex, fc, 0:1], scalar2=7.0, op0=ALU.add, op1=ALU.min),
                        reads=[("ps", pg), "bup"], writes=["tg"])
                    S.op("scalar", lambda e: e.activation(out=tsg[:], in_=tg[:], func=AF.Sigmoid, scale=1.702),
                         reads=["tg"], writes=["tsg"])
                    S.op("vector", lambda e, ex=ex, fc=fc, pl=pl: e.tensor_scalar(
                        out=tl_[:], in0=ps[pl][:, :], scalar1=bup[:, ex, fc, 1:2], scalar2=7.0, op0=ALU.add, op1=ALU.min),
                        reads=[("ps", pl), "bup"], writes=["tl_"])
                    S.op("vector", lambda e: e.tensor_scalar(
                        out=tl_[:], in0=tl_[:], scalar1=-7.0, scalar2=1.0, op0=ALU.max, op1=ALU.add),
                        reads=["tl_"], writes=["tl_"])
                    S.op("vector", lambda e: e.tensor_tensor(out=tg[:], in0=tg[:], in1=tsg[:], op=ALU.mult),
                         reads=["tg", "tsg"], writes=["tg"])
                    S.op("vector", lambda e, gs=gs: e.tensor_tensor(out=tl_[:], in0=tl_[:], in1=gb[:, gs, :], op=ALU.mult),
                         reads=["tl_", ("gb", gs)], writes=["tl_"])
                    S.op("vector", lambda e, fc=fc: e.tensor_tensor(out=actT[:, fc, :], in0=tg[:], in1=tl_[:], op=ALU.mult),
                         reads=["tg", "tl_"], writes=[("actT", fc)])
                for db in range(4):
                    ds_ = ndn % 2
                    ndn += 1
                    S.op("gpsimd", lambda e, ex=ex, db=db, ds_=ds_: e.dma_start(
                        out=wdn[:, ds_], in_=m_w_down[l][ex, :, db * 512:(db + 1) * 512].rearrange("(k p) n -> p k n", p=128)),
                        writes=[("wdn", ds_)], dma=True)
                    for tl in range(4):
                        pb = 6 + (db * 4 + tl) % 2
                        for fc in range(KC):
                            S.op("tensor", lambda e, fc=fc, tl=tl, ds_=ds_, pb=pb: e.matmul(
                                ps[pb][:, :], lhsT=actT[:, fc, tl * 128:(tl + 1) * 128], rhs=wdn[:, ds_, fc, :],
                                start=(fc == 0), stop=(fc == KC - 1)),
                                reads=[("actT", fc), ("wdn", ds_)], writes=[("ps", pb)])
                        S.op("vector", lambda e, tl=tl, db=db, pb=pb: e.tensor_tensor(
                            out=yacc[:, tl, db * 512:(db + 1) * 512], in0=yacc[:, tl, db * 512:(db + 1) * 512],
                            in1=ps[pb][:, :], op=ALU.add),
                            reads=[("ps", pb), ("yacc", tl)], writes=[("yacc", tl)])
            m2 = A.mark()
            xe = A.alloc("xe", [128, D], F32)
            for tl in range(4):
                tt = tb * 4 + tl
                S.op("sync", lambda e, tt=tt: e.dma_start(out=xe[:], in_=x_src[tt * 128:(tt + 1) * 128, :]),
                     writes=["xe"], dma=True)
                S.op("vector", lambda e, tl=tl: e.tensor_tensor(out=yacc[:, tl, :], in0=yacc[:, tl, :], in1=g2bc[:], op=ALU.mult),
                     reads=[("yacc", tl), "g2bc"], writes=[("yacc", tl)])
                S.op("vector", lambda e, tl=tl: e.tensor_tensor(out=xe[:], in0=xe[:], in1=yacc[:, tl, :], op=ALU.add),
                     reads=[("yacc", tl), "xe"], writes=["xe"])
                S.op("sync", lambda e, tt=tt: e.dma_start(out=x_dst[tt * 128:(tt + 1) * 128, :], in_=xe[:]),
                     reads=["xe"], writes=["dram:xdst"], dma=True, semkey="xe_st")
            S.barrier()
            A.reset(m2)
        S.barrier()
        A.reset(m)

    cur = x_in
    nxt = 0
    for stage in flow:
        if stage == "final":
            final_norm(cur)
            cur = None
            continue
        dst = xs_d[nxt]
        nxt += 1
        if stage.startswith("gmlp"):
            dbg = stage.split(":")[1] if ":" in stage else None
            gmlp(2, 1, cur, dst, dbg=dbg)
            if dbg:
                cur = None
                break
        elif stage == "moe0":
            moe(1, 0, cur, dst)
        elif stage == "moe1":
            moe(3, 1, cur, dst)
        elif stage.startswith("ssm"):
            dbg = stage.split(":")[1] if ":" in stage else None
            ssm(0, 0, cur, dst, dbg=dbg)
            if dbg:
                cur = None
                break
        cur = dst
        S.barrier()
    if cur is not None:
        S.op("sync", lambda e: e.dma_start(out=out, in_=cur), writes=["dram:out"], dma=True, semkey="cp_out")
    S.barrier()
    S.emit(stack)
    stack.close()
    return nc


def host_inputs(inputs, b, flow=None):
    f = lambda a: np.ascontiguousarray(a, dtype=np.float32)
    i = np.arange(128)
    consts = np.stack([
        np.eye(128),
        (i[:, None] <= i[None, :]), (i[:, None] >= i[None, :]),
        (i[:, None] > i[None, :]), (i[:, None] < i[None, :]),
        np.ones((128, 128)),
    ]).astype(np.float32)
    ng = np.stack([inputs["norm_mix_g"][0], inputs["norm_ffn_g"][0], inputs["norm_mix_g"][1], inputs["norm_ffn_g"][1]])
    m = {
        "x": f(inputs["x"][b]),
        "c_col": f(inputs["c"][b].reshape(KC, 128).T),
        "consts": consts,
        "ada_w": f(inputs["ada_w"]),
        "ada_b": f(inputs["ada_b"]),
        "ng_col": f(ng.reshape(4, KC, 128).transpose(2, 0, 1)),
        "final_g": f(inputs["final_g"]),
        "s_w_in": f(inputs["ssm_w_in"]),
        "s_conv_col": f(inputs["ssm_conv_w"][0].reshape(5, 48, 128).transpose(2, 1, 0)),
        "s_convb_col": f(inputs["ssm_conv_b"][0].reshape(48, 128).T),
        "s_dtb_col": f(inputs["ssm_dt_bias"][0].reshape(128, 1)),
        "s_alog_col": f(inputs["ssm_a_log"][0].reshape(128, 1)),
        "s_d_rep": f(np.repeat(inputs["ssm_d"][0], 64)),
        "s_norm_g": f(inputs["ssm_norm_g"][0]),
        "s_w_out": f(inputs["ssm_w_out"]),
        "g_w_in": f(inputs["gmlp_w_in"]), "g_b_in": f(inputs["gmlp_b_in"]),
        "g_ln_g": f(inputs["gmlp_ln_g"]), "g_ln_b": f(inputs["gmlp_ln_b"]),
        "g_w_s": f(inputs["gmlp_w_s"]), "g_bs_col": f(inputs["gmlp_b_s"][0].T),
        "g_w_out": f(inputs["gmlp_w_out"]), "g_b_out": f(inputs["gmlp_b_out"]),
        "m_wr_col": f(inputs["moe_w_router"].reshape(2, KC, 128, 32).transpose(2, 0, 1, 3)),
        "m_br": f(inputs["moe_b_router"]),
        "m_bup_col": f(inputs["moe_b_up"].reshape(2, 32, 16, 128, 2).transpose(3, 0, 1, 2, 4)),
        "m_b_down": f(inputs["moe_b_down"]),
    }
    if flow is None or any(st.startswith("moe") for st in flow):
        for i in range(2):
            m["m_w_up%d" % i] = f(inputs["moe_w_up"][i, :DBG_NE])
            m["m_w_down%d" % i] = f(inputs["moe_w_down"][i, :DBG_NE])
    return m


def kernel(**inputs):
    n = 8
    nc = build_program()
    in_maps = [host_inputs(inputs, b) for b in range(n)]
    res = run_bass_kernel_spmd(nc, in_maps, core_ids=list(range(n)))
    return np.stack([r["out"] for r in res.results], axis=0)
```

```python
import numpy as np
import concourse.bass as bass
import concourse.mybir as mybir
from concourse.bass_utils import run_bass_kernel_spmd

F32 = mybir.dt.float32
BF16 = mybir.dt.bfloat16
AF = mybir.ActivationFunctionType
ALU = mybir.AluOpType
AX = mybir.AxisListType

D = 2048
L = 2048
NT = 16
KC = 16
D_INNER = 4096
D_SSM_IN = 10368
import os
N_EXP = 32
DBG_NE = int(os.environ.get("MOE_NE", "32"))
DBG_TB = int(os.environ.get("MOE_TB", "4"))
DBG_LVL = int(os.environ.get("MOE_LVL", "9"))
ENGS = ["tensor", "vector", "scalar", "gpsimd", "sync"]


class Sched:
    def __init__(self, nc):
        self.nc = nc
        self.ops = {e: [] for e in ENGS}
        self.track = {}
        self.dma_cnt = {}
        self.last_dma = {}

    def op(self, eng, fn, reads=(), writes=(), dma=False, semkey=None):
        deps = set()
        for r in reads:
            t = self.track.get(r)
            if t and t["w"] is not None:
                deps.add(t["w"])
        for w in writes:
            t = self.track.get(w)
            if t:
                if t["w"] is not None:
                    deps.add(t["w"])
                deps.update(t["r"])
        idx = len(self.ops[eng])
        if dma:
            if semkey is None:
                semkey = writes[0] if (writes and not str(writes[0]).startswith("dram:")) else reads[0]
            cnt = self.dma_cnt.get(semkey, 0) + 1
            self.dma_cnt[semkey] = cnt
            ref = ("dma", semkey, cnt)
        else:
            ref = ("eng", eng, idx)
        if eng == "tensor":
            deps = {d for d in deps if not (d[0] == "eng" and d[1] == "tensor")}
        self.ops[eng].append(dict(fn=fn, deps=deps, ref=ref, dma=dma, needed=False, semkey=semkey))
        for r in reads:
            self.track.setdefault(r, {"w": None, "r": []})["r"].append(ref)
        for w in writes:
            self.track[w] = {"w": ref, "r": []}
        return ref

    def barrier(self):
        deps = set()
        for e in ENGS:
            for i in range(len(self.ops[e]) - 1, -1, -1):
                o = self.ops[e][i]
                if o["fn"] is not None and not o["dma"]:
                    deps.add(o["ref"])
                    break
        for k, c in self.dma_cnt.items():
            deps.add(("dma", k, c))
        for e in ENGS:
            self.ops[e].append(dict(fn=None, deps=set(deps), ref=None, dma=False, needed=False, semkey=None))
        self.track = {}

    def emit(self, stack):
        nc = self.nc
        for e in ENGS:
            for o in self.ops[e]:
                for d in o["deps"]:
                    if d[0] == "eng":
                        self.ops[d[1]][d[2]]["needed"] = True
        cum = {}
        for e in ENGS:
            c = 0
            arr = []
            for o in self.ops[e]:
                if o["needed"]:
                    c += 1
                arr.append(c)
            cum[e] = arr
        esem = {e: stack.enter_context(nc.semaphore("s_" + e)) for e in ENGS}
        dsem = {}
        for k in self.dma_cnt:
            dsem[k] = stack.enter_context(nc.semaphore("d%d" % len(dsem)))
        block = stack.enter_context(nc.Block())

        def make(e):
            def body(eng):
                waited = {}
                for o in self.ops[e]:
                    for d in sorted(o["deps"], key=lambda z: str(z)):
                        if d[0] == "eng":
                            sem, val, key = esem[d[1]], cum[d[1]][d[2]], ("e", d[1])
                        else:
                            sem, val, key = dsem[d[1]], 16 * d[2], ("d", d[1])
                        if waited.get(key, 0) >= val:
                            continue
                        eng.wait_ge(sem, val)
                        waited[key] = val
                    if o["fn"] is None:
                        continue
                    ins = o["fn"](eng)
                    if o["dma"]:
                        ins.then_inc(dsem[o["semkey"]], 16)
                    elif o["needed"]:
                        ins.then_inc(esem[e], 1)
            return body

        for e in ENGS:
            getattr(block, e)(make(e))


class Arena:
    def __init__(self, nc, limit=229376):
        self.nc = nc
        self.off = 18560
        self.limit = limit
        self.n = 0

    def alloc(self, name, shape, dtype):
        esz = 2 if dtype == BF16 else 4
        per_part = int(np.prod(shape[1:])) * esz
        off = (self.off + 63) // 64 * 64
        assert off + per_part <= self.limit, "SBUF arena overflow %s %d" % (name, off + per_part)
        self.n += 1
        t = self.nc.alloc_sbuf_tensor_at("%s_%d" % (name, self.n), list(shape), dtype, offset=off)
        self.off = off + per_part
        return t

    def mark(self):
        return self.off

    def reset(self, m):
        self.off = m


def build_program(flow=("ssm", "moe0", "gmlp", "moe1", "final"), taps=()):
    import contextlib
    nc = bass.Bass("TRN2", target_bir_lowering=False)
    S = Sched(nc)
    A = Arena(nc)
    stack = contextlib.ExitStack()

    def din(name, shape, dt=F32):
        return nc.dram_tensor(name, list(shape), dt, kind="ExternalInput").ap()

    def dscr(name, shape, dt=F32):
        return nc.dram_tensor(name, list(shape), dt, kind="Internal").ap()

    x_in = din("x", [L, D])
    c_col = din("c_col", [128, KC])
    consts = din("consts", [6, 128, 128])
    ada_w = din("ada_w", [2, D, 6 * D])
    ada_b = din("ada_b", [2, 6 * D])
    ng_col = din("ng_col", [128, 4, KC])
    final_g = din("final_g", [D])
    out = nc.dram_tensor("out", [L, D], F32, kind="ExternalOutput").ap()
    tap_aps = {}
    for (tn, tshape) in taps:
        tap_aps[tn] = nc.dram_tensor("tap_" + tn, list(tshape), F32, kind="ExternalOutput").ap()

    g_w_in = din("g_w_in", [1, D, 8192]); g_b_in = din("g_b_in", [1, 8192])
    g_ln_g = din("g_ln_g", [1, 4096]); g_ln_b = din("g_ln_b", [1, 4096])
    g_w_s = din("g_w_s", [1, 16, 128, 128]); g_bs_col = din("g_bs_col", [128, 16])
    g_w_out = din("g_w_out", [1, 4096, D]); g_b_out = din("g_b_out", [1, D])
    m_wr_col = din("m_wr_col", [128, 2, KC, 32]); m_br = din("m_br", [2, 32])
    has_moe = any(st.startswith("moe") for st in flow)
    m_bup_col = din("m_bup_col", [128, 2, 32, 16, 2]); m_b_down = din("m_b_down", [2, 32, D])
    if has_moe:
        m_w_up = [din("m_w_up%d" % i, [DBG_NE, D, 4096]) for i in range(2)]
        m_w_down = [din("m_w_down%d" % i, [DBG_NE, D, D]) for i in range(2)]
    s_w_in = din("s_w_in", [1, D, D_SSM_IN]); s_conv_col = din("s_conv_col", [128, 48, 5])
    s_convb_col = din("s_convb_col", [128, 48]); s_dtb_col = din("s_dtb_col", [128, 1]); s_alog_col = din("s_alog_col", [128, 1])
    s_d_rep = din("s_d_rep", [4096]); s_norm_g = din("s_norm_g", [4096]); s_w_out = din("s_w_out", [1, 4096, D])
    zs_d = dscr("zs_d", [L, 4096]); xsd = dscr("xsd", [L, 4096])
    bct_d = dscr("bct_d", [16, 128, L], BF16); btok_d = dscr("btok_d", [L, 1024], BF16)
    uv_d = dscr("uv_d", [L, 8192])
    gt_d = dscr("gt_d", [32, 1024])
    at_d = dscr("at_d", [NT, 128, 32, 128], BF16)
    mod_d = dscr("mod_d", [2, 6 * D])
    xs_d = [dscr("xs%d" % i, [L, D]) for i in range(4)]

    cst = A.alloc("cst", [128, 6, 128], F32)
    ident, tri_le, tri_ge, u_gt, u_lt, ones = [cst[:, i, :] for i in range(6)]
    identb = A.alloc("identb", [128, 128], BF16)
    cstb = A.alloc("cstb", [128, 6, 128], BF16)
    tri_le_b, tri_ge_b, ones_b = cstb[:, 1, :], cstb[:, 2, :], cstb[:, 5, :]
    modcol = A.alloc("modcol", [128, 2, 96], F32)
    ngc = A.alloc("ngc", [128, 4, KC], F32)
    gs_col = A.alloc("gs_col", [128, 4, 2, KC], F32)
    ps = [stack.enter_context(nc.psum_tensor("ps%d" % i, [128, 512], F32)) for i in range(8)]

    S.op("sync", lambda e: e.dma_start(out=cst[:], in_=consts.rearrange("c p n -> p c n")),
         writes=["cst"], dma=True)
    S.op("sync", lambda e: e.dma_start(out=ngc[:], in_=ng_col), writes=["ngc"], dma=True)
    S.op("vector", lambda e: e.tensor_copy(out=identb[:], in_=ident), reads=["cst"], writes=["identb"])
    S.op("vector", lambda e: e.tensor_copy(out=cstb[:], in_=cst[:]), reads=["cst"], writes=["cstb"])

    m0 = A.mark()
    ccol = A.alloc("ccol", [128, KC], F32)
    cact = A.alloc("cact", [128, KC], BF16)
    wa = A.alloc("wa", [128, 2, KC, 512], BF16)
    abrow = A.alloc("abrow", [1, 2, 512], F32)
    mrow = A.alloc("mrow", [1, 2, 512], F32)
    S.op("sync", lambda e: e.dma_start(out=ccol[:], in_=c_col), writes=["ccol"], dma=True)
    S.op("scalar", lambda e: e.activation(out=cact[:], in_=ccol[:], func=AF.Silu), reads=["ccol"], writes=["cact"])
    blk = 0
    for l in range(2):
        for nb in range(24):
            s = blk % 2
            S.op("gpsimd", lambda e, l=l, nb=nb, s=s: e.dma_start(
                out=wa[:, s], in_=ada_w[l, :, nb * 512:(nb + 1) * 512].rearrange("(k p) n -> p k n", p=128)),
                writes=[("wa", s)], dma=True)
            S.op("sync", lambda e, l=l, nb=nb, s=s: e.dma_start(
                out=abrow[:, s, :], in_=ada_b[l:l + 1, nb * 512:(nb + 1) * 512]),
                writes=[("abrow", s)], dma=True)
            pb = blk % 2
            for kc in range(KC):
                S.op("tensor", lambda e, kc=kc, s=s, pb=pb: e.matmul(
                    ps[pb][0:1, :], lhsT=cact[:, kc:kc + 1], rhs=wa[:, s, kc, :],
                    start=(kc == 0), stop=(kc == KC - 1)),
                    reads=["cact", ("wa", s)], writes=[("ps", pb)])
            S.op("vector", lambda e, s=s, pb=pb: e.tensor_tensor(
                out=mrow[:, s, :], in0=ps[pb][0:1, :], in1=abrow[:, s, :], op=ALU.add),
                reads=[("ps", pb), ("abrow", s)], writes=[("mrow", s)])
            S.op("sync", lambda e, l=l, nb=nb, s=s: e.dma_start(
                out=mod_d[l:l + 1, nb * 512:(nb + 1) * 512], in_=mrow[:, s, :]),
                reads=[("mrow", s)], writes=["dram:mod"], dma=True, semkey=("mrow_st", s))
            blk += 1
    mrows = A.alloc("mrows", [96, 128], F32)
    for l in range(2):
        S.op("sync", lambda e, l=l: e.dma_start(out=mrows[:], in_=mod_d[l].rearrange("(j p) -> j p", p=128)),
             reads=["dram:mod"], writes=["mrows"], dma=True)
        S.op("tensor", lambda e: e.transpose(ps[2][:, 0:96], mrows[:], ident[0:96, 0:96]),
             reads=["mrows", "cst"], writes=[("ps", 2)])
        S.op("vector", lambda e, l=l: e.tensor_copy(out=modcol[:, l, :], in_=ps[2][:, 0:96]),
             reads=[("ps", 2)], writes=["modcol"])
    for l in range(2):
        for half in range(2):
            sub = 2 * l + half
            base = 48 * half
            S.op("vector", lambda e, l=l, sub=sub, base=base: e.scalar_tensor_tensor(
                out=gs_col[:, sub, 0, :], in0=modcol[:, l, base + 16:base + 32], scalar=1.0,
                in1=ngc[:, sub, :], op0=ALU.add, op1=ALU.mult),
                reads=["modcol", "ngc"], writes=["gs_col"])
            S.op("vector", lambda e, l=l, sub=sub, base=base: e.tensor_copy(
                out=gs_col[:, sub, 1, :], in_=modcol[:, l, base:base + 16]),
                reads=["modcol"], writes=["gs_col"])
    S.barrier()
    A.reset(m0)
    if "modcol" in tap_aps:
        S.op("sync", lambda e: e.dma_start(out=tap_aps["modcol"], in_=modcol[:].rearrange("p l j -> p (l j)")),
             reads=["modcol"], writes=["dram:tap1"], dma=True, semkey="tap1")

    def prologue(sub, x_src, hT, tiles=None, router=None):
        m = A.mark()
        tiles = list(range(NT)) if tiles is None else tiles
        t0 = tiles[0]
        if router is not None:
            wrh, wrl, brbc, gT = router
            hf = A.alloc("hf", [128, 2, 4, 128], F32)
            hfh = A.alloc("hfh", [128, 2, 4, 128], BF16)
            hfl = A.alloc("hfl", [128, 2, 4, 128], BF16)
            rg = A.alloc("rg", [128, 2, 160], F32)
        xt = A.alloc("xt", [128, 2, D], F32)
        st = A.alloc("st", [128, 2, 4], F32)
        xn1 = A.alloc("xn", [128, D], F32)
        sq = xn1
        for tt in tiles:
            s = tt % 2
            tl = tt - t0
            S.op("sync", lambda e, tt=tt, s=s: e.dma_start(out=xt[:, s, :], in_=x_src[tt * 128:(tt + 1) * 128, :]),
                 writes=[("xt", s)], dma=True)
            S.op("vector", lambda e, s=s: e.tensor_tensor(out=sq[:], in0=xt[:, s, :], in1=xt[:, s, :], op=ALU.mult),
                 reads=[("xt", s)], writes=["xn"])
            S.op("vector", lambda e, s=s: e.reduce_sum(out=st[:, s, 0:1], in_=sq[:], axis=AX.X),
                 reads=["xn"], writes=[("st", s)])
            S.op("vector", lambda e, s=s: e.tensor_scalar(out=st[:, s, 1:2], in0=st[:, s, 0:1], scalar1=1.0 / D,
                                                          scalar2=1e-6, op0=ALU.mult, op1=ALU.add),
                 reads=[("st", s)], writes=[("st", s)])
            S.op("scalar", lambda e, s=s: e.activation(out=st[:, s, 3:4], in_=st[:, s, 1:2], func=AF.Sqrt),
                 reads=[("st", s)], writes=[("st", s)])
            S.op("vector", lambda e, s=s: e.reciprocal(out=st[:, s, 2:3], in_=st[:, s, 3:4]),
                 reads=[("st", s)], writes=[("st", s)])
            S.op("scalar", lambda e, s=s: e.activation(out=xn1[:], in_=xt[:, s, :], func=AF.Copy,
                                                       scale=st[:, s, 2:3]),
                 reads=[("xt", s), ("st", s)], writes=["xn"])
            for q in range(4):
                pb = (tt * 4 + q) % 4
                for j in range(4):
                    kc = q * 4 + j
                    S.op("tensor", lambda e, s=s, kc=kc, pb=pb, j=j: e.transpose(
                        ps[pb][:, j * 128:(j + 1) * 128], xn1[:, kc * 128:(kc + 1) * 128], ident),
                        reads=["xn", "cst"], writes=[("ps", pb)])
                for j in range(4):
                    kc = q * 4 + j
                    S.op("scalar", lambda e, kc=kc, pb=pb, j=j, tl=tl: e.activation(
                        out=hT[:, kc, tl * 128:(tl + 1) * 128], in_=ps[pb][:, j * 128:(j + 1) * 128],
                        func=AF.Identity, scale=gs_col[:, sub, 0, kc:kc + 1], bias=gs_col[:, sub, 1, kc:kc + 1]),
                        reads=[("ps", pb), "gs_col"], writes=[("hT", tl)])
                if router is not None:
                    hs = q % 2
                    for j in range(4):
                        kc = q * 4 + j
                        S.op("vector", lambda e, kc=kc, pb=pb, j=j, hs=hs: e.tensor_scalar(
                            out=hf[:, hs, j, :], in0=ps[pb][:, j * 128:(j + 1) * 128],
                            scalar1=gs_col[:, sub, 0, kc:kc + 1], scalar2=gs_col[:, sub, 1, kc:kc + 1],
                            op0=ALU.mult, op1=ALU.add),
                            reads=["gs_col"], writes=[("hf", hs), ("ps", pb)])
                    S.op("vector", lambda e, hs=hs: e.tensor_copy(out=hfh[:, hs], in_=hf[:, hs]),
                         reads=[("hf", hs)], writes=[("hfh", hs)])
                    S.op("vector", lambda e, hs=hs: e.tensor_tensor(out=hfl[:, hs], in0=hf[:, hs], in1=hfh[:, hs], op=ALU.subtract),
                         reads=[("hf", hs), ("hfh", hs)], writes=[("hfl", hs)])
                    for j in range(4):
                        kc = q * 4 + j
                        for pi, (a_, b_) in enumerate(((hfh, wrh), (hfh, wrl), (hfl, wrh))):
                            S.op("tensor", lambda e, kc=kc, j=j, hs=hs, a_=a_, b_=b_, pi=pi: e.matmul(
                                ps[5][:, 0:32], lhsT=a_[:, hs, j, :], rhs=b_[:, kc, :],
                                start=(kc == 0 and pi == 0), stop=(kc == KC - 1 and pi == 2)),
                                reads=[("hfh", hs), ("hfl", hs), "wr"], writes=[("ps", 5)])
            if router is not None and DBG_LVL >= 0:
                r = tt % 2
                lg, mx8, sel, nm, ex, ssum = (rg[:, r, 0:32], rg[:, r, 32:40], rg[:, r, 40:72], rg[:, r, 72:73],
                                              rg[:, r, 80:112], rg[:, r, 112:114])
                gts = rg[:, r, 120:152]
                K_ = ("rg", r)
                S.op("vector", lambda e, lg=lg: e.tensor_tensor(out=lg, in0=ps[5][:, 0:32], in1=brbc[:], op=ALU.add),
                     reads=[("ps", 5), "brbc"], writes=[K_])
                S.op("vector", lambda e, lg=lg, mx8=mx8: e.max(out=mx8, in_=lg), reads=[K_], writes=[K_])
                S.op("vector", lambda e, lg=lg, mx8=mx8, sel=sel: e.tensor_scalar(
                    out=sel, in0=lg, scalar1=mx8[:, 3:4], scalar2=None, op0=ALU.is_ge), reads=[K_], writes=[K_])
                S.op("vector", lambda e, mx8=mx8, nm=nm: e.tensor_scalar(
                    out=nm, in0=mx8[:, 0:1], scalar1=-1.0, scalar2=None, op0=ALU.mult), reads=[K_], writes=[K_])
                S.op("scalar", lambda e, lg=lg, nm=nm, ex=ex: e.activation(out=ex, in_=lg, func=AF.Exp, bias=nm),
                     reads=[K_], writes=[K_])
                S.op("vector", lambda e, ex=ex, sel=sel: e.tensor_tensor(out=ex, in0=ex, in1=sel, op=ALU.mult),
                     reads=[K_], writes=[K_])
                S.op("vector", lambda e, ex=ex, ssum=ssum: e.reduce_sum(out=ssum[:, 0:1], in_=ex, axis=AX.X),
                     reads=[K_], writes=[K_])
                S.op("vector", lambda e, ssum=ssum: e.reciprocal(out=ssum[:, 1:2], in_=ssum[:, 0:1]),
                     reads=[K_], writes=[K_])
                S.op("vector", lambda e, ex=ex, ssum=ssum, gts=gts: e.tensor_scalar(
                    out=gts, in0=ex, scalar1=ssum[:, 1:2], scalar2=None, op0=ALU.mult), reads=[K_], writes=[K_])
                S.op("tensor", lambda e, gts=gts: e.transpose(ps[6][0:32, 0:128], gts, ident),
                     reads=[K_, "cst"], writes=[("ps", 6)])
                S.op("vector", lambda e, tl=tl: e.tensor_copy(out=gT[:, tl * 128:(tl + 1) * 128], in_=ps[6][0:32, 0:128]),
                     reads=[("ps", 6)], writes=["gT"])
        S.barrier()
        A.reset(m)

    def final_norm(x_src):
        m = A.mark()
        xt = A.alloc("xt", [128, 2, D], F32)
        sq = A.alloc("sq", [128, D], F32)
        st = A.alloc("st", [128, 2, 4], F32)
        fg = A.alloc("fg", [128, D], F32)
        yo = A.alloc("yo", [128, 2, D], F32)
        S.op("sync", lambda e: e.dma_start(out=fg[:], in_=final_g.partition_broadcast(128)), writes=["fg"], dma=True)
        for tt in range(NT):
            s = tt % 2
            S.op("sync", lambda e, tt=tt, s=s: e.dma_start(out=xt[:, s, :], in_=x_src[tt * 128:(tt + 1) * 128, :]),
                 writes=[("xt", s)], dma=True)
            S.op("vector", lambda e, s=s: e.tensor_tensor(out=sq[:], in0=xt[:, s, :], in1=xt[:, s, :], op=ALU.mult),
                 reads=[("xt", s)], writes=["sq"])
            S.op("vector", lambda e, s=s: e.reduce_sum(out=st[:, s, 0:1], in_=sq[:], axis=AX.X),
                 reads=["sq"], writes=[("st", s)])
            S.op("vector", lambda e, s=s: e.tensor_scalar(out=st[:, s, 1:2], in0=st[:, s, 0:1], scalar1=1.0 / D,
                                                          scalar2=1e-6, op0=ALU.mult, op1=ALU.add),
                 reads=[("st", s)], writes=[("st", s)])
            S.op("scalar", lambda e, s=s: e.activation(out=st[:, s, 3:4], in_=st[:, s, 1:2], func=AF.Sqrt),
                 reads=[("st", s)], writes=[("st", s)])
            S.op("vector", lambda e, s=s: e.reciprocal(out=st[:, s, 2:3], in_=st[:, s, 3:4]),
                 reads=[("st", s)], writes=[("st", s)])
            S.op("vector", lambda e, s=s: e.scalar_tensor_tensor(
                out=yo[:, s, :], in0=xt[:, s, :], scalar=st[:, s, 2:3], in1=fg[:], op0=ALU.mult, op1=ALU.mult),
                reads=[("xt", s), ("st", s), "fg"], writes=[("yo", s)])
            S.op("sync", lambda e, tt=tt, s=s: e.dma_start(out=out[tt * 128:(tt + 1) * 128, :], in_=yo[:, s, :]),
                 reads=[("yo", s)], writes=["dram:out"], dma=True, semkey=("yo_st", s))
        A.reset(m)

    def linear_tok(hT, w_ap, ncols, evac, kcn=KC, tag="lt"):
        m = A.mark()
        wp = A.alloc("wp_" + tag, [128, 2, kcn, 512], BF16)
        cnt = 0
        for nb in range(ncols // 512):
            s = nb % 2
            S.op("gpsimd", lambda e, nb=nb, s=s: e.dma_start(
                out=wp[:, s], in_=w_ap[:, nb * 512:(nb + 1) * 512].rearrange("(k p) n -> p k n", p=128)),
                writes=[("wp", s)], dma=True)
            for tt in range(NT):
                pb = cnt % 4
                cnt += 1
                for kc in range(kcn):
                    S.op("tensor", lambda e, kc=kc, s=s, pb=pb, tt=tt: e.matmul(
                        ps[pb][:, :], lhsT=hT[:, kc, tt * 128:(tt + 1) * 128], rhs=wp[:, s, kc, :],
                        start=(kc == 0), stop=(kc == kcn - 1)),
                        reads=[("hT", tt), ("wp", s)], writes=[("ps", pb)])
                evac(tt, nb, ps[pb], pb)
        A.reset(m)

    def out_proj(at_d, w_ap, bias_ap, gate_row_ap, x_src, x_dst):
        m = A.mark()
        at = A.alloc("at", [128, 8, 32, 128], BF16)
        wp = A.alloc("wpo", [128, 2, 32, 512], BF16)
        gbc = A.alloc("gbc", [128, D], F32)
        bbc = A.alloc("bbc", [128, D], F32)
        xt = A.alloc("xto", [128, 2, 512], F32)
        yt = A.alloc("yto", [128, 2, 512], F32)
        S.op("sync", lambda e: e.dma_start(out=gbc[:], in_=gate_row_ap.partition_broadcast(128)), writes=["gbc"], dma=True)
        if bias_ap is not None:
            S.op("sync", lambda e: e.dma_start(out=bbc[:], in_=bias_ap.partition_broadcast(128)), writes=["bbc"], dma=True)
        cnt = 0
        for th in range(2):
            for t8 in range(8):
                S.op("sync", lambda e, th=th, t8=t8: e.dma_start(out=at[:, t8], in_=at_d[th * 8 + t8]),
                     writes=[("at", t8)], dma=True)
            for db in range(4):
                s = (th * 4 + db) % 2
                S.op("gpsimd", lambda e, db=db, s=s: e.dma_start(
                    out=wp[:, s], in_=w_ap[:, db * 512:(db + 1) * 512].rearrange("(k p) n -> p k n", p=128)),
                    writes=[("wpo", s)], dma=True)
                for t8 in range(8):
                    tt = th * 8 + t8
                    pb = cnt % 4
                    xs_ = cnt % 2
                    cnt += 1
                    S.op("sync", lambda e, tt=tt, db=db, xs_=xs_: e.dma_start(
                        out=xt[:, xs_, :], in_=x_src[tt * 128:(tt + 1) * 128, db * 512:(db + 1) * 512]),
                        writes=[("xto", xs_)], dma=True)
                    for kc in range(32):
                        S.op("tensor", lambda e, kc=kc, s=s, pb=pb, t8=t8: e.matmul(
                            ps[pb][:, :], lhsT=at[:, t8, kc, :], rhs=wp[:, s, kc, :],
                            start=(kc == 0), stop=(kc == 31)),
                            reads=[("at", t8), ("wpo", s)], writes=[("ps", pb)])
                    if bias_ap is not None:
                        S.op("vector", lambda e, pb=pb, xs_=xs_, db=db: e.tensor_tensor(
                            out=yt[:, xs_, :], in0=ps[pb][:, :], in1=bbc[:, db * 512:(db + 1) * 512], op=ALU.add),
                            reads=[("ps", pb), "bbc"], writes=[("yto", xs_)])
                        S.op("vector", lambda e, xs_=xs_, db=db: e.tensor_tensor(
                            out=yt[:, xs_, :], in0=yt[:, xs_, :], in1=gbc[:, db * 512:(db + 1) * 512], op=ALU.mult),
                            reads=[("yto", xs_), "gbc"], writes=[("yto", xs_)])
                    else:
                        S.op("vector", lambda e, pb=pb, xs_=xs_, db=db: e.tensor_tensor(
                            out=yt[:, xs_, :], in0=ps[pb][:, :], in1=gbc[:, db * 512:(db + 1) * 512], op=ALU.mult),
                            reads=[("ps", pb), "gbc"], writes=[("yto", xs_)])
                    S.op("vector", lambda e, xs_=xs_: e.tensor_tensor(
                        out=yt[:, xs_, :], in0=yt[:, xs_, :], in1=xt[:, xs_, :], op=ALU.add),
                        reads=[("yto", xs_), ("xto", xs_)], writes=[("yto", xs_)])
                    S.op("sync", lambda e, tt=tt, db=db, xs_=xs_: e.dma_start(
                        out=x_dst[tt * 128:(tt + 1) * 128, db * 512:(db + 1) * 512], in_=yt[:, xs_, :]),
                        reads=[("yto", xs_)], writes=["dram:xdst"], dma=True, semkey=("yto_st", xs_))
        S.barrier()
        A.reset(m)

    psb = [p[:].bitcast(BF16) for p in ps]

    def gmlp(sub, l, x_src, x_dst, dbg=None):
        w_in = g_w_in[0]
        m = A.mark()
        hT = A.alloc("hT", [128, KC, L], BF16)
        prologue(sub, x_src, hT)
        bb = A.alloc("bb", [128, 2, 512], F32)
        uvt = A.alloc("uvt", [128, 4, 512], F32)
        state = {"n": 0}

        def evac(tt, nb, pt, pb):
            k = state["n"] % 4
            state["n"] += 1
            bs_ = nb % 2
            if tt == 0:
                S.op("sync", lambda e, nb=nb, bs_=bs_: e.dma_start(
                    out=bb[:, bs_, :], in_=g_b_in[0, nb * 512:(nb + 1) * 512].partition_broadcast(128)),
                    writes=[("bb", bs_)], dma=True)
            S.op("vector", lambda e, k=k, bs_=bs_, pt=pt: e.tensor_tensor(
                out=uvt[:, k, :], in0=pt[:, :], in1=bb[:, bs_, :], op=ALU.add),
                reads=[("ps", pb), ("bb", bs_)], writes=[("uvt", k)])
            S.op("scalar", lambda e, k=k: e.activation(out=uvt[:, k, :], in_=uvt[:, k, :], func=AF.Gelu),
                 reads=[("uvt", k)], writes=[("uvt", k)])
            S.op("sync", lambda e, k=k, tt=tt, nb=nb: e.dma_start(
                out=uv_d[tt * 128:(tt + 1) * 128, nb * 512:(nb + 1) * 512], in_=uvt[:, k, :]),
                reads=[("uvt", k)], writes=["dram:uv"], dma=True, semkey=("uvt_st", k))
        linear_tok(hT, w_in, 8192, evac, tag="g1")
        S.barrier()
        A.reset(m)
        if dbg in ("uv", "uv2"):
            c0 = 0 if dbg == "uv" else 4096
            S.op("sync", lambda e: e.dma_start(out=out, in_=uv_d[:, c0:c0 + 2048]), writes=["dram:out"], dma=True, semkey="cp_out")
            return
        m = A.mark()
        lng = A.alloc("lng", [128, 4096], F32)
        lnb = A.alloc("lnb", [128, 4096], F32)
        wsT = A.alloc("wsT", [128, 16, 128], BF16)
        wsf = A.alloc("wsf", [128, 128], F32)
        bsc = A.alloc("bsc", [128, 16], F32)
        ut = A.alloc("ut", [128, 2, 4096], F32)
        vt = A.alloc("vt", [128, 2, 4096], F32)
        sq2 = A.alloc("sq2", [128, 4096], F32)
        vn = A.alloc("vn", [128, 4096], BF16)
        pp = A.alloc("pp", [128, 4096], BF16)
        ppT = A.alloc("ppT", [128, 2, 32, 128], BF16)
        st = A.alloc("st2", [128, 2, 8], F32)
        S.op("sync", lambda e: e.dma_start(out=lng[:], in_=g_ln_g[0].partition_broadcast(128)), writes=["lng"], dma=True)
        S.op("sync", lambda e: e.dma_start(out=lnb[:], in_=g_ln_b[0].partition_broadcast(128)), writes=["lnb"], dma=True)
        S.op("sync", lambda e: e.dma_start(out=bsc[:], in_=g_bs_col), writes=["bsc"], dma=True)
        for g in range(16):
            S.op("sync", lambda e, g=g: e.dma_start(out=wsf[:], in_=g_w_s[0, g]), writes=["wsf"], dma=True)
            S.op("tensor", lambda e: e.transpose(ps[4][:, 0:128], wsf[:], ident), reads=["wsf", "cst"], writes=[("ps", 4)])
            S.op("vector", lambda e, g=g: e.tensor_copy(out=wsT[:, g, :], in_=ps[4][:, 0:128]),
                 reads=[("ps", 4)], writes=["wsT"])
        for tt in range(NT):
            s = tt % 2
            S.op("sync", lambda e, tt=tt, s=s: e.dma_start(out=ut[:, s, :], in_=uv_d[tt * 128:(tt + 1) * 128, 0:4096]),
                 reads=["dram:uv"], writes=[("ut", s)], dma=True)
            S.op("sync", lambda e, tt=tt, s=s: e.dma_start(out=vt[:, s, :], in_=uv_d[tt * 128:(tt + 1) * 128, 4096:8192]),
                 reads=["dram:uv"], writes=[("vt", s)], dma=True)
            S.op("vector", lambda e, s=s: e.reduce_sum(out=st[:, s, 0:1], in_=vt[:, s, :], axis=AX.X),
                 reads=[("vt", s)], writes=[("st2", s)])
            S.op("vector", lambda e, s=s: e.tensor_tensor(out=sq2[:], in0=vt[:, s, :], in1=vt[:, s, :], op=ALU.mult),
                 reads=[("vt", s)], writes=["sq2"])
            S.op("vector", lambda e, s=s: e.reduce_sum(out=st[:, s, 1:2], in_=sq2[:], axis=AX.X),
                 reads=["sq2"], writes=[("st2", s)])
            S.op("vector", lambda e, s=s: e.tensor_scalar(out=st[:, s, 2:4], in0=st[:, s, 0:2], scalar1=1.0 / 4096,
                                                          scalar2=None, op0=ALU.mult),
                 reads=[("st2", s)], writes=[("st2", s)])
            S.op("vector", lambda e, s=s: e.tensor_tensor(out=st[:, s, 4:5], in0=st[:, s, 2:3], in1=st[:, s, 2:3], op=ALU.mult),
                 reads=[("st2", s)], writes=[("st2", s)])
            S.op("vector", lambda e, s=s: e.tensor_tensor(out=st[:, s, 5:6], in0=st[:, s, 3:4], in1=st[:, s, 4:5], op=ALU.subtract),
                 reads=[("st2", s)], writes=[("st2", s)])
            S.op("scalar", lambda e, s=s: e.activation(out=st[:, s, 6:7], in_=st[:, s, 5:6], func=AF.Sqrt, bias=1e-6),
                 reads=[("st2", s)], writes=[("st2", s)])
            S.op("vector", lambda e, s=s: e.reciprocal(out=st[:, s, 7:8], in_=st[:, s, 6:7]),
                 reads=[("st2", s)], writes=[("st2", s)])
            S.op("vector", lambda e, s=s: e.tensor_scalar(out=sq2[:], in0=vt[:, s, :], scalar1=st[:, s, 2:3],
                                                          scalar2=st[:, s, 7:8], op0=ALU.subtract, op1=ALU.mult),
                 reads=[("vt", s), ("st2", s)], writes=["sq2"])
            S.op("vector", lambda e: e.tensor_tensor(out=sq2[:], in0=sq2[:], in1=lng[:], op=ALU.mult),
                 reads=["sq2", "lng"], writes=["sq2"])
            S.op("vector", lambda e: e.tensor_tensor(out=vn[:], in0=sq2[:], in1=lnb[:], op=ALU.add),
                 reads=["sq2", "lnb"], writes=["vn"])
            for g2 in range(8):
                pb = g2 % 4
                for gg in range(2):
                    g = g2 * 2 + gg
                    S.op("tensor", lambda e, g=g, gg=gg, pb=pb: e.matmul(
                        ps[pb][:, gg * 256:(gg + 1) * 256], lhsT=wsT[:, g, :], rhs=vn[:, g * 256:(g + 1) * 256],
                        start=True, stop=True),
                        reads=["wsT", "vn"], writes=[("ps", pb)])
                for gg in range(2):
                    g = g2 * 2 + gg
                    S.op("vector", lambda e, g=g, gg=gg, pb=pb, s=s: e.scalar_tensor_tensor(
                        out=pp[:, g * 256:(g + 1) * 256], in0=ps[pb][:, gg * 256:(gg + 1) * 256], scalar=bsc[:, g:g + 1],
                        in1=ut[:, s, g * 256:(g + 1) * 256], op0=ALU.add, op1=ALU.mult),
                        reads=[("ps", pb), "bsc", ("ut", s)], writes=["pp"])
            for q in range(4):
                pb = 4 + q % 4
                for j in range(8):
                    kc = q * 8 + j
                    S.op("tensor", lambda e, kc=kc, pb=pb, j=j: e.transpose(
                        psb[pb][:, j * 128:(j + 1) * 128], pp[:, kc * 128:(kc + 1) * 128], identb[:]),
                        reads=["pp", "identb"], writes=[("ps", pb)])
                S.op("scalar", lambda e, q=q, pb=pb, s=s: e.copy(
                    out=ppT[:, s, q * 8:(q + 1) * 8, :].rearrange("p a b -> p (a b)"), in_=psb[pb][:, :]),
                    reads=[("ps", pb)], writes=[("ppT", s)])
            S.op("sync", lambda e, tt=tt, s=s: e.dma_start(out=at_d[tt], in_=ppT[:, s]),
                 reads=[("ppT", s)], writes=["dram:at"], dma=True, semkey=("ppT_st", s))
        S.barrier()
        A.reset(m)
        if dbg == "at":
            for tt in range(NT):
                S.op("gpsimd", lambda e, tt=tt: e.dma_start(
                    out=out[tt * 128:(tt + 1) * 128, :].rearrange("p (k t) -> p k t", k=16), in_=at_d[tt, :, 0:16, :]),
                    writes=["dram:out"], dma=True, semkey="cp_out")
            return
        out_proj(at_d, g_w_out[0], g_b_out[0], mod_d[l, 2 * D:3 * D], x_src, x_dst)

    def ssm(sub, l, x_src, x_dst, dbg=None):
        w_in = s_w_in[0]
        m_all = A.mark()
        dt_tok = A.alloc("dt_tok", [128, NT, 128], F32)
        dta_tok = A.alloc("dta_tok", [128, NT, 128], F32)
        dta_hi = A.alloc("dta_hi", [128, NT, 128], F32)
        m = A.mark()
        hT = A.alloc("hT", [128, KC, L], BF16)
        prologue(sub, x_src, hT)
        zt = A.alloc("zt", [128, 4, 512], F32)
        state = {"n": 0}

        def evac_z(tt, nb, pt, pb):
            k = state["n"] % 4
            state["n"] += 1
            S.op("scalar", lambda e, k=k, pt=pt: e.activation(out=zt[:, k, :], in_=pt[:, :], func=AF.Silu),
                 reads=[("ps", pb)], writes=[("zt", k)])
            S.op("sync", lambda e, k=k, tt=tt, nb=nb: e.dma_start(
                out=zs_d[tt * 128:(tt + 1) * 128, nb * 512:(nb + 1) * 512], in_=zt[:, k, :]),
                reads=[("zt", k)], writes=["dram:zs"], dma=True, semkey=("zt_st", k))
        linear_tok(hT, w_in, 4096, evac_z, tag="s1")
        S.barrier()
        if dbg == "s1":
            S.op("sync", lambda e: e.dma_start(out=out, in_=zs_d[:, 0:2048]), writes=["dram:out"], dma=True, semkey="cp_out")
            return
        wp = A.alloc("wp2", [128, 2, KC, 512], BF16)
        cw = A.alloc("cw", [128, 48, 5], F32)
        cb = A.alloc("cb", [128, 48], F32)
        dtb = A.alloc("dtb", [128, 1], F32)
        acol = A.alloc("acol", [128, 1], F32)
        xc = A.alloc("xc", [128, 2, L + 4], F32)
        acc = A.alloc("acc", [128, L], F32)
        xo = A.alloc("xo", [128, L], F32)
        bco = A.alloc("bco", [128, 2, L], BF16)
        xtok = A.alloc("xtok", [128, 2, NT, 128], F32)
        btk = A.alloc("btk", [128, 2, NT, 128], BF16)
        S.op("sync", lambda e: e.dma_start(out=cw[:], in_=s_conv_col), writes=["cw"], dma=True)
        S.op("sync", lambda e: e.dma_start(out=cb[:], in_=s_convb_col), writes=["cb"], dma=True)
        S.op("sync", lambda e: e.dma_start(out=dtb[:], in_=s_dtb_col), writes=["dtb"], dma=True)
        S.op("sync", lambda e: e.dma_start(out=acol[:], in_=s_alog_col), writes=["acol"], dma=True)
        S.op("scalar", lambda e: e.activation(out=acol[:], in_=acol[:], func=AF.Exp), reads=["acol"], writes=["acol"])
        S.op("vector", lambda e: e.tensor_scalar(out=acol[:], in0=acol[:], scalar1=-1.0, scalar2=None, op0=ALU.mult),
             reads=["acol"], writes=["acol"])
        for s_ in range(2):
            S.op("vector", lambda e, s_=s_: e.memset(xc[:, s_, :], 0.0), writes=[("xc", s_)])
        for grp in range(13):
            ws_ = grp % 2
            ncol = 512 if grp < 12 else 128
            c0 = 4096 + grp * 512
            S.op("gpsimd", lambda e, ws_=ws_, c0=c0, ncol=ncol: e.dma_start(
                out=wp[:, ws_, :, 0:ncol], in_=w_in[:, c0:c0 + ncol].rearrange("(k p) n -> p k n", p=128)),
                writes=[("wp2", ws_)], dma=True)
            for j in range(ncol // 128):
                ch = grp * 4 + j
                for tb in range(4):
                    for kc in range(KC):
                        S.op("tensor", lambda e, kc=kc, ws_=ws_, j=j, tb=tb: e.matmul(
                            ps[tb][:, :], lhsT=wp[:, ws_, kc, j * 128:(j + 1) * 128], rhs=hT[:, kc, tb * 512:(tb + 1) * 512],
                            start=(kc == 0), stop=(kc == KC - 1)),
                            reads=[("wp2", ws_)] + [("hT", t) for t in range(tb * 4, tb * 4 + 4)], writes=[("ps", tb)])
                if ch < 48:
                    xs_ = ch % 2
                    for tb in range(4):
                        S.op("scalar", lambda e, tb=tb, xs_=xs_: e.copy(out=xc[:, xs_, 2 + tb * 512:2 + (tb + 1) * 512], in_=ps[tb][:, :]),
                             reads=[("ps", tb)], writes=[("xc", xs_)])
                    S.op("vector", lambda e, ch=ch, xs_=xs_: e.tensor_scalar(
                        out=acc[:], in0=xc[:, xs_, 0:L], scalar1=cw[:, ch, 0:1], scalar2=None, op0=ALU.mult),
                        reads=[("xc", xs_), "cw"], writes=["acc"])
                    for k in range(1, 5):
                        S.op("vector", lambda e, ch=ch, xs_=xs_, k=k: e.scalar_tensor_tensor(
                            out=acc[:], in0=xc[:, xs_, k:k + L], scalar=cw[:, ch, k:k + 1], in1=acc[:], op0=ALU.mult, op1=ALU.add),
                            reads=[("xc", xs_), "cw", "acc"], writes=["acc"])
                    if ch < 32:
                        S.op("scalar", lambda e, ch=ch: e.activation(out=xo[:], in_=acc[:], func=AF.Silu, bias=cb[:, ch:ch + 1]),
                             reads=["acc", "cb"], writes=["xo"])
                        ts_ = ch % 2
                        for q in range(4):
                            pb = 4 + q
                            for jj in range(4):
                                tt = q * 4 + jj
                                S.op("tensor", lambda e, tt=tt, pb=pb, jj=jj: e.transpose(
                                    ps[pb][:, jj * 128:(jj + 1) * 128], xo[:, tt * 128:(tt + 1) * 128], ident),
                                    reads=["xo", "cst"], writes=[("ps", pb)])
                            S.op("vector", lambda e, q=q, pb=pb, ts_=ts_: e.tensor_copy(
                                out=xtok[:, ts_, q * 4:(q + 1) * 4, :].rearrange("p a b -> p (a b)"), in_=ps[pb][:, :]),
                                reads=[("ps", pb)], writes=[("xtok", ts_)])
                        S.op("sync", lambda e, ch=ch, ts_=ts_: e.dma_start(
                            out=xsd[:, ch * 128:(ch + 1) * 128].rearrange("(t p) f -> p t f", p=128), in_=xtok[:, ts_]),
                            reads=[("xtok", ts_)], writes=["dram:xsd"], dma=True, semkey=("xtok_st", ts_))
                    else:
                        bs_ = ch % 2
                        S.op("scalar", lambda e, ch=ch, bs_=bs_: e.activation(out=bco[:, bs_, :], in_=acc[:], func=AF.Silu, bias=cb[:, ch:ch + 1]),
                             reads=["acc", "cb"], writes=[("bco", bs_)])
                        S.op("sync", lambda e, ch=ch, bs_=bs_: e.dma_start(out=bct_d[ch - 32], in_=bco[:, bs_, :]),
                             reads=[("bco", bs_)], writes=["dram:bct"], dma=True, semkey=("bco_st", bs_))
                        if ch < 40:
                            for q in range(2):
                                pb = 4 + q
                                for jj in range(8):
                                    tt = q * 8 + jj
                                    S.op("tensor", lambda e, tt=tt, pb=pb, jj=jj, bs_=bs_: e.transpose(
                                        psb[pb][:, jj * 128:(jj + 1) * 128], bco[:, bs_, tt * 128:(tt + 1) * 128], identb[:]),
                                        reads=[("bco", bs_), "identb"], writes=[("ps", pb)])
                                S.op("vector", lambda e, q=q, pb=pb, bs_=bs_: e.tensor_copy(
                                    out=btk[:, bs_, q * 8:(q + 1) * 8, :].rearrange("p a b -> p (a b)"), in_=psb[pb][:, :]),
                                    reads=[("ps", pb)], writes=[("btk", bs_)])
                            S.op("sync", lambda e, ch=ch, bs_=bs_: e.dma_start(
                                out=btok_d[:, (ch - 32) * 128:(ch - 31) * 128].rearrange("(t p) f -> p t f", p=128), in_=btk[:, bs_]),
                                reads=[("btk", bs_)], writes=["dram:btok"], dma=True, semkey=("btk_st", bs_))
                else:
                    for tb in range(4):
                        S.op("scalar", lambda e, tb=tb: e.activation(out=acc[:, tb * 512:(tb + 1) * 512], in_=ps[tb][:, :], func=AF.Exp, bias=dtb[:, 0:1]),
                             reads=[("ps", tb), "dtb"], writes=["acc"])
                    S.op("scalar", lambda e: e.activation(out=xo[:], in_=acc[:], func=AF.Ln, bias=1.0), reads=["acc"], writes=["xo"])
                    S.op("vector", lambda e: e.tensor_scalar(out=acc[:], in0=xo[:], scalar1=acol[:, 0:1], scalar2=None, op0=ALU.mult),
                         reads=["xo", "acol"], writes=["acc"])
                    for src, dstt, nm in ((xo, dt_tok, "dt_tok"), (acc, dta_tok, "dta_tok")):
                        for q in range(4):
                            pb = 4 + q
                            for jj in range(4):
                                tt = q * 4 + jj
                                S.op("tensor", lambda e, tt=tt, pb=pb, jj=jj, src=src: e.transpose(
                                    ps[pb][:, jj * 128:(jj + 1) * 128], src[:, tt * 128:(tt + 1) * 128], ident),
                                    reads=["xo", "acc", "cst"], writes=[("ps", pb)])
                            S.op("vector", lambda e, q=q, pb=pb, dstt=dstt: e.tensor_copy(
                                out=dstt[:, q * 4:(q + 1) * 4, :].rearrange("p a b -> p (a b)"), in_=ps[pb][:, :]),
                                reads=[("ps", pb)], writes=[nm])
        hbt = A.alloc("hbt", [128, NT, 128], BF16)
        S.op("vector", lambda e: e.tensor_copy(out=hbt[:], in_=dta_tok[:]), reads=["dta_tok"], writes=["hbt"])
        S.op("vector", lambda e: e.tensor_copy(out=dta_hi[:], in_=hbt[:]), reads=["hbt"], writes=["dta_hi"])
        S.op("vector", lambda e: e.tensor_tensor(out=dta_tok[:], in0=dta_tok[:], in1=dta_hi[:], op=ALU.subtract),
             reads=["dta_tok", "dta_hi"], writes=["dta_tok"])
        S.barrier()
        A.reset(m)
        if dbg in ("s2", "s2b"):
            if dbg == "s2":
                S.op("sync", lambda e: e.dma_start(out=out, in_=xsd[:, 0:2048]), writes=["dram:out"], dma=True, semkey="cp_out")
            else:
                S.op("gpsimd", lambda e: e.dma_start(out=out.rearrange("(c p) t -> c p t", p=128), in_=bct_d), writes=["dram:out"], dma=True, semkey="cp_out")
            return
        Et = A.alloc("Et", [128, NT, 128], F32)
        DTEt = A.alloc("DTEt", [128, NT, 128], F32)
        CDt = A.alloc("CDt", [128, NT, 128], F32)
        tmpc = A.alloc("tmpc", [128, 128], F32)
        hlb = A.alloc("hlb", [128, 2, 128], BF16)
        import os
        for tt in range(NT if not os.environ.get("SSM_SKIP_PRE") else 0):
            S.op("vector", lambda e, tt=tt: e.tensor_copy(out=hlb[:, 0, :], in_=dta_hi[:, tt, :]), reads=["dta_hi"], writes=["hlb"])
            S.op("vector", lambda e, tt=tt: e.tensor_copy(out=hlb[:, 1, :], in_=dta_tok[:, tt, :]), reads=["dta_tok"], writes=["hlb"])
            for pi in range(2):
                S.op("tensor", lambda e, pi=pi: e.matmul(ps[0][:, 0:64], lhsT=tri_le_b, rhs=hlb[:, pi, 0:64], start=(pi == 0), stop=(pi == 1)),
                     reads=["hlb", "cstb"], writes=[("ps", 0)])
            for pi in range(2):
                S.op("tensor", lambda e, pi=pi: e.matmul(ps[0][:, 64:128], lhsT=tri_ge_b, rhs=hlb[:, pi, 64:128], start=(pi == 0), stop=(pi == 1)),
                     reads=["hlb", "cstb"], writes=[("ps", 0)])
            for pi in range(2):
                S.op("tensor", lambda e, pi=pi: e.matmul(ps[1][:, 0:128], lhsT=ones_b, rhs=hlb[:, pi, :], start=(pi == 0), stop=(pi == 1)),
                     reads=["hlb", "cstb"], writes=[("ps", 1)])
            PL = int(os.environ.get("PRE_LEVEL", "9"))
            if PL < 2:
                continue
            S.op("scalar", lambda e, tt=tt: e.activation(out=Et[:, tt, :], in_=ps[0][:, 0:128], func=AF.Exp), reads=[("ps", 0)], writes=["Et"])
            S.op("scalar", lambda e, tt=tt: e.activation(out=CDt[:, tt, :], in_=ps[1][:, 0:128], func=AF.Exp), reads=[("ps", 1)], writes=["CDt"])
            for pi in range(2):
                S.op("tensor", lambda e, pi=pi: e.matmul(ps[2][:, 0:64], lhsT=cstb[:, 3, :], rhs=hlb[:, pi, 0:64], start=(pi == 0), stop=(pi == 1)),
                     reads=["hlb", "cstb"], writes=[("ps", 2)])
            for pi in range(2):
                S.op("tensor", lambda e, pi=pi: e.matmul(ps[2][:, 64:128], lhsT=cstb[:, 4, :], rhs=hlb[:, pi, 64:128], start=(pi == 0), stop=(pi == 1)),
                     reads=["hlb", "cstb"], writes=[("ps", 2)])
            S.op("scalar", lambda e, tt=tt: e.activation(out=DTEt[:, tt, :], in_=ps[2][:, 0:128], func=AF.Exp), reads=[("ps", 2)], writes=["DTEt"])
        import os
        STOP = int(os.environ.get("SSM_STOP", "0"))
        if STOP == 1:
            S.barrier(); return
        BT = A.alloc("BT", [128, L], BF16); CT = A.alloc("CT", [128, L], BF16)
        Btok = A.alloc("Btok", [128, NT, 128], BF16)
        xsg = A.alloc("xsg", [128, NT, 512], F32)
        dtx1 = A.alloc("dtx1", [128, NT, 512], BF16)
        dtx = [dtx1, dtx1]
        yacc = A.alloc("yacc_s", [128, NT, 512], F32)
        SM = [A.alloc("SMF", [128, NT, 128], F32), A.alloc("SMB", [128, NT, 128], F32)]
        prevT = A.alloc("prevT", [128, 512], F32); prevB = A.alloc("prevB", [128, 512], BF16)
        LW = A.alloc("LW", [128, 2, 2, 4, 128], BF16)
        EX = A.alloc("EX", [128, 2, 512], F32)
        MT = A.alloc("MT", [128, 2, 4, 128], BF16)
        t1 = A.alloc("t1", [128, 512], F32)
        dtxe = A.alloc("dtxe", [128, 512], BF16)
        zst = A.alloc("zst", [128, 2, 512], F32)
        ut = A.alloc("ut_s", [128, 512], F32); usq = t1
        vb = A.alloc("vb", [128, 512], BF16)
        vT = A.alloc("vT", [128, 2, 4, 128], BF16)
        Dbc = A.alloc("Dbc", [128, 512], F32); ngb = A.alloc("ngb", [128, 512], F32)
        stt = A.alloc("stt", [128, 2, 4], F32)
        nlw = 0
        for g in range(8):
            S.op("sync", lambda e, g=g: e.dma_start(out=BT[:], in_=bct_d[g]), reads=["dram:bct"], writes=["BT"], dma=True)
            S.op("sync", lambda e, g=g: e.dma_start(out=CT[:], in_=bct_d[8 + g]), reads=["dram:bct"], writes=["CT"], dma=True)
            S.op("sync", lambda e, g=g: e.dma_start(out=Btok[:], in_=btok_d[:, g * 128:(g + 1) * 128].rearrange("(t p) n -> p t n", p=128)),
                 reads=["dram:btok"], writes=["Btok"], dma=True)
            S.op("sync", lambda e, g=g: e.dma_start(out=xsg[:], in_=xsd[:, g * 512:(g + 1) * 512].rearrange("(t p) n -> p t n", p=128)),
                 reads=["dram:xsd"], writes=["xsg"], dma=True)
            S.op("sync", lambda e, g=g: e.dma_start(out=Dbc[:], in_=s_d_rep[g * 512:(g + 1) * 512].partition_broadcast(128)), writes=["Dbc"], dma=True)
            S.op("sync", lambda e, g=g: e.dma_start(out=ngb[:], in_=s_norm_g[g * 512:(g + 1) * 512].partition_broadcast(128)), writes=["ngb"], dma=True)
            for c in range(NT):
                S.op("vector", lambda e, c=c: e.tensor_tensor(out=yacc[:, c, :], in0=xsg[:, c, :], in1=Dbc[:], op=ALU.mult),
                     reads=["xsg", "Dbc"], writes=[("yacc_s", c)])
                S.op("tensor", lambda e, c=c: e.matmul(ps[7][:, 0:128], lhsT=BT[:, c * 128:(c + 1) * 128], rhs=CT[:, c * 128:(c + 1) * 128],
                                                        start=True, stop=True), reads=["BT", "CT"], writes=[("ps", 7)])
                S.op("vector", lambda e, c=c: e.tensor_tensor(out=SM[0][:, c, :], in0=ps[7][:, 0:128], in1=tri_le, op=ALU.mult),
                     reads=[("ps", 7), "cst"], writes=[("SM", 0, c)])
                S.op("vector", lambda e, c=c: e.tensor_tensor(out=SM[1][:, c, :], in0=ps[7][:, 0:128], in1=tri_ge, op=ALU.mult),
                     reads=[("ps", 7), "cst"], writes=[("SM", 1, c)])
            if STOP == 2:
                S.barrier(); return
            for d_ in range(2):
                if STOP == 3 and d_ == 1:
                    S.barrier(); return
                order = list(range(NT)) if d_ == 0 else list(range(NT - 1, -1, -1))
                umask = u_gt if d_ == 0 else u_lt
                trimb = tri_le_b if d_ == 0 else tri_ge_b
                hb = d_ * 64 + g * 8
                for c in range(NT):
                    S.op("vector", lambda e, c=c, d_=d_, hb=hb: e.tensor_tensor(
                        out=dtx[d_][:, c, :].rearrange("p (h d) -> p h d", h=8), in0=xsg[:, c, :].rearrange("p (h d) -> p h d", h=8),
                        in1=dt_tok[:, c, hb:hb + 8].unsqueeze(2).to_broadcast([128, 8, 64]), op=ALU.mult),
                        reads=["xsg", "dt_tok"], writes=[("dtx", c)])
                for ci, c in enumerate(order):
                    for hh in range(2):
                        ls = nlw % 2
                        nlw += 1
                        for h in range(4):
                            col = hb + hh * 4 + h
                            for pi, part in enumerate((dta_hi, dta_tok)):
                                S.op("scalar", lambda e, c=c, col=col, ls=ls, h=h, umask=umask, pi=pi, part=part: e.activation(
                                    out=LW[:, ls, pi, h, :], in_=umask, func=AF.Copy, scale=part[:, c, col:col + 1]),
                                    reads=["cst", "dta_tok", "dta_hi"], writes=[("LW", ls)])
                        pseg = 0 + ls
                        for h in range(4):
                            for pi in range(2):
                                S.op("tensor", lambda e, ls=ls, h=h, pseg=pseg, trimb=trimb, pi=pi: e.matmul(
                                    ps[pseg][:, h * 128:(h + 1) * 128], lhsT=LW[:, ls, pi, h, :], rhs=trimb,
                                    start=(pi == 0), stop=(pi == 1)),
                                    reads=[("LW", ls), "cstb"], writes=[("ps", pseg)])
                        S.op("scalar", lambda e, ls=ls, pseg=pseg: e.activation(out=EX[:, ls, :], in_=ps[pseg][:, :], func=AF.Exp),
                             reads=[("ps", pseg)], writes=[("EX", ls)])
                        S.op("vector", lambda e, ls=ls, c=c, d_=d_: e.tensor_tensor(
                            out=MT[:, ls], in0=EX[:, ls, :].rearrange("p (h i) -> p h i", h=4),
                            in1=SM[d_][:, c, :].unsqueeze(1).to_broadcast([128, 4, 128]), op=ALU.mult),
                            reads=[("EX", ls), ("SM", d_, c)], writes=[("MT", ls)])
                        for h in range(4):
                            hd = hh * 4 + h
                            S.op("tensor", lambda e, ls=ls, h=h, hd=hd, c=c, d_=d_: e.matmul(
                                ps[2][:, hd * 64:(hd + 1) * 64], lhsT=MT[:, ls, h, :], rhs=dtx[d_][:, c, hd * 64:(hd + 1) * 64],
                                start=True, stop=True),
                                reads=[("MT", ls), ("dtx", c)], writes=[("ps", 2)])
                    if ci > 0:
                        S.op("tensor", lambda e, c=c: e.matmul(ps[3][:, :], lhsT=CT[:, c * 128:(c + 1) * 128], rhs=prevB[:], start=True, stop=True),
                             reads=["CT", "prevB"], writes=[("ps", 3)])
                        S.op("vector", lambda e, c=c, hb=hb: e.tensor_tensor(
                            out=t1[:].rearrange("p (h d) -> p h d", h=8), in0=ps[3][:, :].rearrange("p (h d) -> p h d", h=8),
                            in1=Et[:, c, hb:hb + 8].unsqueeze(2).to_broadcast([128, 8, 64]), op=ALU.mult),
                            reads=[("ps", 3), "Et"], writes=["t1"])
                        S.op("vector", lambda e, c=c: e.tensor_tensor(out=t1[:], in0=t1[:], in1=yacc[:, c, :], op=ALU.add),
                             reads=["t1", ("yacc_s", c)], writes=["t1"])
                        S.op("vector", lambda e, c=c: e.tensor_tensor(out=yacc[:, c, :], in0=ps[2][:, :], in1=t1[:], op=ALU.add),
                             reads=[("ps", 2), "t1"], writes=[("yacc_s", c)])
                    else:
                        S.op("vector", lambda e, c=c: e.tensor_tensor(out=yacc[:, c, :], in0=ps[2][:, :], in1=yacc[:, c, :], op=ALU.add),
                             reads=[("ps", 2), ("yacc_s", c)], writes=[("yacc_s", c)])
                    if ci < NT - 1:
                        S.op("vector", lambda e, c=c, d_=d_, hb=hb: e.tensor_tensor(
                            out=dtxe[:].rearrange("p (h d) -> p h d", h=8), in0=dtx[d_][:, c, :].rearrange("p (h d) -> p h d", h=8),
                            in1=DTEt[:, c, hb:hb + 8].unsqueeze(2).to_broadcast([128, 8, 64]), op=ALU.mult),
                            reads=[("dtx", c), "DTEt"], writes=["dtxe"])
                        S.op("tensor", lambda e, c=c: e.matmul(ps[6][:, :], lhsT=Btok[:, c, :], rhs=dtxe[:], start=True, stop=True),
                             reads=["Btok", "dtxe"], writes=[("ps", 6)])
                        if ci == 0:
                            S.op("vector", lambda e: e.tensor_copy(out=prevT[:], in_=ps[6][:, :]), reads=[("ps", 6)], writes=["prevT"])
                        else:
                            S.op("vector", lambda e, c=c, hb=hb: e.tensor_tensor(
                                out=prevT[:].rearrange("p (h d) -> p h d", h=8), in0=prevT[:].rearrange("p (h d) -> p h d", h=8),
                                in1=CDt[:, c, hb:hb + 8].unsqueeze(2).to_broadcast([128, 8, 64]), op=ALU.mult),
                                reads=["prevT", "CDt"], writes=["prevT"])
                            S.op("vector", lambda e: e.tensor_tensor(out=prevT[:], in0=prevT[:], in1=ps[6][:, :], op=ALU.add),
                                 reads=["prevT", ("ps", 6)], writes=["prevT"])
                        S.op("scalar", lambda e: e.copy(out=prevB[:], in_=prevT[:]), reads=["prevT"], writes=["prevB"])
            if STOP == 4:
                S.barrier(); return
            for c in range(NT):
                zs_ = c % 2
                S.op("sync", lambda e, c=c, g=g, zs_=zs_: e.dma_start(out=zst[:, zs_, :], in_=zs_d[c * 128:(c + 1) * 128, g * 512:(g + 1) * 512]),
                     reads=["dram:zs"], writes=[("zst", zs_)], dma=True)
                S.op("vector", lambda e, c=c, zs_=zs_: e.tensor_tensor(out=ut[:], in0=yacc[:, c, :], in1=zst[:, zs_, :], op=ALU.mult),
                     reads=[("yacc_s", c), ("zst", zs_)], writes=["ut_s"])
                S.op("vector", lambda e: e.tensor_tensor(out=usq[:], in0=ut[:], in1=ut[:], op=ALU.mult), reads=["ut_s"], writes=["t1"])
                S.op("vector", lambda e, zs_=zs_: e.reduce_sum(out=stt[:, zs_, 0:1], in_=usq[:], axis=AX.X), reads=["t1"], writes=[("stt", zs_)])
                S.op("vector", lambda e, zs_=zs_: e.tensor_scalar(out=stt[:, zs_, 1:2], in0=stt[:, zs_, 0:1], scalar1=1.0 / 512, scalar2=1e-5,
                                                                  op0=ALU.mult, op1=ALU.add), reads=[("stt", zs_)], writes=[("stt", zs_)])
                S.op("scalar", lambda e, zs_=zs_: e.activation(out=stt[:, zs_, 2:3], in_=stt[:, zs_, 1:2], func=AF.Sqrt),
                     reads=[("stt", zs_)], writes=[("stt", zs_)])
                S.op("vector", lambda e, zs_=zs_: e.reciprocal(out=stt[:, zs_, 3:4], in_=stt[:, zs_, 2:3]), reads=[("stt", zs_)], writes=[("stt", zs_)])
                S.op("vector", lambda e, zs_=zs_: e.scalar_tensor_tensor(out=vb[:], in0=ut[:], scalar=stt[:, zs_, 3:4], in1=ngb[:],
                                                                          op0=ALU.mult, op1=ALU.mult),
                     reads=["ut_s", ("stt", zs_), "ngb"], writes=["vb"])
                for j in range(4):
                    S.op("tensor", lambda e, j=j: e.transpose(psb[5][:, j * 128:(j + 1) * 128], vb[:, j * 128:(j + 1) * 128], identb[:]),
                         reads=["vb", "identb"], writes=[("ps", 5)])
                S.op("scalar", lambda e, zs_=zs_: e.copy(out=vT[:, zs_].rearrange("p a b -> p (a b)"), in_=psb[5][:, 0:512]),
                     reads=[("ps", 5)], writes=[("vT", zs_)])
                S.op("sync", lambda e, c=c, g=g, zs_=zs_: e.dma_start(out=at_d[c, :, g * 4:(g + 1) * 4, :], in_=vT[:, zs_]),
                     reads=[("vT", zs_)], writes=["dram:at"], dma=True, semkey=("vT_st", zs_))
        S.barrier()
        A.reset(m_all)
        if dbg == "s3":
            for tt in range(NT):
                S.op("gpsimd", lambda e, tt=tt: e.dma_start(
                    out=out[tt * 128:(tt + 1) * 128, :].rearrange("p (k t) -> p k t", k=16), in_=at_d[tt, :, 0:16, :]),
                    writes=["dram:out"], dma=True, semkey="cp_out")
            return
        out_proj(at_d, s_w_out[0], None, mod_d[l, 2 * D:3 * D], x_src, x_dst)

    def moe(sub, l, x_src, x_dst):
        TBS = 8
        NTOK = TBS * 128
        NBLK = NT // TBS
        m = A.mark()
        hTb = A.alloc("hTb", [128, KC, NTOK], BF16)
        gTh = A.alloc("gTh", [32, NTOK], BF16)
        wrh = A.alloc("wrh", [128, KC, 32], BF16)
        wrl = A.alloc("wrl", [128, KC, 32], BF16)
        brbc = A.alloc("brbc", [128, 32], F32)
        bupe = A.alloc("bupe", [128, 2, 16, 2], F32)
        bdnb = A.alloc("bdnb", [32, D], BF16)
        wup = A.alloc("wup", [128, 2, KC, 256], BF16)
        wdn = A.alloc("wdn", [128, 2, KC, 512], BF16)
        yacc = A.alloc("yacc", [128, TBS, D], F32)
        gb = A.alloc("gb", [128, 2, NTOK], F32)
        MP = A.mark()
        actT = A.alloc("actT", [128, KC, NTOK], BF16)
        tg = A.alloc("tg", [128, 512], F32)
        tsg = A.alloc("tsg", [128, 512], F32)
        tl_ = A.alloc("tl_", [128, 512], F32)
        TOP = A.mark()
        A.reset(MP)
        wr = A.alloc("wr", [128, KC, 32], F32)
        S.op("sync", lambda e: e.dma_start(out=wr[:], in_=m_wr_col[:, l]), writes=["wr0"], dma=True)
        S.op("vector", lambda e: e.tensor_copy(out=wrh[:], in_=wr[:]), reads=["wr0"], writes=["wr"])
        S.op("vector", lambda e: e.tensor_tensor(out=wrl[:], in0=wr[:], in1=wrh[:], op=ALU.subtract), reads=["wr0", "wr"], writes=["wr"])
        S.op("sync", lambda e: e.dma_start(out=brbc[:], in_=m_br[l].partition_broadcast(128)), writes=["brbc"], dma=True)
        S.op("gpsimd", lambda e: e.dma_start(out=bdnb[:], in_=m_b_down[l]), writes=["bdn"], dma=True)
        S.barrier()
        nup = 0
        ndn = 0
        for tb in range(min(NBLK, DBG_TB)):
            A.reset(MP)
            gT = A.alloc("gT", [32, NTOK], F32)
            prologue(sub, x_src, hTb, tiles=[tb * TBS + i for i in range(TBS)], router=(wrh, wrl, brbc, gT))
            S.op("vector", lambda e: e.tensor_copy(out=gTh[:], in_=gT[:]), reads=["gT"], writes=["gTh"])
            S.op("sync", lambda e: e.dma_start(out=gt_d, in_=gT[:]), reads=["gT"], writes=["dram:gt"], dma=True, semkey="gt_st")
            S.barrier()
            A.reset(TOP)
            for tl in range(TBS):
                for db in range(4):
                    pb = 6 + (tl * 4 + db) % 2
                    S.op("tensor", lambda e, tl=tl, db=db, pb=pb: e.matmul(
                        ps[pb][:, :], lhsT=gTh[:, tl * 128:(tl + 1) * 128], rhs=bdnb[:, db * 512:(db + 1) * 512],
                        start=True, stop=True), reads=["gTh", "bdn"], writes=[("ps", pb)])
                    S.op("vector", lambda e, tl=tl, db=db, pb=pb: e.tensor_copy(
                        out=yacc[:, tl, db * 512:(db + 1) * 512], in_=ps[pb][:, :]),
                        reads=[("ps", pb)], writes=[("yacc", tl)])
            for ex in range(DBG_NE):
                bs_ = ex % 2
                S.op("sync", lambda e, ex=ex, bs_=bs_: e.dma_start(out=bupe[:, bs_], in_=m_bup_col[:, l, ex]),
                     writes=[("bupe", bs_)], dma=True)
                S.op("sync", lambda e, ex=ex, bs_=bs_: e.dma_start(out=gb[:, bs_, :], in_=gt_d[ex].partition_broadcast(128)),
                     reads=["dram:gt"], writes=[("gb", bs_)], dma=True)
                for fc in range(KC):
                    us = nup % 2
                    nup += 1
                    S.op("gpsimd", lambda e, ex=ex, fc=fc, us=us: e.dma_start(
                        out=wup[:, us], in_=m_w_up[l][ex, :, fc * 256:(fc + 1) * 256].rearrange("(k p) n -> p k n", p=128)),
                        writes=[("wup", us)], dma=True)
                    for hf_ in range(NTOK // 512):
                        pg = hf_ * 2
                        pl = pg + 1
                        for half, pbank in ((0, pg), (1, pl)):
                            for kc in range(KC):
                                S.op("tensor", lambda e, kc=kc, us=us, half=half, pbank=pbank, hf_=hf_: e.matmul(
                                    ps[pbank][:, :], lhsT=wup[:, us, kc, half::2], rhs=hTb[:, kc, hf_ * 512:(hf_ + 1) * 512],
                                    start=(kc == 0), stop=(kc == KC - 1)),
                                    reads=[("wup", us)] + [("hT", t) for t in range(hf_ * 4, hf_ * 4 + 4)], writes=[("ps", pbank)])
                        S.op("vector", lambda e, bs_=bs_, fc=fc, pg=pg: e.tensor_scalar(
                            out=tg[:], in0=ps[pg][:, :], scalar1=bupe[:, bs_, fc, 0:1], scalar2=7.0, op0=ALU.add, op1=ALU.min),
                            reads=[("ps", pg), ("bupe", bs_)], writes=["tg"])
                        S.op("scalar", lambda e: e.activation(out=tsg[:], in_=tg[:], func=AF.Sigmoid, scale=1.702),
                             reads=["tg"], writes=["tsg"])
                        S.op("vector", lambda e, bs_=bs_, fc=fc, pl=pl: e.tensor_scalar(
                            out=tl_[:], in0=ps[pl][:, :], scalar1=bupe[:, bs_, fc, 1:2], scalar2=7.0, op0=ALU.add, op1=ALU.min),
                            reads=[("ps", pl), ("bupe", bs_)], writes=["tl_"])
                        S.op("vector", lambda e: e.tensor_scalar(
                            out=tl_[:], in0=tl_[:], scalar1=-7.0, scalar2=1.0, op0=ALU.max, op1=ALU.add),
                            reads=["tl_"], writes=["tl_"])
                        S.op("vector", lambda e: e.tensor_tensor(out=tg[:], in0=tg[:], in1=tsg[:], op=ALU.mult),
                             reads=["tg", "tsg"], writes=["tg"])
                        S.op("vector", lambda e, hf_=hf_, bs_=bs_: e.tensor_tensor(
                            out=tl_[:], in0=tl_[:], in1=gb[:, bs_, hf_ * 512:(hf_ + 1) * 512], op=ALU.mult),
                            reads=["tl_", ("gb", bs_)], writes=["tl_"])
                        S.op("vector", lambda e, fc=fc, hf_=hf_: e.tensor_tensor(
                            out=actT[:, fc, hf_ * 512:(hf_ + 1) * 512], in0=tg[:], in1=tl_[:], op=ALU.mult),
                            reads=["tg", "tl_"], writes=[("actT", fc, hf_)])
                for db in range(4):
                    ds_ = ndn % 2
                    ndn += 1
                    S.op("gpsimd", lambda e, ex=ex, db=db, ds_=ds_: e.dma_start(
                        out=wdn[:, ds_], in_=m_w_down[l][ex, :, db * 512:(db + 1) * 512].rearrange("(k p) n -> p k n", p=128)),
                        writes=[("wdn", ds_)], dma=True)
                    for tl in range(TBS):
                        pb = 6 + (db * TBS + tl) % 2
                        for fc in range(KC):
                            S.op("tensor", lambda e, fc=fc, tl=tl, ds_=ds_, pb=pb: e.matmul(
                                ps[pb][:, :], lhsT=actT[:, fc, tl * 128:(tl + 1) * 128], rhs=wdn[:, ds_, fc, :],
                                start=(fc == 0), stop=(fc == KC - 1)),
                                reads=[("actT", fc, tl // 4), ("wdn", ds_)], writes=[("ps", pb)])
                        S.op("vector", lambda e, tl=tl, db=db, pb=pb: e.tensor_tensor(
                            out=yacc[:, tl, db * 512:(db + 1) * 512], in0=yacc[:, tl, db * 512:(db + 1) * 512],
                            in1=ps[pb][:, :], op=ALU.add),
                            reads=[("ps", pb), ("yacc", tl)], writes=[("yacc", tl)])
            S.barrier()
            A.reset(MP)
            g2bc = A.alloc("g2bc", [128, D], F32)
            xe = A.alloc("xe", [128, D], F32)
            S.op("sync", lambda e: e.dma_start(out=g2bc[:], in_=mod_d[l, 5 * D:6 * D].partition_broadcast(128)),
                 writes=["g2bc"], dma=True)
            for tl in range(TBS):
                tt = tb * TBS + tl
                S.op("sync", lambda e, tt=tt: e.dma_start(out=xe[:], in_=x_src[tt * 128:(tt + 1) * 128, :]),
                     writes=["xe"], dma=True)
                S.op("vector", lambda e, tl=tl: e.tensor_tensor(out=yacc[:, tl, :], in0=yacc[:, tl, :], in1=g2bc[:], op=ALU.mult),
                     reads=[("yacc", tl), "g2bc"], writes=[("yacc", tl)])
                S.op("vector", lambda e, tl=tl: e.tensor_tensor(out=xe[:], in0=xe[:], in1=yacc[:, tl, :], op=ALU.add),
                     reads=[("yacc", tl), "xe"], writes=["xe"])
                S.op("sync", lambda e, tt=tt: e.dma_start(out=x_dst[tt * 128:(tt + 1) * 128, :], in_=xe[:]),
                     reads=["xe"], writes=["dram:xdst"], dma=True, semkey="xe_st")
            S.barrier()
        S.barrier()
        A.reset(m)

    cur = x_in
    nxt = 0
    for stage in flow:
        if stage == "final":
            final_norm(cur)
            cur = None
            continue
        dst = xs_d[nxt]
        nxt += 1
        if stage.startswith("gmlp"):
            dbg = stage.split(":")[1] if ":" in stage else None
            gmlp(2, 1, cur, dst, dbg=dbg)
            if dbg:
                cur = None
                break
        elif stage == "moe0":
            moe(1, 0, cur, dst)
        elif stage == "moe1":
            moe(3, 1, cur, dst)
        elif stage.startswith("ssm"):
            dbg = stage.split(":")[1] if ":" in stage else None
            ssm(0, 0, cur, dst, dbg=dbg)
            if dbg:
                cur = None
                break
        cur = dst
        S.barrier()
    if cur is not None:
        S.op("sync", lambda e: e.dma_start(out=out, in_=cur), writes=["dram:out"], dma=True, semkey="cp_out")
    S.barrier()
    S.emit(stack)
    stack.close()
    return nc


def host_inputs(inputs, b, flow=None):
    f = lambda a: np.ascontiguousarray(a, dtype=np.float32)
    i = np.arange(128)
    consts = np.stack([
        np.eye(128),
        (i[:, None] <= i[None, :]), (i[:, None] >= i[None, :]),
        (i[:, None] > i[None, :]), (i[:, None] < i[None, :]),
        np.ones((128, 128)),
    ]).astype(np.float32)
    ng = np.stack([inputs["norm_mix_g"][0], inputs["norm_ffn_g"][0], inputs["norm_mix_g"][1], inputs["norm_ffn_g"][1]])
    m = {
        "x": f(inputs["x"][b]),
        "c_col": f(inputs["c"][b].reshape(KC, 128).T),
        "consts": consts,
        "ada_w": f(inputs["ada_w"]),
        "ada_b": f(inputs["ada_b"]),
        "ng_col": f(ng.reshape(4, KC, 128).transpose(2, 0, 1)),
        "final_g": f(inputs["final_g"]),
        "s_w_in": f(inputs["ssm_w_in"]),
        "s_conv_col": f(inputs["ssm_conv_w"][0].reshape(5, 48, 128).transpose(2, 1, 0)),
        "s_convb_col": f(inputs["ssm_conv_b"][0].reshape(48, 128).T),
        "s_dtb_col": f(inputs["ssm_dt_bias"][0].reshape(128, 1)),
        "s_alog_col": f(inputs["ssm_a_log"][0].reshape(128, 1)),
        "s_d_rep": f(np.repeat(inputs["ssm_d"][0], 64)),
        "s_norm_g": f(inputs["ssm_norm_g"][0]),
        "s_w_out": f(inputs["ssm_w_out"]),
        "g_w_in": f(inputs["gmlp_w_in"]), "g_b_in": f(inputs["gmlp_b_in"]),
        "g_ln_g": f(inputs["gmlp_ln_g"]), "g_ln_b": f(inputs["gmlp_ln_b"]),
        "g_w_s": f(inputs["gmlp_w_s"]), "g_bs_col": f(inputs["gmlp_b_s"][0].T),
        "g_w_out": f(inputs["gmlp_w_out"]), "g_b_out": f(inputs["gmlp_b_out"]),
        "m_wr_col": f(inputs["moe_w_router"].reshape(2, KC, 128, 32).transpose(2, 0, 1, 3)),
        "m_br": f(inputs["moe_b_router"]),
        "m_bup_col": f(inputs["moe_b_up"].reshape(2, 32, 16, 128, 2).transpose(3, 0, 1, 2, 4)),
        "m_b_down": f(inputs["moe_b_down"]),
    }
    if flow is None or any(st.startswith("moe") for st in flow):
        for i in range(2):
            m["m_w_up%d" % i] = f(inputs["moe_w_up"][i, :DBG_NE])
            m["m_w_down%d" % i] = f(inputs["moe_w_down"][i, :DBG_NE])
    return m


def kernel(**inputs):
    n = 8
    nc = build_program()
    in_maps = [host_inputs(inputs, b) for b in range(n)]
    res = run_bass_kernel_spmd(nc, in_maps, core_ids=list(range(n)))
    return np.stack([r["out"] for r in res.results], axis=0)
```

```python
import numpy as np
import concourse.bass as bass
import concourse.mybir as mybir
from concourse.bass_utils import run_bass_kernel_spmd

F32 = mybir.dt.float32
BF16 = mybir.dt.bfloat16
AF = mybir.ActivationFunctionType
ALU = mybir.AluOpType
AX = mybir.AxisListType

D = 2048
L = 2048
NT = 16
KC = 16
D_INNER = 4096
D_SSM_IN = 10368
import os
N_EXP = 32
DBG_NE = int(os.environ.get("MOE_NE", "32"))
DBG_TB = int(os.environ.get("MOE_TB", "4"))
DBG_LVL = int(os.environ.get("MOE_LVL", "9"))
ENGS = ["tensor", "vector", "scalar", "gpsimd", "sync"]


class Sched:
    def __init__(self, nc):
        self.nc = nc
        self.ops = {e: [] for e in ENGS}
        self.track = {}
        self.dma_cnt = {}
        self.last_dma = {}

    def op(self, eng, fn, reads=(), writes=(), dma=False, semkey=None):
        deps = set()
        for r in reads:
            t = self.track.get(r)
            if t and t["w"] is not None:
                deps.add(t["w"])
        for w in writes:
            t = self.track.get(w)
            if t:
                if t["w"] is not None:
                    deps.add(t["w"])
                deps.update(t["r"])
        idx = len(self.ops[eng])
        if dma:
            if semkey is None:
                semkey = writes[0] if (writes and not str(writes[0]).startswith("dram:")) else reads[0]
            cnt = self.dma_cnt.get(semkey, 0) + 1
            self.dma_cnt[semkey] = cnt
            ref = ("dma", semkey, cnt)
        else:
            ref = ("eng", eng, idx)
        if eng == "tensor":
            deps = {d for d in deps if not (d[0] == "eng" and d[1] == "tensor")}
        self.ops[eng].append(dict(fn=fn, deps=deps, ref=ref, dma=dma, needed=False, semkey=semkey))
        for r in reads:
            self.track.setdefault(r, {"w": None, "r": []})["r"].append(ref)
        for w in writes:
            self.track[w] = {"w": ref, "r": []}
        return ref

    def barrier(self):
        deps = set()
        for e in ENGS:
            for i in range(len(self.ops[e]) - 1, -1, -1):
                o = self.ops[e][i]
                if o["fn"] is not None and not o["dma"]:
                    deps.add(o["ref"])
                    break
        for k, c in self.dma_cnt.items():
            deps.add(("dma", k, c))
        for e in ENGS:
            self.ops[e].append(dict(fn=None, deps=set(deps), ref=None, dma=False, needed=False, semkey=None))
        self.track = {}

    def emit(self, stack):
        nc = self.nc
        for e in ENGS:
            for o in self.ops[e]:
                for d in o["deps"]:
                    if d[0] == "eng":
                        self.ops[d[1]][d[2]]["needed"] = True
        cum = {}
        for e in ENGS:
            c = 0
            arr = []
            for o in self.ops[e]:
                if o["needed"]:
                    c += 1
                arr.append(c)
            cum[e] = arr
        esem = {e: stack.enter_context(nc.semaphore("s_" + e)) for e in ENGS}
        dsem = {}
        for k in self.dma_cnt:
            dsem[k] = stack.enter_context(nc.semaphore("d%d" % len(dsem)))
        block = stack.enter_context(nc.Block())

        def make(e):
            def body(eng):
                waited = {}
                for o in self.ops[e]:
                    for d in sorted(o["deps"], key=lambda z: str(z)):
                        if d[0] == "eng":
                            sem, val, key = esem[d[1]], cum[d[1]][d[2]], ("e", d[1])
                        else:
                            sem, val, key = dsem[d[1]], 16 * d[2], ("d", d[1])
                        if waited.get(key, 0) >= val:
                            continue
                        eng.wait_ge(sem, val)
                        waited[key] = val
                    if o["fn"] is None:
                        continue
                    ins = o["fn"](eng)
                    if o["dma"]:
                        ins.then_inc(dsem[o["semkey"]], 16)
                    elif o["needed"]:
                        ins.then_inc(esem[e], 1)
            return body

        for e in ENGS:
            getattr(block, e)(make(e))


class Arena:
    def __init__(self, nc, limit=229376):
        self.nc = nc
        self.off = 18560
        self.limit = limit
        self.n = 0

    def alloc(self, name, shape, dtype):
        esz = 2 if dtype == BF16 else 4
        per_part = int(np.prod(shape[1:])) * esz
        off = (self.off + 63) // 64 * 64
        assert off + per_part <= self.limit, "SBUF arena overflow %s %d" % (name, off + per_part)
        self.n += 1
        t = self.nc.alloc_sbuf_tensor_at("%s_%d" % (name, self.n), list(shape), dtype, offset=off)
        self.off = off + per_part
        return t

    def mark(self):
        return self.off

    def reset(self, m):
        self.off = m


def build_program(flow=("ssm", "moe0", "gmlp", "moe1", "final"), taps=()):
    import contextlib
    nc = bass.Bass("TRN2", target_bir_lowering=False)
    S = Sched(nc)
    A = Arena(nc)
    stack = contextlib.ExitStack()

    def din(name, shape, dt=F32):
        return nc.dram_tensor(name, list(shape), dt, kind="ExternalInput").ap()

    def dscr(name, shape, dt=F32):
        return nc.dram_tensor(name, list(shape), dt, kind="Internal").ap()

    x_in = din("x", [L, D])
    c_col = din("c_col", [128, KC])
    consts = din("consts", [6, 128, 128])
    ada_w = din("ada_w", [2, D, 6 * D])
    ada_b = din("ada_b", [2, 6 * D])
    ng_col = din("ng_col", [128, 4, KC])
    final_g = din("final_g", [D])
    out = nc.dram_tensor("out", [L, D], F32, kind="ExternalOutput").ap()
    tap_aps = {}
    for (tn, tshape) in taps:
        tap_aps[tn] = nc.dram_tensor("tap_" + tn, list(tshape), F32, kind="ExternalOutput").ap()

    g_w_in = din("g_w_in", [1, D, 8192]); g_b_in = din("g_b_in", [1, 8192])
    g_ln_g = din("g_ln_g", [1, 4096]); g_ln_b = din("g_ln_b", [1, 4096])
    g_w_s = din("g_w_s", [1, 16, 128, 128]); g_bs_col = din("g_bs_col", [128, 16])
    g_w_out = din("g_w_out", [1, 4096, D]); g_b_out = din("g_b_out", [1, D])
    m_wr_col = din("m_wr_col", [128, 2, KC, 32]); m_br = din("m_br", [2, 32])
    has_moe = any(st.startswith("moe") for st in flow)
    m_bup_col = din("m_bup_col", [128, 2, 32, 16, 2]); m_b_down = din("m_b_down", [2, 32, D])
    if has_moe:
        m_w_up = [din("m_w_up%d" % i, [DBG_NE, D, 4096]) for i in range(2)]
        m_w_down = [din("m_w_down%d" % i, [DBG_NE, D, D]) for i in range(2)]
    s_w_in = din("s_w_in", [1, D, D_SSM_IN]); s_conv_col = din("s_conv_col", [128, 48, 5])
    s_convb_col = din("s_convb_col", [128, 48]); s_dtb_col = din("s_dtb_col", [128, 1]); s_alog_col = din("s_alog_col", [128, 1])
    s_d_rep = din("s_d_rep", [4096]); s_norm_g = din("s_norm_g", [4096]); s_w_out = din("s_w_out", [1, 4096, D])
    zs_d = dscr("zs_d", [L, 4096]); xsd = dscr("xsd", [L, 4096])
    bct_d = dscr("bct_d", [16, 128, L], BF16); btok_d = dscr("btok_d", [L, 1024], BF16)
    uv_d = dscr("uv_d", [L, 8192])
    gt_d = dscr("gt_d", [32, 1024])
    at_d = dscr("at_d", [NT, 128, 32, 128], BF16)
    mod_d = dscr("mod_d", [2, 6 * D])
    xs_d = [dscr("xs%d" % i, [L, D]) for i in range(4)]

    cst = A.alloc("cst", [128, 6, 128], F32)
    ident, tri_le, tri_ge, u_gt, u_lt, ones = [cst[:, i, :] for i in range(6)]
    identb = A.alloc("identb", [128, 128], BF16)
    cstb = A.alloc("cstb", [128, 6, 128], BF16)
    tri_le_b, tri_ge_b, ones_b = cstb[:, 1, :], cstb[:, 2, :], cstb[:, 5, :]
    modcol = A.alloc("modcol", [128, 2, 96], F32)
    ngc = A.alloc("ngc", [128, 4, KC], F32)
    gs_col = A.alloc("gs_col", [128, 4, 2, KC], F32)
    ps = [stack.enter_context(nc.psum_tensor("ps%d" % i, [128, 512], F32)) for i in range(8)]

    S.op("sync", lambda e: e.dma_start(out=cst[:], in_=consts.rearrange("c p n -> p c n")),
         writes=["cst"], dma=True)
    S.op("sync", lambda e: e.dma_start(out=ngc[:], in_=ng_col), writes=["ngc"], dma=True)
    S.op("vector", lambda e: e.tensor_copy(out=identb[:], in_=ident), reads=["cst"], writes=["identb"])
    S.op("vector", lambda e: e.tensor_copy(out=cstb[:], in_=cst[:]), reads=["cst"], writes=["cstb"])

    m0 = A.mark()
    ccol = A.alloc("ccol", [128, KC], F32)
    cact = A.alloc("cact", [128, KC], BF16)
    wa = A.alloc("wa", [128, 2, KC, 512], BF16)
    abrow = A.alloc("abrow", [1, 2, 512], F32)
    mrow = A.alloc("mrow", [1, 2, 512], F32)
    S.op("sync", lambda e: e.dma_start(out=ccol[:], in_=c_col), writes=["ccol"], dma=True)
    S.op("scalar", lambda e: e.activation(out=cact[:], in_=ccol[:], func=AF.Silu), reads=["ccol"], writes=["cact"])
    blk = 0
    for l in range(2):
        for nb in range(24):
            s = blk % 2
            S.op("gpsimd", lambda e, l=l, nb=nb, s=s: e.dma_start(
                out=wa[:, s], in_=ada_w[l, :, nb * 512:(nb + 1) * 512].rearrange("(k p) n -> p k n", p=128)),
                writes=[("wa", s)], dma=True)
            S.op("sync", lambda e, l=l, nb=nb, s=s: e.dma_start(
                out=abrow[:, s, :], in_=ada_b[l:l + 1, nb * 512:(nb + 1) * 512]),
                writes=[("abrow", s)], dma=True)
            pb = blk % 2
            for kc in range(KC):
                S.op("tensor", lambda e, kc=kc, s=s, pb=pb: e.matmul(
                    ps[pb][0:1, :], lhsT=cact[:, kc:kc + 1], rhs=wa[:, s, kc, :],
                    start=(kc == 0), stop=(kc == KC - 1)),
                    reads=["cact", ("wa", s)], writes=[("ps", pb)])
            S.op("vector", lambda e, s=s, pb=pb: e.tensor_tensor(
                out=mrow[:, s, :], in0=ps[pb][0:1, :], in1=abrow[:, s, :], op=ALU.add),
                reads=[("ps", pb), ("abrow", s)], writes=[("mrow", s)])
            S.op("sync", lambda e, l=l, nb=nb, s=s: e.dma_start(
                out=mod_d[l:l + 1, nb * 512:(nb + 1) * 512], in_=mrow[:, s, :]),
                reads=[("mrow", s)], writes=["dram:mod"], dma=True, semkey=("mrow_st", s))
            blk += 1
    mrows = A.alloc("mrows", [96, 128], F32)
    for l in range(2):
        S.op("sync", lambda e, l=l: e.dma_start(out=mrows[:], in_=mod_d[l].rearrange("(j p) -> j p", p=128)),
             reads=["dram:mod"], writes=["mrows"], dma=True)
        S.op("tensor", lambda e: e.transpose(ps[2][:, 0:96], mrows[:], ident[0:96, 0:96]),
             reads=["mrows", "cst"], writes=[("ps", 2)])
        S.op("vector", lambda e, l=l: e.tensor_copy(out=modcol[:, l, :], in_=ps[2][:, 0:96]),
             reads=[("ps", 2)], writes=["modcol"])
    for l in range(2):
        for half in range(2):
            sub = 2 * l + half
            base = 48 * half
            S.op("vector", lambda e, l=l, sub=sub, base=base: e.scalar_tensor_tensor(
                out=gs_col[:, sub, 0, :], in0=modcol[:, l, base + 16:base + 32], scalar=1.0,
                in1=ngc[:, sub, :], op0=ALU.add, op1=ALU.mult),
                reads=["modcol", "ngc"], writes=["gs_col"])
            S.op("vector", lambda e, l=l, sub=sub, base=base: e.tensor_copy(
                out=gs_col[:, sub, 1, :], in_=modcol[:, l, base:base + 16]),
                reads=["modcol"], writes=["gs_col"])
    S.barrier()
    A.reset(m0)
    if "modcol" in tap_aps:
        S.op("sync", lambda e: e.dma_start(out=tap_aps["modcol"], in_=modcol[:].rearrange("p l j -> p (l j)")),
             reads=["modcol"], writes=["dram:tap1"], dma=True, semkey="tap1")

    def prologue(sub, x_src, hT, tiles=None, router=None):
        m = A.mark()
        tiles = list(range(NT)) if tiles is None else tiles
        t0 = tiles[0]
        if router is not None:
            wrh, wrl, brbc, gT = router
            hf = A.alloc("hf", [128, 2, 4, 128], F32)
            hfh = A.alloc("hfh", [128, 2, 4, 128], BF16)
            hfl = A.alloc("hfl", [128, 2, 4, 128], BF16)
            rg = A.alloc("rg", [128, 2, 160], F32)
        xt = A.alloc("xt", [128, 2, D], F32)
        st = A.alloc("st", [128, 2, 4], F32)
        xn1 = A.alloc("xn", [128, D], F32)
        sq = xn1
        for tt in tiles:
            s = tt % 2
            tl = tt - t0
            S.op("sync", lambda e, tt=tt, s=s: e.dma_start(out=xt[:, s, :], in_=x_src[tt * 128:(tt + 1) * 128, :]),
                 writes=[("xt", s)], dma=True)
            S.op("vector", lambda e, s=s: e.tensor_tensor(out=sq[:], in0=xt[:, s, :], in1=xt[:, s, :], op=ALU.mult),
                 reads=[("xt", s)], writes=["xn"])
            S.op("vector", lambda e, s=s: e.reduce_sum(out=st[:, s, 0:1], in_=sq[:], axis=AX.X),
                 reads=["xn"], writes=[("st", s)])
            S.op("vector", lambda e, s=s: e.tensor_scalar(out=st[:, s, 1:2], in0=st[:, s, 0:1], scalar1=1.0 / D,
                                                          scalar2=1e-6, op0=ALU.mult, op1=ALU.add),
                 reads=[("st", s)], writes=[("st", s)])
            S.op("scalar", lambda e, s=s: e.activation(out=st[:, s, 3:4], in_=st[:, s, 1:2], func=AF.Sqrt),
                 reads=[("st", s)], writes=[("st", s)])
            S.op("vector", lambda e, s=s: e.reciprocal(out=st[:, s, 2:3], in_=st[:, s, 3:4]),
                 reads=[("st", s)], writes=[("st", s)])
            S.op("scalar", lambda e, s=s: e.activation(out=xn1[:], in_=xt[:, s, :], func=AF.Copy,
                                                       scale=st[:, s, 2:3]),
                 reads=[("xt", s), ("st", s)], writes=["xn"])
            for q in range(4):
                pb = (tt * 4 + q) % 4
                for j in range(4):
                    kc = q * 4 + j
                    S.op("tensor", lambda e, s=s, kc=kc, pb=pb, j=j: e.transpose(
                        ps[pb][:, j * 128:(j + 1) * 128], xn1[:, kc * 128:(kc + 1) * 128], ident),
                        reads=["xn", "cst"], writes=[("ps", pb)])
                for j in range(4):
                    kc = q * 4 + j
                    S.op("scalar", lambda e, kc=kc, pb=pb, j=j, tl=tl: e.activation(
                        out=hT[:, kc, tl * 128:(tl + 1) * 128], in_=ps[pb][:, j * 128:(j + 1) * 128],
                        func=AF.Identity, scale=gs_col[:, sub, 0, kc:kc + 1], bias=gs_col[:, sub, 1, kc:kc + 1]),
                        reads=[("ps", pb), "gs_col"], writes=[("hT", tl)])
                if router is not None:
                    hs = q % 2
                    for j in range(4):
                        kc = q * 4 + j
                        S.op("vector", lambda e, kc=kc, pb=pb, j=j, hs=hs: e.tensor_scalar(
                            out=hf[:, hs, j, :], in0=ps[pb][:, j * 128:(j + 1) * 128],
                            scalar1=gs_col[:, sub, 0, kc:kc + 1], scalar2=gs_col[:, sub, 1, kc:kc + 1],
                            op0=ALU.mult, op1=ALU.add),
                            reads=["gs_col"], writes=[("hf", hs), ("ps", pb)])
                    S.op("vector", lambda e, hs=hs: e.tensor_copy(out=hfh[:, hs], in_=hf[:, hs]),
                         reads=[("hf", hs)], writes=[("hfh", hs)])
                    S.op("vector", lambda e, hs=hs: e.tensor_tensor(out=hfl[:, hs], in0=hf[:, hs], in1=hfh[:, hs], op=ALU.subtract),
                         reads=[("hf", hs), ("hfh", hs)], writes=[("hfl", hs)])
                    for j in range(4):
                        kc = q * 4 + j
                        for pi, (a_, b_) in enumerate(((hfh, wrh), (hfh, wrl), (hfl, wrh))):
                            S.op("tensor", lambda e, kc=kc, j=j, hs=hs, a_=a_, b_=b_, pi=pi: e.matmul(
                                ps[5][:, 0:32], lhsT=a_[:, hs, j, :], rhs=b_[:, kc, :],
                                start=(kc == 0 and pi == 0), stop=(kc == KC - 1 and pi == 2)),
                                reads=[("hfh", hs), ("hfl", hs), "wr"], writes=[("ps", 5)])
            if router is not None and DBG_LVL >= 0:
                r = tt % 2
                lg, mx8, sel, nm, ex, ssum = (rg[:, r, 0:32], rg[:, r, 32:40], rg[:, r, 40:72], rg[:, r, 72:73],
                                              rg[:, r, 80:112], rg[:, r, 112:114])
                gts = rg[:, r, 120:152]
                K_ = ("rg", r)
                S.op("vector", lambda e, lg=lg: e.tensor_tensor(out=lg, in0=ps[5][:, 0:32], in1=brbc[:], op=ALU.add),
                     reads=[("ps", 5), "brbc"], writes=[K_])
                S.op("vector", lambda e, lg=lg, mx8=mx8: e.max(out=mx8, in_=lg), reads=[K_], writes=[K_])
                S.op("vector", lambda e, lg=lg, mx8=mx8, sel=sel: e.tensor_scalar(
                    out=sel, in0=lg, scalar1=mx8[:, 3:4], scalar2=None, op0=ALU.is_ge), reads=[K_], writes=[K_])
                S.op("vector", lambda e, mx8=mx8, nm=nm: e.tensor_scalar(
                    out=nm, in0=mx8[:, 0:1], scalar1=-1.0, scalar2=None, op0=ALU.mult), reads=[K_], writes=[K_])
                S.op("scalar", lambda e, lg=lg, nm=nm, ex=ex: e.activation(out=ex, in_=lg, func=AF.Exp, bias=nm),
                     reads=[K_], writes=[K_])
                S.op("vector", lambda e, ex=ex, sel=sel: e.tensor_tensor(out=ex, in0=ex, in1=sel, op=ALU.mult),
                     reads=[K_], writes=[K_])
                S.op("vector", lambda e, ex=ex, ssum=ssum: e.reduce_sum(out=ssum[:, 0:1], in_=ex, axis=AX.X),
                     reads=[K_], writes=[K_])
                S.op("vector", lambda e, ssum=ssum: e.reciprocal(out=ssum[:, 1:2], in_=ssum[:, 0:1]),
                     reads=[K_], writes=[K_])
                S.op("vector", lambda e, ex=ex, ssum=ssum, gts=gts: e.tensor_scalar(
                    out=gts, in0=ex, scalar1=ssum[:, 1:2], scalar2=None, op0=ALU.mult), reads=[K_], writes=[K_])
                S.op("tensor", lambda e, gts=gts: e.transpose(ps[6][0:32, 0:128], gts, ident),
                     reads=[K_, "cst"], writes=[("ps", 6)])
                S.op("vector", lambda e, tl=tl: e.tensor_copy(out=gT[:, tl * 128:(tl + 1) * 128], in_=ps[6][0:32, 0:128]),
                     reads=[("ps", 6)], writes=["gT"])
        S.barrier()
        A.reset(m)

    def final_norm(x_src):
        m = A.mark()
        xt = A.alloc("xt", [128, 2, D], F32)
        sq = A.alloc("sq", [128, D], F32)
        st = A.alloc("st", [128, 2, 4], F32)
        fg = A.alloc("fg", [128, D], F32)
        yo = A.alloc("yo", [128, 2, D], F32)
        S.op("sync", lambda e: e.dma_start(out=fg[:], in_=final_g.partition_broadcast(128)), writes=["fg"], dma=True)
        for tt in range(NT):
            s = tt % 2
            S.op("sync", lambda e, tt=tt, s=s: e.dma_start(out=xt[:, s, :], in_=x_src[tt * 128:(tt + 1) * 128, :]),
                 writes=[("xt", s)], dma=True)
            S.op("vector", lambda e, s=s: e.tensor_tensor(out=sq[:], in0=xt[:, s, :], in1=xt[:, s, :], op=ALU.mult),
                 reads=[("xt", s)], writes=["sq"])
            S.op("vector", lambda e, s=s: e.reduce_sum(out=st[:, s, 0:1], in_=sq[:], axis=AX.X),
                 reads=["sq"], writes=[("st", s)])
            S.op("vector", lambda e, s=s: e.tensor_scalar(out=st[:, s, 1:2], in0=st[:, s, 0:1], scalar1=1.0 / D,
                                                          scalar2=1e-6, op0=ALU.mult, op1=ALU.add),
                 reads=[("st", s)], writes=[("st", s)])
            S.op("scalar", lambda e, s=s: e.activation(out=st[:, s, 3:4], in_=st[:, s, 1:2], func=AF.Sqrt),
                 reads=[("st", s)], writes=[("st", s)])
            S.op("vector", lambda e, s=s: e.reciprocal(out=st[:, s, 2:3], in_=st[:, s, 3:4]),
                 reads=[("st", s)], writes=[("st", s)])
            S.op("vector", lambda e, s=s: e.scalar_tensor_tensor(
                out=yo[:, s, :], in0=xt[:, s, :], scalar=st[:, s, 2:3], in1=fg[:], op0=ALU.mult, op1=ALU.mult),
                reads=[("xt", s), ("st", s), "fg"], writes=[("yo", s)])
            S.op("sync", lambda e, tt=tt, s=s: e.dma_start(out=out[tt * 128:(tt + 1) * 128, :], in_=yo[:, s, :]),
                 reads=[("yo", s)], writes=["dram:out"], dma=True, semkey=("yo_st", s))
        A.reset(m)

    def linear_tok(hT, w_ap, ncols, evac, kcn=KC, tag="lt"):
        m = A.mark()
        wp = A.alloc("wp_" + tag, [128, 2, kcn, 512], BF16)
        cnt = 0
        for nb in range(ncols // 512):
            s = nb % 2
            S.op("gpsimd", lambda e, nb=nb, s=s: e.dma_start(
                out=wp[:, s], in_=w_ap[:, nb * 512:(nb + 1) * 512].rearrange("(k p) n -> p k n", p=128)),
                writes=[("wp", s)], dma=True)
            for tt in range(NT):
                pb = cnt % 4
                cnt += 1
                for kc in range(kcn):
                    S.op("tensor", lambda e, kc=kc, s=s, pb=pb, tt=tt: e.matmul(
                        ps[pb][:, :], lhsT=hT[:, kc, tt * 128:(tt + 1) * 128], rhs=wp[:, s, kc, :],
                        start=(kc == 0), stop=(kc == kcn - 1)),
                        reads=[("hT", tt), ("wp", s)], writes=[("ps", pb)])
                evac(tt, nb, ps[pb], pb)
        A.reset(m)

    def out_proj(at_d, w_ap, bias_ap, gate_row_ap, x_src, x_dst):
        m = A.mark()
        at = A.alloc("at", [128, 8, 32, 128], BF16)
        wp = A.alloc("wpo", [128, 2, 32, 512], BF16)
        gbc = A.alloc("gbc", [128, D], F32)
        bbc = A.alloc("bbc", [128, D], F32)
        xt = A.alloc("xto", [128, 2, 512], F32)
        yt = A.alloc("yto", [128, 2, 512], F32)
        S.op("sync", lambda e: e.dma_start(out=gbc[:], in_=gate_row_ap.partition_broadcast(128)), writes=["gbc"], dma=True)
        if bias_ap is not None:
            S.op("sync", lambda e: e.dma_start(out=bbc[:], in_=bias_ap.partition_broadcast(128)), writes=["bbc"], dma=True)
        cnt = 0
        for th in range(2):
            for t8 in range(8):
                S.op("sync", lambda e, th=th, t8=t8: e.dma_start(out=at[:, t8], in_=at_d[th * 8 + t8]),
                     writes=[("at", t8)], dma=True)
            for db in range(4):
                s = (th * 4 + db) % 2
                S.op("gpsimd", lambda e, db=db, s=s: e.dma_start(
                    out=wp[:, s], in_=w_ap[:, db * 512:(db + 1) * 512].rearrange("(k p) n -> p k n", p=128)),
                    writes=[("wpo", s)], dma=True)
                for t8 in range(8):
                    tt = th * 8 + t8
                    pb = cnt % 4
                    xs_ = cnt % 2
                    cnt += 1
                    S.op("sync", lambda e, tt=tt, db=db, xs_=xs_: e.dma_start(
                        out=xt[:, xs_, :], in_=x_src[tt * 128:(tt + 1) * 128, db * 512:(db + 1) * 512]),
                        writes=[("xto", xs_)], dma=True)
                    for kc in range(32):
                        S.op("tensor", lambda e, kc=kc, s=s, pb=pb, t8=t8: e.matmul(
                            ps[pb][:, :], lhsT=at[:, t8, kc, :], rhs=wp[:, s, kc, :],
                            start=(kc == 0), stop=(kc == 31)),
                            reads=[("at", t8), ("wpo", s)], writes=[("ps", pb)])
                    if bias_ap is not None:
                        S.op("vector", lambda e, pb=pb, xs_=xs_, db=db: e.tensor_tensor(
                            out=yt[:, xs_, :], in0=ps[pb][:, :], in1=bbc[:, db * 512:(db + 1) * 512], op=ALU.add),
                            reads=[("ps", pb), "bbc"], writes=[("yto", xs_)])
                        S.op("vector", lambda e, xs_=xs_, db=db: e.tensor_tensor(
                            out=yt[:, xs_, :], in0=yt[:, xs_, :], in1=gbc[:, db * 512:(db + 1) * 512], op=ALU.mult),
                            reads=[("yto", xs_), "gbc"], writes=[("yto", xs_)])
                    else:
                        S.op("vector", lambda e, pb=pb, xs_=xs_, db=db: e.tensor_tensor(
                            out=yt[:, xs_, :], in0=ps[pb][:, :], in1=gbc[:, db * 512:(db + 1) * 512], op=ALU.mult),
                            reads=[("ps", pb), "gbc"], writes=[("yto", xs_)])
                    S.op("vector", lambda e, xs_=xs_: e.tensor_tensor(
                        out=yt[:, xs_, :], in0=yt[:, xs_, :], in1=xt[:, xs_, :], op=ALU.add),
                        reads=[("yto", xs_), ("xto", xs_)], writes=[("yto", xs_)])
                    S.op("sync", lambda e, tt=tt, db=db, xs_=xs_: e.dma_start(
                        out=x_dst[tt * 128:(tt + 1) * 128, db * 512:(db + 1) * 512], in_=yt[:, xs_, :]),
                        reads=[("yto", xs_)], writes=["dram:xdst"], dma=True, semkey=("yto_st", xs_))
        S.barrier()
        A.reset(m)

    psb = [p[:].bitcast(BF16) for p in ps]

    def gmlp(sub, l, x_src, x_dst, dbg=None):
        w_in = g_w_in[0]
        m = A.mark()
        hT = A.alloc("hT", [128, KC, L], BF16)
        prologue(sub, x_src, hT)
        bb = A.alloc("bb", [128, 2, 512], F32)
        uvt = A.alloc("uvt", [128, 4, 512], F32)
        state = {"n": 0}

        def evac(tt, nb, pt, pb):
            k = state["n"] % 4
            state["n"] += 1
            bs_ = nb % 2
            if tt == 0:
                S.op("sync", lambda e, nb=nb, bs_=bs_: e.dma_start(
                    out=bb[:, bs_, :], in_=g_b_in[0, nb * 512:(nb + 1) * 512].partition_broadcast(128)),
                    writes=[("bb", bs_)], dma=True)
            S.op("vector", lambda e, k=k, bs_=bs_, pt=pt: e.tensor_tensor(
                out=uvt[:, k, :], in0=pt[:, :], in1=bb[:, bs_, :], op=ALU.add),
                reads=[("ps", pb), ("bb", bs_)], writes=[("uvt", k)])
            S.op("scalar", lambda e, k=k: e.activation(out=uvt[:, k, :], in_=uvt[:, k, :], func=AF.Gelu),
                 reads=[("uvt", k)], writes=[("uvt", k)])
            S.op("sync", lambda e, k=k, tt=tt, nb=nb: e.dma_start(
                out=uv_d[tt * 128:(tt + 1) * 128, nb * 512:(nb + 1) * 512], in_=uvt[:, k, :]),
                reads=[("uvt", k)], writes=["dram:uv"], dma=True, semkey=("uvt_st", k))
        linear_tok(hT, w_in, 8192, evac, tag="g1")
        S.barrier()
        A.reset(m)
        if dbg in ("uv", "uv2"):
            c0 = 0 if dbg == "uv" else 4096
            S.op("sync", lambda e: e.dma_start(out=out, in_=uv_d[:, c0:c0 + 2048]), writes=["dram:out"], dma=True, semkey="cp_out")
            return
        m = A.mark()
        lng = A.alloc("lng", [128, 4096], F32)
        lnb = A.alloc("lnb", [128, 4096], F32)
        wsT = A.alloc("wsT", [128, 16, 128], BF16)
        wsf = A.alloc("wsf", [128, 128], F32)
        bsc = A.alloc("bsc", [128, 16], F32)
        ut = A.alloc("ut", [128, 2, 4096], F32)
        vt = A.alloc("vt", [128, 2, 4096], F32)
        sq2 = A.alloc("sq2", [128, 4096], F32)
        vn = A.alloc("vn", [128, 4096], BF16)
        pp = A.alloc("pp", [128, 4096], BF16)
        ppT = A.alloc("ppT", [128, 2, 32, 128], BF16)
        st = A.alloc("st2", [128, 2, 8], F32)
        S.op("sync", lambda e: e.dma_start(out=lng[:], in_=g_ln_g[0].partition_broadcast(128)), writes=["lng"], dma=True)
        S.op("sync", lambda e: e.dma_start(out=lnb[:], in_=g_ln_b[0].partition_broadcast(128)), writes=["lnb"], dma=True)
        S.op("sync", lambda e: e.dma_start(out=bsc[:], in_=g_bs_col), writes=["bsc"], dma=True)
        for g in range(16):
            S.op("sync", lambda e, g=g: e.dma_start(out=wsf[:], in_=g_w_s[0, g]), writes=["wsf"], dma=True)
            S.op("tensor", lambda e: e.transpose(ps[4][:, 0:128], wsf[:], ident), reads=["wsf", "cst"], writes=[("ps", 4)])
            S.op("vector", lambda e, g=g: e.tensor_copy(out=wsT[:, g, :], in_=ps[4][:, 0:128]),
                 reads=[("ps", 4)], writes=["wsT"])
        for tt in range(NT):
            s = tt % 2
            S.op("sync", lambda e, tt=tt, s=s: e.dma_start(out=ut[:, s, :], in_=uv_d[tt * 128:(tt + 1) * 128, 0:4096]),
                 reads=["dram:uv"], writes=[("ut", s)], dma=True)
            S.op("sync", lambda e, tt=tt, s=s: e.dma_start(out=vt[:, s, :], in_=uv_d[tt * 128:(tt + 1) * 128, 4096:8192]),
                 reads=["dram:uv"], writes=[("vt", s)], dma=True)
            S.op("vector", lambda e, s=s: e.reduce_sum(out=st[:, s, 0:1], in_=vt[:, s, :], axis=AX.X),
                 reads=[("vt", s)], writes=[("st2", s)])
            S.op("vector", lambda e, s=s: e.tensor_tensor(out=sq2[:], in0=vt[:, s, :], in1=vt[:, s, :], op=ALU.mult),
                 reads=[("vt", s)], writes=["sq2"])
            S.op("vector", lambda e, s=s: e.reduce_sum(out=st[:, s, 1:2], in_=sq2[:], axis=AX.X),
                 reads=["sq2"], writes=[("st2", s)])
            S.op("vector", lambda e, s=s: e.tensor_scalar(out=st[:, s, 2:4], in0=st[:, s, 0:2], scalar1=1.0 / 4096,
                                                          scalar2=None, op0=ALU.mult),
                 reads=[("st2", s)], writes=[("st2", s)])
            S.op("vector", lambda e, s=s: e.tensor_tensor(out=st[:, s, 4:5], in0=st[:, s, 2:3], in1=st[:, s, 2:3], op=ALU.mult),
                 reads=[("st2", s)], writes=[("st2", s)])
            S.op("vector", lambda e, s=s: e.tensor_tensor(out=st[:, s, 5:6], in0=st[:, s, 3:4], in1=st[:, s, 4:5], op=ALU.subtract),
                 reads=[("st2", s)], writes=[("st2", s)])
            S.op("scalar", lambda e, s=s: e.activation(out=st[:, s, 6:7], in_=st[:, s, 5:6], func=AF.Sqrt, bias=1e-6),
                 reads=[("st2", s)], writes=[("st2", s)])
            S.op("vector", lambda e, s=s: e.reciprocal(out=st[:, s, 7:8], in_=st[:, s, 6:7]),
                 reads=[("st2", s)], writes=[("st2", s)])
            S.op("vector", lambda e, s=s: e.tensor_scalar(out=sq2[:], in0=vt[:, s, :], scalar1=st[:, s, 2:3],
                                                          scalar2=st[:, s, 7:8], op0=ALU.subtract, op1=ALU.mult),
                 reads=[("vt", s), ("st2", s)], writes=["sq2"])
            S.op("vector", lambda e: e.tensor_tensor(out=sq2[:], in0=sq2[:], in1=lng[:], op=ALU.mult),
                 reads=["sq2", "lng"], writes=["sq2"])
            S.op("vector", lambda e: e.tensor_tensor(out=vn[:], in0=sq2[:], in1=lnb[:], op=ALU.add),
                 reads=["sq2", "lnb"], writes=["vn"])
            for g2 in range(8):
                pb = g2 % 4
                for gg in range(2):
                    g = g2 * 2 + gg
                    S.op("tensor", lambda e, g=g, gg=gg, pb=pb: e.matmul(
                        ps[pb][:, gg * 256:(gg + 1) * 256], lhsT=wsT[:, g, :], rhs=vn[:, g * 256:(g + 1) * 256],
                        start=True, stop=True),
                        reads=["wsT", "vn"], writes=[("ps", pb)])
                for gg in range(2):
                    g = g2 * 2 + gg
                    S.op("vector", lambda e, g=g, gg=gg, pb=pb, s=s: e.scalar_tensor_tensor(
                        out=pp[:, g * 256:(g + 1) * 256], in0=ps[pb][:, gg * 256:(gg + 1) * 256], scalar=bsc[:, g:g + 1],
                        in1=ut[:, s, g * 256:(g + 1) * 256], op0=ALU.add, op1=ALU.mult),
                        reads=[("ps", pb), "bsc", ("ut", s)], writes=["pp"])
            for q in range(4):
                pb = 4 + q % 4
                for j in range(8):
                    kc = q * 8 + j
                    S.op("tensor", lambda e, kc=kc, pb=pb, j=j: e.transpose(
                        psb[pb][:, j * 128:(j + 1) * 128], pp[:, kc * 128:(kc + 1) * 128], identb[:]),
                        reads=["pp", "identb"], writes=[("ps", pb)])
                S.op("scalar", lambda e, q=q, pb=pb, s=s: e.copy(
                    out=ppT[:, s, q * 8:(q + 1) * 8, :].rearrange("p a b -> p (a b)"), in_=psb[pb][:, :]),
                    reads=[("ps", pb)], writes=[("ppT", s)])
            S.op("sync", lambda e, tt=tt, s=s: e.dma_start(out=at_d[tt], in_=ppT[:, s]),
                 reads=[("ppT", s)], writes=["dram:at"], dma=True, semkey=("ppT_st", s))
        S.barrier()
        A.reset(m)
        if dbg == "at":
            for tt in range(NT):
                S.op("gpsimd", lambda e, tt=tt: e.dma_start(
                    out=out[tt * 128:(tt + 1) * 128, :].rearrange("p (k t) -> p k t", k=16), in_=at_d[tt, :, 0:16, :]),
                    writes=["dram:out"], dma=True, semkey="cp_out")
            return
        out_proj(at_d, g_w_out[0], g_b_out[0], mod_d[l, 2 * D:3 * D], x_src, x_dst)

    def ssm(sub, l, x_src, x_dst, dbg=None):
        w_in = s_w_in[0]
        m_all = A.mark()
        dt_tok = A.alloc("dt_tok", [128, NT, 128], F32)
        dta_tok = A.alloc("dta_tok", [128, NT, 128], F32)
        dta_hi = A.alloc("dta_hi", [128, NT, 128], F32)
        m = A.mark()
        hT = A.alloc("hT", [128, KC, L], BF16)
        prologue(sub, x_src, hT)
        zt = A.alloc("zt", [128, 4, 512], F32)
        state = {"n": 0}

        def evac_z(tt, nb, pt, pb):
            k = state["n"] % 4
            state["n"] += 1
            S.op("scalar", lambda e, k=k, pt=pt: e.activation(out=zt[:, k, :], in_=pt[:, :], func=AF.Silu),
                 reads=[("ps", pb)], writes=[("zt", k)])
            S.op("sync", lambda e, k=k, tt=tt, nb=nb: e.dma_start(
                out=zs_d[tt * 128:(tt + 1) * 128, nb * 512:(nb + 1) * 512], in_=zt[:, k, :]),
                reads=[("zt", k)], writes=["dram:zs"], dma=True, semkey=("zt_st", k))
        linear_tok(hT, w_in, 4096, evac_z, tag="s1")
        S.barrier()
        if dbg == "s1":
            S.op("sync", lambda e: e.dma_start(out=out, in_=zs_d[:, 0:2048]), writes=["dram:out"], dma=True, semkey="cp_out")
            return
        wp = A.alloc("wp2", [128, 2, KC, 512], BF16)
        cw = A.alloc("cw", [128, 48, 5], F32)
        cb = A.alloc("cb", [128, 48], F32)
        dtb = A.alloc("dtb", [128, 1], F32)
        acol = A.alloc("acol", [128, 1], F32)
        xc = A.alloc("xc", [128, 2, L + 4], F32)
        acc = A.alloc("acc", [128, L], F32)
        xo = A.alloc("xo", [128, L], F32)
        bco = A.alloc("bco", [128, 2, L], BF16)
        xtok = A.alloc("xtok", [128, 2, NT, 128], F32)
        btk = A.alloc("btk", [128, 2, NT, 128], BF16)
        S.op("sync", lambda e: e.dma_start(out=cw[:], in_=s_conv_col), writes=["cw"], dma=True)
        S.op("sync", lambda e: e.dma_start(out=cb[:], in_=s_convb_col), writes=["cb"], dma=True)
        S.op("sync", lambda e: e.dma_start(out=dtb[:], in_=s_dtb_col), writes=["dtb"], dma=True)
        S.op("sync", lambda e: e.dma_start(out=acol[:], in_=s_alog_col), writes=["acol"], dma=True)
        S.op("scalar", lambda e: e.activation(out=acol[:], in_=acol[:], func=AF.Exp), reads=["acol"], writes=["acol"])
        S.op("vector", lambda e: e.tensor_scalar(out=acol[:], in0=acol[:], scalar1=-1.0, scalar2=None, op0=ALU.mult),
             reads=["acol"], writes=["acol"])
        for s_ in range(2):
            S.op("vector", lambda e, s_=s_: e.memset(xc[:, s_, :], 0.0), writes=[("xc", s_)])
        for grp in range(13):
            ws_ = grp % 2
            ncol = 512 if grp < 12 else 128
            c0 = 4096 + grp * 512
            S.op("gpsimd", lambda e, ws_=ws_, c0=c0, ncol=ncol: e.dma_start(
                out=wp[:, ws_, :, 0:ncol], in_=w_in[:, c0:c0 + ncol].rearrange("(k p) n -> p k n", p=128)),
                writes=[("wp2", ws_)], dma=True)
            for j in range(ncol // 128):
                ch = grp * 4 + j
                for tb in range(4):
                    for kc in range(KC):
                        S.op("tensor", lambda e, kc=kc, ws_=ws_, j=j, tb=tb: e.matmul(
                            ps[tb][:, :], lhsT=wp[:, ws_, kc, j * 128:(j + 1) * 128], rhs=hT[:, kc, tb * 512:(tb + 1) * 512],
                            start=(kc == 0), stop=(kc == KC - 1)),
                            reads=[("wp2", ws_)] + [("hT", t) for t in range(tb * 4, tb * 4 + 4)], writes=[("ps", tb)])
                if ch < 48:
                    xs_ = ch % 2
                    for tb in range(4):
                        S.op("scalar", lambda e, tb=tb, xs_=xs_: e.copy(out=xc[:, xs_, 2 + tb * 512:2 + (tb + 1) * 512], in_=ps[tb][:, :]),
                             reads=[("ps", tb)], writes=[("xc", xs_)])
                    S.op("vector", lambda e, ch=ch, xs_=xs_: e.tensor_scalar(
                        out=acc[:], in0=xc[:, xs_, 0:L], scalar1=cw[:, ch, 0:1], scalar2=None, op0=ALU.mult),
                        reads=[("xc", xs_), "cw"], writes=["acc"])
                    for k in range(1, 5):
                        S.op("vector", lambda e, ch=ch, xs_=xs_, k=k: e.scalar_tensor_tensor(
                            out=acc[:], in0=xc[:, xs_, k:k + L], scalar=cw[:, ch, k:k + 1], in1=acc[:], op0=ALU.mult, op1=ALU.add),
                            reads=[("xc", xs_), "cw", "acc"], writes=["acc"])
                    if ch < 32:
                        S.op("scalar", lambda e, ch=ch: e.activation(out=xo[:], in_=acc[:], func=AF.Silu, bias=cb[:, ch:ch + 1]),
                             reads=["acc", "cb"], writes=["xo"])
                        ts_ = ch % 2
                        for q in range(4):
                            pb = 4 + q
                            for jj in range(4):
                                tt = q * 4 + jj
                                S.op("tensor", lambda e, tt=tt, pb=pb, jj=jj: e.transpose(
                                    ps[pb][:, jj * 128:(jj + 1) * 128], xo[:, tt * 128:(tt + 1) * 128], ident),
                                    reads=["xo", "cst"], writes=[("ps", pb)])
                            S.op("vector", lambda e, q=q, pb=pb, ts_=ts_: e.tensor_copy(
                                out=xtok[:, ts_, q * 4:(q + 1) * 4, :].rearrange("p a b -> p (a b)"), in_=ps[pb][:, :]),
                                reads=[("ps", pb)], writes=[("xtok", ts_)])
                        S.op("sync", lambda e, ch=ch, ts_=ts_: e.dma_start(
                            out=xsd[:, ch * 128:(ch + 1) * 128].rearrange("(t p) f -> p t f", p=128), in_=xtok[:, ts_]),
                            reads=[("xtok", ts_)], writes=["dram:xsd"], dma=True, semkey=("xtok_st", ts_))
                    else:
                        bs_ = ch % 2
                        S.op("scalar", lambda e, ch=ch, bs_=bs_: e.activation(out=bco[:, bs_, :], in_=acc[:], func=AF.Silu, bias=cb[:, ch:ch + 1]),
                             reads=["acc", "cb"], writes=[("bco", bs_)])
                        S.op("sync", lambda e, ch=ch, bs_=bs_: e.dma_start(out=bct_d[ch - 32], in_=bco[:, bs_, :]),
                             reads=[("bco", bs_)], writes=["dram:bct"], dma=True, semkey=("bco_st", bs_))
                        if ch < 40:
                            for q in range(2):
                                pb = 4 + q
                                for jj in range(8):
                                    tt = q * 8 + jj
                                    S.op("tensor", lambda e, tt=tt, pb=pb, jj=jj, bs_=bs_: e.transpose(
                                        psb[pb][:, jj * 128:(jj + 1) * 128], bco[:, bs_, tt * 128:(tt + 1) * 128], identb[:]),
                                        reads=[("bco", bs_), "identb"], writes=[("ps", pb)])
                                S.op("vector", lambda e, q=q, pb=pb, bs_=bs_: e.tensor_copy(
                                    out=btk[:, bs_, q * 8:(q + 1) * 8, :].rearrange("p a b -> p (a b)"), in_=psb[pb][:, :]),
                                    reads=[("ps", pb)], writes=[("btk", bs_)])
                            S.op("sync", lambda e, ch=ch, bs_=bs_: e.dma_start(
                                out=btok_d[:, (ch - 32) * 128:(ch - 31) * 128].rearrange("(t p) f -> p t f", p=128), in_=btk[:, bs_]),
                                reads=[("btk", bs_)], writes=["dram:btok"], dma=True, semkey=("btk_st", bs_))
                else:
                    for tb in range(4):
                        S.op("scalar", lambda e, tb=tb: e.activation(out=acc[:, tb * 512:(tb + 1) * 512], in_=ps[tb][:, :], func=AF.Exp, bias=dtb[:, 0:1]),
                             reads=[("ps", tb), "dtb"], writes=["acc"])
                    S.op("scalar", lambda e: e.activation(out=xo[:], in_=acc[:], func=AF.Ln, bias=1.0), reads=["acc"], writes=["xo"])
                    S.op("vector", lambda e: e.tensor_scalar(out=acc[:], in0=xo[:], scalar1=acol[:, 0:1], scalar2=None, op0=ALU.mult),
                         reads=["xo", "acol"], writes=["acc"])
                    for src, dstt, nm in ((xo, dt_tok, "dt_tok"), (acc, dta_tok, "dta_tok")):
                        for q in range(4):
                            pb = 4 + q
                            for jj in range(4):
                                tt = q * 4 + jj
                                S.op("tensor", lambda e, tt=tt, pb=pb, jj=jj, src=src: e.transpose(
                                    ps[pb][:, jj * 128:(jj + 1) * 128], src[:, tt * 128:(tt + 1) * 128], ident),
                                    reads=["xo", "acc", "cst"], writes=[("ps", pb)])
                            S.op("vector", lambda e, q=q, pb=pb, dstt=dstt: e.tensor_copy(
                                out=dstt[:, q * 4:(q + 1) * 4, :].rearrange("p a b -> p (a b)"), in_=ps[pb][:, :]),
                                reads=[("ps", pb)], writes=[nm])
        hbt = A.alloc("hbt", [128, NT, 128], BF16)
        S.op("vector", lambda e: e.tensor_copy(out=hbt[:], in_=dta_tok[:]), reads=["dta_tok"], writes=["hbt"])
        S.op("vector", lambda e: e.tensor_copy(out=dta_hi[:], in_=hbt[:]), reads=["hbt"], writes=["dta_hi"])
        S.op("vector", lambda e: e.tensor_tensor(out=dta_tok[:], in0=dta_tok[:], in1=dta_hi[:], op=ALU.subtract),
             reads=["dta_tok", "dta_hi"], writes=["dta_tok"])
        S.barrier()
        A.reset(m)
        if dbg in ("s2", "s2b"):
            if dbg == "s2":
                S.op("sync", lambda e: e.dma_start(out=out, in_=xsd[:, 0:2048]), writes=["dram:out"], dma=True, semkey="cp_out")
            else:
                S.op("gpsimd", lambda e: e.dma_start(out=out.rearrange("(c p) t -> c p t", p=128), in_=bct_d), writes=["dram:out"], dma=True, semkey="cp_out")
            return
        Et = A.alloc("Et", [128, NT, 128], F32)
        DTEt = A.alloc("DTEt", [128, NT, 128], F32)
        CDt = A.alloc("CDt", [128, NT, 128], F32)
        tmpc = A.alloc("tmpc", [128, 128], F32)
        hlb = A.alloc("hlb", [128, 2, 128], BF16)
        import os
        for tt in range(NT if not os.environ.get("SSM_SKIP_PRE") else 0):
            S.op("vector", lambda e, tt=tt: e.tensor_copy(out=hlb[:, 0, :], in_=dta_hi[:, tt, :]), reads=["dta_hi"], writes=["hlb"])
            S.op("vector", lambda e, tt=tt: e.tensor_copy(out=hlb[:, 1, :], in_=dta_tok[:, tt, :]), reads=["dta_tok"], writes=["hlb"])
            for pi in range(2):
                S.op("tensor", lambda e, pi=pi: e.matmul(ps[0][:, 0:64], lhsT=tri_le_b, rhs=hlb[:, pi, 0:64], start=(pi == 0), stop=(pi == 1)),
                     reads=["hlb", "cstb"], writes=[("ps", 0)])
            for pi in range(2):
                S.op("tensor", lambda e, pi=pi: e.matmul(ps[0][:, 64:128], lhsT=tri_ge_b, rhs=hlb[:, pi, 64:128], start=(pi == 0), stop=(pi == 1)),
                     reads=["hlb", "cstb"], writes=[("ps", 0)])
            for pi in range(2):
                S.op("tensor", lambda e, pi=pi: e.matmul(ps[1][:, 0:128], lhsT=ones_b, rhs=hlb[:, pi, :], start=(pi == 0), stop=(pi == 1)),
                     reads=["hlb", "cstb"], writes=[("ps", 1)])
            PL = int(os.environ.get("PRE_LEVEL", "9"))
            if PL < 2:
                continue
            S.op("scalar", lambda e, tt=tt: e.activation(out=Et[:, tt, :], in_=ps[0][:, 0:128], func=AF.Exp), reads=[("ps", 0)], writes=["Et"])
            S.op("scalar", lambda e, tt=tt: e.activation(out=CDt[:, tt, :], in_=ps[1][:, 0:128], func=AF.Exp), reads=[("ps", 1)], writes=["CDt"])
            for pi in range(2):
                S.op("tensor", lambda e, pi=pi: e.matmul(ps[2][:, 0:64], lhsT=cstb[:, 3, :], rhs=hlb[:, pi, 0:64], start=(pi == 0), stop=(pi == 1)),
                     reads=["hlb", "cstb"], writes=[("ps", 2)])
            for pi in range(2):
                S.op("tensor", lambda e, pi=pi: e.matmul(ps[2][:, 64:128], lhsT=cstb[:, 4, :], rhs=hlb[:, pi, 64:128], start=(pi == 0), stop=(pi == 1)),
                     reads=["hlb", "cstb"], writes=[("ps", 2)])
            S.op("scalar", lambda e, tt=tt: e.activation(out=DTEt[:, tt, :], in_=ps[2][:, 0:128], func=AF.Exp), reads=[("ps", 2)], writes=["DTEt"])
        import os
        STOP = int(os.environ.get("SSM_STOP", "0"))
        if STOP == 1:
            S.barrier(); return
        BT = A.alloc("BT", [128, L], BF16); CT = A.alloc("CT", [128, L], BF16)
        Btok = A.alloc("Btok", [128, NT, 128], BF16)
        xsg = A.alloc("xsg", [128, NT, 512], F32)
        dtx1 = A.alloc("dtx1", [128, NT, 512], BF16)
        dtx = [dtx1, dtx1]
        yacc = A.alloc("yacc_s", [128, NT, 512], F32)
        SM = [A.alloc("SMF", [128, NT, 128], F32), A.alloc("SMB", [128, NT, 128], F32)]
        prevT = A.alloc("prevT", [128, 512], F32); prevB = A.alloc("prevB", [128, 512], BF16)
        LW = A.alloc("LW", [128, 2, 2, 4, 128], BF16)
        EX = A.alloc("EX", [128, 2, 512], F32)
        MT = A.alloc("MT", [128, 2, 4, 128], BF16)
        t1 = A.alloc("t1", [128, 512], F32)
        dtxe = A.alloc("dtxe", [128, 512], BF16)
        zst = A.alloc("zst", [128, 2, 512], F32)
        ut = A.alloc("ut_s", [128, 512], F32); usq = t1
        vb = A.alloc("vb", [128, 512], BF16)
        vT = A.alloc("vT", [128, 2, 4, 128], BF16)
        Dbc = A.alloc("Dbc", [128, 512], F32); ngb = A.alloc("ngb", [128, 512], F32)
        stt = A.alloc("stt", [128, 2, 4], F32)
        nlw = 0
        for g in range(8):
            S.op("sync", lambda e, g=g: e.dma_start(out=BT[:], in_=bct_d[g]), reads=["dram:bct"], writes=["BT"], dma=True)
            S.op("sync", lambda e, g=g: e.dma_start(out=CT[:], in_=bct_d[8 + g]), reads=["dram:bct"], writes=["CT"], dma=True)
            S.op("sync", lambda e, g=g: e.dma_start(out=Btok[:], in_=btok_d[:, g * 128:(g + 1) * 128].rearrange("(t p) n -> p t n", p=128)),
                 reads=["dram:btok"], writes=["Btok"], dma=True)
            S.op("sync", lambda e, g=g: e.dma_start(out=xsg[:], in_=xsd[:, g * 512:(g + 1) * 512].rearrange("(t p) n -> p t n", p=128)),
                 reads=["dram:xsd"], writes=["xsg"], dma=True)
            S.op("sync", lambda e, g=g: e.dma_start(out=Dbc[:], in_=s_d_rep[g * 512:(g + 1) * 512].partition_broadcast(128)), writes=["Dbc"], dma=True)
            S.op("sync", lambda e, g=g: e.dma_start(out=ngb[:], in_=s_norm_g[g * 512:(g + 1) * 512].partition_broadcast(128)), writes=["ngb"], dma=True)
            for c in range(NT):
                S.op("vector", lambda e, c=c: e.tensor_tensor(out=yacc[:, c, :], in0=xsg[:, c, :], in1=Dbc[:], op=ALU.mult),
                     reads=["xsg", "Dbc"], writes=[("yacc_s", c)])
                S.op("tensor", lambda e, c=c: e.matmul(ps[7][:, 0:128], lhsT=BT[:, c * 128:(c + 1) * 128], rhs=CT[:, c * 128:(c + 1) * 128],
                                                        start=True, stop=True), reads=["BT", "CT"], writes=[("ps", 7)])
                S.op("vector", lambda e, c=c: e.tensor_tensor(out=SM[0][:, c, :], in0=ps[7][:, 0:128], in1=tri_le, op=ALU.mult),
                     reads=[("ps", 7), "cst"], writes=[("SM", 0, c)])
                S.op("vector", lambda e, c=c: e.tensor_tensor(out=SM[1][:, c, :], in0=ps[7][:, 0:128], in1=tri_ge, op=ALU.mult),
                     reads=[("ps", 7), "cst"], writes=[("SM", 1, c)])
            if STOP == 2:
                S.barrier(); return
            for d_ in range(2):
                if STOP == 3 and d_ == 1:
                    S.barrier(); return
                order = list(range(NT)) if d_ == 0 else list(range(NT - 1, -1, -1))
                umask = u_gt if d_ == 0 else u_lt
                trimb = tri_le_b if d_ == 0 else tri_ge_b
                hb = d_ * 64 + g * 8
                for c in range(NT):
                    S.op("vector", lambda e, c=c, d_=d_, hb=hb: e.tensor_tensor(
                        out=dtx[d_][:, c, :].rearrange("p (h d) -> p h d", h=8), in0=xsg[:, c, :].rearrange("p (h d) -> p h d", h=8),
                        in1=dt_tok[:, c, hb:hb + 8].unsqueeze(2).to_broadcast([128, 8, 64]), op=ALU.mult),
                        reads=["xsg", "dt_tok"], writes=[("dtx", c)])
                for ci, c in enumerate(order):
                    for hh in range(2):
                        ls = nlw % 2
                        nlw += 1
                        for h in range(4):
                            col = hb + hh * 4 + h
                            for pi, part in enumerate((dta_hi, dta_tok)):
                                S.op("scalar", lambda e, c=c, col=col, ls=ls, h=h, umask=umask, pi=pi, part=part: e.activation(
                                    out=LW[:, ls, pi, h, :], in_=umask, func=AF.Copy, scale=part[:, c, col:col + 1]),
                                    reads=["cst", "dta_tok", "dta_hi"], writes=[("LW", ls)])
                        pseg = 0 + ls
                        for h in range(4):
                            for pi in range(2):
                                S.op("tensor", lambda e, ls=ls, h=h, pseg=pseg, trimb=trimb, pi=pi: e.matmul(
                                    ps[pseg][:, h * 128:(h + 1) * 128], lhsT=LW[:, ls, pi, h, :], rhs=trimb,
                                    start=(pi == 0), stop=(pi == 1)),
                                    reads=[("LW", ls), "cstb"], writes=[("ps", pseg)])
                        S.op("scalar", lambda e, ls=ls, pseg=pseg: e.activation(out=EX[:, ls, :], in_=ps[pseg][:, :], func=AF.Exp),
                             reads=[("ps", pseg)], writes=[("EX", ls)])
                        S.op("vector", lambda e, ls=ls, c=c, d_=d_: e.tensor_tensor(
                            out=MT[:, ls], in0=EX[:, ls, :].rearrange("p (h i) -> p h i", h=4),
                            in1=SM[d_][:, c, :].unsqueeze(1).to_broadcast([128, 4, 128]), op=ALU.mult),
                            reads=[("EX", ls), ("SM", d_, c)], writes=[("MT", ls)])
                        py = 2 if ci % 2 == 0 else 4
                        for h in range(4):
                            hd = hh * 4 + h
                            S.op("tensor", lambda e, ls=ls, h=h, hd=hd, c=c, d_=d_, py=py: e.matmul(
                                ps[py][:, hd * 64:(hd + 1) * 64], lhsT=MT[:, ls, h, :], rhs=dtx[d_][:, c, hd * 64:(hd + 1) * 64],
                                start=True, stop=True),
                                reads=[("MT", ls), ("dtx", c)], writes=[("ps", py)])
                    if ci > 0:
                        S.op("tensor", lambda e, c=c: e.matmul(ps[3][:, :], lhsT=CT[:, c * 128:(c + 1) * 128], rhs=prevB[:], start=True, stop=True),
                             reads=["CT", "prevB"], writes=[("ps", 3)])
                        S.op("vector", lambda e, c=c, hb=hb: e.tensor_tensor(
                            out=t1[:].rearrange("p (h d) -> p h d", h=8), in0=ps[3][:, :].rearrange("p (h d) -> p h d", h=8),
                            in1=Et[:, c, hb:hb + 8].unsqueeze(2).to_broadcast([128, 8, 64]), op=ALU.mult),
                            reads=[("ps", 3), "Et"], writes=["t1"])
                        S.op("vector", lambda e, c=c: e.tensor_tensor(out=t1[:], in0=t1[:], in1=yacc[:, c, :], op=ALU.add),
                             reads=["t1", ("yacc_s", c)], writes=["t1"])
                        S.op("vector", lambda e, c=c, py=py: e.tensor_tensor(out=yacc[:, c, :], in0=ps[py][:, :], in1=t1[:], op=ALU.add),
                             reads=[("ps", py), "t1"], writes=[("yacc_s", c)])
                    else:
                        S.op("vector", lambda e, c=c, py=py: e.tensor_tensor(out=yacc[:, c, :], in0=ps[py][:, :], in1=yacc[:, c, :], op=ALU.add),
                             reads=[("ps", py), ("yacc_s", c)], writes=[("yacc_s", c)])
                    if ci < NT - 1:
                        S.op("vector", lambda e, c=c, d_=d_, hb=hb: e.tensor_tensor(
                            out=dtxe[:].rearrange("p (h d) -> p h d", h=8), in0=dtx[d_][:, c, :].rearrange("p (h d) -> p h d", h=8),
                            in1=DTEt[:, c, hb:hb + 8].unsqueeze(2).to_broadcast([128, 8, 64]), op=ALU.mult),
                            reads=[("dtx", c), "DTEt"], writes=["dtxe"])
                        S.op("tensor", lambda e, c=c: e.matmul(ps[6][:, :], lhsT=Btok[:, c, :], rhs=dtxe[:], start=True, stop=True),
                             reads=["Btok", "dtxe"], writes=[("ps", 6)])
                        if ci == 0:
                            S.op("vector", lambda e: e.tensor_copy(out=prevT[:], in_=ps[6][:, :]), reads=[("ps", 6)], writes=["prevT"])
                        else:
                            S.op("vector", lambda e, c=c, hb=hb: e.tensor_tensor(
                                out=prevT[:].rearrange("p (h d) -> p h d", h=8), in0=prevT[:].rearrange("p (h d) -> p h d", h=8),
                                in1=CDt[:, c, hb:hb + 8].unsqueeze(2).to_broadcast([128, 8, 64]), op=ALU.mult),
                                reads=["prevT", "CDt"], writes=["prevT"])
                            S.op("vector", lambda e: e.tensor_tensor(out=prevT[:], in0=prevT[:], in1=ps[6][:, :], op=ALU.add),
                                 reads=["prevT", ("ps", 6)], writes=["prevT"])
                        S.op("scalar", lambda e: e.copy(out=prevB[:], in_=prevT[:]), reads=["prevT"], writes=["prevB"])
            if STOP == 4:
                S.barrier(); return
            for c in range(NT):
                zs_ = c % 2
                S.op("sync", lambda e, c=c, g=g, zs_=zs_: e.dma_start(out=zst[:, zs_, :], in_=zs_d[c * 128:(c + 1) * 128, g * 512:(g + 1) * 512]),
                     reads=["dram:zs"], writes=[("zst", zs_)], dma=True)
                S.op("vector", lambda e, c=c, zs_=zs_: e.tensor_tensor(out=ut[:], in0=yacc[:, c, :], in1=zst[:, zs_, :], op=ALU.mult),
                     reads=[("yacc_s", c), ("zst", zs_)], writes=["ut_s"])
                S.op("vector", lambda e: e.tensor_tensor(out=usq[:], in0=ut[:], in1=ut[:], op=ALU.mult), reads=["ut_s"], writes=["t1"])
                S.op("vector", lambda e, zs_=zs_: e.reduce_sum(out=stt[:, zs_, 0:1], in_=usq[:], axis=AX.X), reads=["t1"], writes=[("stt", zs_)])
                S.op("vector", lambda e, zs_=zs_: e.tensor_scalar(out=stt[:, zs_, 1:2], in0=stt[:, zs_, 0:1], scalar1=1.0 / 512, scalar2=1e-5,
                                                                  op0=ALU.mult, op1=ALU.add), reads=[("stt", zs_)], writes=[("stt", zs_)])
                S.op("scalar", lambda e, zs_=zs_: e.activation(out=stt[:, zs_, 2:3], in_=stt[:, zs_, 1:2], func=AF.Sqrt),
                     reads=[("stt", zs_)], writes=[("stt", zs_)])
                S.op("vector", lambda e, zs_=zs_: e.reciprocal(out=stt[:, zs_, 3:4], in_=stt[:, zs_, 2:3]), reads=[("stt", zs_)], writes=[("stt", zs_)])
                S.op("vector", lambda e, zs_=zs_: e.scalar_tensor_tensor(out=vb[:], in0=ut[:], scalar=stt[:, zs_, 3:4], in1=ngb[:],
                                                                          op0=ALU.mult, op1=ALU.mult),
                     reads=["ut_s", ("stt", zs_), "ngb"], writes=["vb"])
                for j in range(4):
                    S.op("tensor", lambda e, j=j: e.transpose(psb[5][:, j * 128:(j + 1) * 128], vb[:, j * 128:(j + 1) * 128], identb[:]),
                         reads=["vb", "identb"], writes=[("ps", 5)])
                S.op("scalar", lambda e, zs_=zs_: e.copy(out=vT[:, zs_].rearrange("p a b -> p (a b)"), in_=psb[5][:, 0:512]),
                     reads=[("ps", 5)], writes=[("vT", zs_)])
                S.op("sync", lambda e, c=c, g=g, zs_=zs_: e.dma_start(out=at_d[c, :, g * 4:(g + 1) * 4, :], in_=vT[:, zs_]),
                     reads=[("vT", zs_)], writes=["dram:at"], dma=True, semkey=("vT_st", zs_))
        S.barrier()
        A.reset(m_all)
        if dbg == "s3":
            for tt in range(NT):
                S.op("gpsimd", lambda e, tt=tt: e.dma_start(
                    out=out[tt * 128:(tt + 1) * 128, :].rearrange("p (k t) -> p k t", k=16), in_=at_d[tt, :, 0:16, :]),
                    writes=["dram:out"], dma=True, semkey="cp_out")
            return
        out_proj(at_d, s_w_out[0], None, mod_d[l, 2 * D:3 * D], x_src, x_dst)

    def moe(sub, l, x_src, x_dst):
        TBS = 8
        NTOK = TBS * 128
        NBLK = NT // TBS
        m = A.mark()
        hTb = A.alloc("hTb", [128, KC, NTOK], BF16)
        gTh = A.alloc("gTh", [32, NTOK], BF16)
        wrh = A.alloc("wrh", [128, KC, 32], BF16)
        wrl = A.alloc("wrl", [128, KC, 32], BF16)
        brbc = A.alloc("brbc", [128, 32], F32)
        bupe = A.alloc("bupe", [128, 2, 16, 2], F32)
        bdnb = A.alloc("bdnb", [32, D], BF16)
        wup = A.alloc("wup", [128, 2, KC, 256], BF16)
        wdn = A.alloc("wdn", [128, 2, KC, 512], BF16)
        yacc = A.alloc("yacc", [128, TBS, D], F32)
        gb = A.alloc("gb", [128, 2, NTOK], F32)
        MP = A.mark()
        actT = A.alloc("actT", [128, KC, NTOK], BF16)
        tg = A.alloc("tg", [128, 512], F32)
        tsg = A.alloc("tsg", [128, 512], F32)
        tl_ = A.alloc("tl_", [128, 512], F32)
        TOP = A.mark()
        A.reset(MP)
        wr = A.alloc("wr", [128, KC, 32], F32)
        S.op("sync", lambda e: e.dma_start(out=wr[:], in_=m_wr_col[:, l]), writes=["wr0"], dma=True)
        S.op("vector", lambda e: e.tensor_copy(out=wrh[:], in_=wr[:]), reads=["wr0"], writes=["wr"])
        S.op("vector", lambda e: e.tensor_tensor(out=wrl[:], in0=wr[:], in1=wrh[:], op=ALU.subtract), reads=["wr0", "wr"], writes=["wr"])
        S.op("sync", lambda e: e.dma_start(out=brbc[:], in_=m_br[l].partition_broadcast(128)), writes=["brbc"], dma=True)
        S.op("gpsimd", lambda e: e.dma_start(out=bdnb[:], in_=m_b_down[l]), writes=["bdn"], dma=True)
        S.barrier()
        nup = 0
        ndn = 0
        for tb in range(min(NBLK, DBG_TB)):
            A.reset(MP)
            gT = A.alloc("gT", [32, NTOK], F32)
            prologue(sub, x_src, hTb, tiles=[tb * TBS + i for i in range(TBS)], router=(wrh, wrl, brbc, gT))
            S.op("vector", lambda e: e.tensor_copy(out=gTh[:], in_=gT[:]), reads=["gT"], writes=["gTh"])
            S.op("sync", lambda e: e.dma_start(out=gt_d, in_=gT[:]), reads=["gT"], writes=["dram:gt"], dma=True, semkey="gt_st")
            S.barrier()
            A.reset(TOP)
            for tl in range(TBS):
                for db in range(4):
                    pb = 6 + (tl * 4 + db) % 2
                    S.op("tensor", lambda e, tl=tl, db=db, pb=pb: e.matmul(
                        ps[pb][:, :], lhsT=gTh[:, tl * 128:(tl + 1) * 128], rhs=bdnb[:, db * 512:(db + 1) * 512],
                        start=True, stop=True), reads=["gTh", "bdn"], writes=[("ps", pb)])
                    S.op("vector", lambda e, tl=tl, db=db, pb=pb: e.tensor_copy(
                        out=yacc[:, tl, db * 512:(db + 1) * 512], in_=ps[pb][:, :]),
                        reads=[("ps", pb)], writes=[("yacc", tl)])
            for ex in range(DBG_NE):
                bs_ = ex % 2
                S.op("sync", lambda e, ex=ex, bs_=bs_: e.dma_start(out=bupe[:, bs_], in_=m_bup_col[:, l, ex]),
                     writes=[("bupe", bs_)], dma=True)
                S.op("sync", lambda e, ex=ex, bs_=bs_: e.dma_start(out=gb[:, bs_, :], in_=gt_d[ex].partition_broadcast(128)),
                     reads=["dram:gt"], writes=[("gb", bs_)], dma=True)
                for fc in range(KC):
                    us = nup % 2
                    nup += 1
                    S.op("gpsimd", lambda e, ex=ex, fc=fc, us=us: e.dma_start(
                        out=wup[:, us], in_=m_w_up[l][ex, :, fc * 256:(fc + 1) * 256].rearrange("(k p) n -> p k n", p=128)),
                        writes=[("wup", us)], dma=True)
                    for hf_ in range(NTOK // 512):
                        pg = hf_ * 2
                        pl = pg + 1
                        for half, pbank in ((0, pg), (1, pl)):
                            for kc in range(KC):
                                S.op("tensor", lambda e, kc=kc, us=us, half=half, pbank=pbank, hf_=hf_: e.matmul(
                                    ps[pbank][:, :], lhsT=wup[:, us, kc, half::2], rhs=hTb[:, kc, hf_ * 512:(hf_ + 1) * 512],
                                    start=(kc == 0), stop=(kc == KC - 1)),
                                    reads=[("wup", us)] + [("hT", t) for t in range(hf_ * 4, hf_ * 4 + 4)], writes=[("ps", pbank)])
                        S.op("vector", lambda e, bs_=bs_, fc=fc, pg=pg: e.tensor_scalar(
                            out=tg[:], in0=ps[pg][:, :], scalar1=bupe[:, bs_, fc, 0:1], scalar2=7.0, op0=ALU.add, op1=ALU.min),
                            reads=[("ps", pg), ("bupe", bs_)], writes=["tg"])
                        S.op("scalar", lambda e: e.activation(out=tsg[:], in_=tg[:], func=AF.Sigmoid, scale=1.702),
                             reads=["tg"], writes=["tsg"])
                        S.op("vector", lambda e, bs_=bs_, fc=fc, pl=pl: e.tensor_scalar(
                            out=tl_[:], in0=ps[pl][:, :], scalar1=bupe[:, bs_, fc, 1:2], scalar2=7.0, op0=ALU.add, op1=ALU.min),
                            reads=[("ps", pl), ("bupe", bs_)], writes=["tl_"])
                        S.op("vector", lambda e: e.tensor_scalar(
                            out=tl_[:], in0=tl_[:], scalar1=-7.0, scalar2=1.0, op0=ALU.max, op1=ALU.add),
                            reads=["tl_"], writes=["tl_"])
                        S.op("vector", lambda e: e.tensor_tensor(out=tg[:], in0=tg[:], in1=tsg[:], op=ALU.mult),
                             reads=["tg", "tsg"], writes=["tg"])
                        S.op("vector", lambda e, hf_=hf_, bs_=bs_: e.tensor_tensor(
                            out=tl_[:], in0=tl_[:], in1=gb[:, bs_, hf_ * 512:(hf_ + 1) * 512], op=ALU.mult),
                            reads=["tl_", ("gb", bs_)], writes=["tl_"])
                        S.op("vector", lambda e, fc=fc, hf_=hf_: e.tensor_tensor(
                            out=actT[:, fc, hf_ * 512:(hf_ + 1) * 512], in0=tg[:], in1=tl_[:], op=ALU.mult),
                            reads=["tg", "tl_"], writes=[("actT", fc, hf_)])
                for db in range(4):
                    ds_ = ndn % 2
                    ndn += 1
                    S.op("gpsimd", lambda e, ex=ex, db=db, ds_=ds_: e.dma_start(
                        out=wdn[:, ds_], in_=m_w_down[l][ex, :, db * 512:(db + 1) * 512].rearrange("(k p) n -> p k n", p=128)),
                        writes=[("wdn", ds_)], dma=True)
                    for tl in range(TBS):
                        pb = 6 + (db * TBS + tl) % 2
                        for fc in range(KC):
                            S.op("tensor", lambda e, fc=fc, tl=tl, ds_=ds_, pb=pb: e.matmul(
                                ps[pb][:, :], lhsT=actT[:, fc, tl * 128:(tl + 1) * 128], rhs=wdn[:, ds_, fc, :],
                                start=(fc == 0), stop=(fc == KC - 1)),
                                reads=[("actT", fc, tl // 4), ("wdn", ds_)], writes=[("ps", pb)])
                        S.op("vector", lambda e, tl=tl, db=db, pb=pb: e.tensor_tensor(
                            out=yacc[:, tl, db * 512:(db + 1) * 512], in0=yacc[:, tl, db * 512:(db + 1) * 512],
                            in1=ps[pb][:, :], op=ALU.add),
                            reads=[("ps", pb), ("yacc", tl)], writes=[("yacc", tl)])
            S.barrier()
            A.reset(MP)
            g2bc = A.alloc("g2bc", [128, D], F32)
            xe = A.alloc("xe", [128, D], F32)
            S.op("sync", lambda e: e.dma_start(out=g2bc[:], in_=mod_d[l, 5 * D:6 * D].partition_broadcast(128)),
                 writes=["g2bc"], dma=True)
            for tl in range(TBS):
                tt = tb * TBS + tl
                S.op("sync", lambda e, tt=tt: e.dma_start(out=xe[:], in_=x_src[tt * 128:(tt + 1) * 128, :]),
                     writes=["xe"], dma=True)
                S.op("vector", lambda e, tl=tl: e.tensor_tensor(out=yacc[:, tl, :], in0=yacc[:, tl, :], in1=g2bc[:], op=ALU.mult),
                     reads=[("yacc", tl), "g2bc"], writes=[("yacc", tl)])
                S.op("vector", lambda e, tl=tl: e.tensor_tensor(out=xe[:], in0=xe[:], in1=yacc[:, tl, :], op=ALU.add),
                     reads=[("yacc", tl), "xe"], writes=["xe"])
                S.op("sync", lambda e, tt=tt: e.dma_start(out=x_dst[tt * 128:(tt + 1) * 128, :], in_=xe[:]),
                     reads=["xe"], writes=["dram:xdst"], dma=True, semkey="xe_st")
            S.barrier()
        S.barrier()
        A.reset(m)

    cur = x_in
    nxt = 0
    for stage in flow:
        if stage == "final":
            final_norm(cur)
            cur = None
            continue
        dst = xs_d[nxt]
        nxt += 1
        if stage.startswith("gmlp"):
            dbg = stage.split(":")[1] if ":" in stage else None
            gmlp(2, 1, cur, dst, dbg=dbg)
            if dbg:
                cur = None
                break
        elif stage == "moe0":
            moe(1, 0, cur, dst)
        elif stage == "moe1":
            moe(3, 1, cur, dst)
        elif stage.startswith("ssm"):
            dbg = stage.split(":")[1] if ":" in stage else None
            ssm(0, 0, cur, dst, dbg=dbg)
            if dbg:
                cur = None
                break
        cur = dst
        S.barrier()
    if cur is not None:
        S.op("sync", lambda e: e.dma_start(out=out, in_=cur), writes=["dram:out"], dma=True, semkey="cp_out")
    S.barrier()
    S.emit(stack)
    stack.close()
    return nc


def host_inputs(inputs, b, flow=None):
    f = lambda a: np.ascontiguousarray(a, dtype=np.float32)
    i = np.arange(128)
    consts = np.stack([
        np.eye(128),
        (i[:, None] <= i[None, :]), (i[:, None] >= i[None, :]),
        (i[:, None] > i[None, :]), (i[:, None] < i[None, :]),
        np.ones((128, 128)),
    ]).astype(np.float32)
    ng = np.stack([inputs["norm_mix_g"][0], inputs["norm_ffn_g"][0], inputs["norm_mix_g"][1], inputs["norm_ffn_g"][1]])
    m = {
        "x": f(inputs["x"][b]),
        "c_col": f(inputs["c"][b].reshape(KC, 128).T),
        "consts": consts,
        "ada_w": f(inputs["ada_w"]),
        "ada_b": f(inputs["ada_b"]),
        "ng_col": f(ng.reshape(4, KC, 128).transpose(2, 0, 1)),
        "final_g": f(inputs["final_g"]),
        "s_w_in": f(inputs["ssm_w_in"]),
        "s_conv_col": f(inputs["ssm_conv_w"][0].reshape(5, 48, 128).transpose(2, 1, 0)),
        "s_convb_col": f(inputs["ssm_conv_b"][0].reshape(48, 128).T),
        "s_dtb_col": f(inputs["ssm_dt_bias"][0].reshape(128, 1)),
        "s_alog_col": f(inputs["ssm_a_log"][0].reshape(128, 1)),
        "s_d_rep": f(np.repeat(inputs["ssm_d"][0], 64)),
        "s_norm_g": f(inputs["ssm_norm_g"][0]),
        "s_w_out": f(inputs["ssm_w_out"]),
        "g_w_in": f(inputs["gmlp_w_in"]), "g_b_in": f(inputs["gmlp_b_in"]),
        "g_ln_g": f(inputs["gmlp_ln_g"]), "g_ln_b": f(inputs["gmlp_ln_b"]),
        "g_w_s": f(inputs["gmlp_w_s"]), "g_bs_col": f(inputs["gmlp_b_s"][0].T),
        "g_w_out": f(inputs["gmlp_w_out"]), "g_b_out": f(inputs["gmlp_b_out"]),
        "m_wr_col": f(inputs["moe_w_router"].reshape(2, KC, 128, 32).transpose(2, 0, 1, 3)),
        "m_br": f(inputs["moe_b_router"]),
        "m_bup_col": f(inputs["moe_b_up"].reshape(2, 32, 16, 128, 2).transpose(3, 0, 1, 2, 4)),
        "m_b_down": f(inputs["moe_b_down"]),
    }
    if flow is None or any(st.startswith("moe") for st in flow):
        for i in range(2):
            m["m_w_up%d" % i] = f(inputs["moe_w_up"][i, :DBG_NE])
            m["m_w_down%d" % i] = f(inputs["moe_w_down"][i, :DBG_NE])
    return m


def kernel(**inputs):
    n = 8
    nc = build_program()
    in_maps = [host_inputs(inputs, b) for b in range(n)]
    res = run_bass_kernel_spmd(nc, in_maps, core_ids=list(range(n)))
    return np.stack([r["out"] for r in res.results], axis=0)
```
